# Optimizing a Trainium2 kernel written in Bass

```python
import math
import jax, jax.numpy as jnp
from jax import lax
import numpy as np

D_MODEL = 1024
BATCH = 4
SEQ = 8192
DEPTH = 2

MOBA_HEAD_DIM = 128
MOBA_HEADS = D_MODEL // (2 * MOBA_HEAD_DIM)
MOBA_BLOCK = 256
MOBA_TOPK = 3
MOBA_QBLOCK = 32
GLA_DV = 128
GLA_HEADS = D_MODEL // (2 * GLA_DV)
GLA_DK = GLA_DV // 2
GLA_GATE_RANK = 16
GLA_GATE_NORM = 16.0
GLA_CHUNK = 64
GDN_DK = 128
GDN_DV = 128
GDN_HEADS = D_MODEL // GDN_DV
GDN_CONV = 4
GDN_CHUNK = 64
RPE_BUCKETS = 32
RPE_MAX_DIST = 2048
N_EXPERTS = 64
TOP_K = 6
N_GROUPS = 8
TOPK_GROUPS = 4
D_EXPERT = 256
D_SHARED = 256
ROUTED_SCALE = 2.5
MOE_BLOCK = 128
DEEPNORM_ALPHA = float((2 * DEPTH) ** 0.25)
DEEPNORM_BETA = float((8 * DEPTH) ** -0.25)
LN_EPS = 1e-5
NORM_EPS = 1e-6
N_EVEN = (DEPTH + 1) // 2
N_ODD = DEPTH // 2

MOBA_W = MOBA_HEADS * MOBA_HEAD_DIM
GLA_QK_W = GLA_HEADS * GLA_DK
GLA_V_W = GLA_HEADS * GLA_DV
EVEN_SPLITS = (MOBA_W, MOBA_W, MOBA_W, GLA_QK_W, GLA_QK_W, GLA_V_W, GLA_V_W, GLA_GATE_RANK)
EVEN_IN = sum(EVEN_SPLITS)
EVEN_MIX = MOBA_W + GLA_V_W
GDN_QK_W = GDN_HEADS * GDN_DK
GDN_V_W = GDN_HEADS * GDN_DV
GDN_QKV_W = 2 * GDN_QK_W + GDN_V_W
ODD_SPLITS = (GDN_QK_W, GDN_QK_W, GDN_V_W, GDN_V_W, GDN_HEADS, GDN_HEADS)
ODD_IN = sum(ODD_SPLITS)
ODD_MIX = GDN_V_W

kernel_name = "hybrid_moba_gla_gdn_moe_block"

F32 = jnp.float32


def _split(t, sizes):
    return jnp.split(t, np.cumsum(sizes)[:-1].tolist(), axis=-1)


def layer_norm(x, g, b):
    xf = x.astype(F32)
    mu = jnp.mean(xf, -1, keepdims=True)
    var = jnp.mean(jnp.square(xf - mu), -1, keepdims=True)
    return ((xf - mu) * lax.rsqrt(var + LN_EPS)).astype(x.dtype) * g + b


def l2norm(t):
    tf = t.astype(F32)
    return tf * lax.rsqrt(jnp.sum(tf * tf, -1, keepdims=True) + NORM_EPS)


def gated_rmsnorm(o, gate, w):
    of = o.astype(F32)
    of = of * lax.rsqrt(jnp.mean(of * of, -1, keepdims=True) + NORM_EPS)
    return of * w.astype(F32) * jax.nn.silu(gate.astype(F32))


def rpe_bucket(dist):
    exact = RPE_BUCKETS // 2
    d = jnp.maximum(dist, 0)
    logd = jnp.log(jnp.maximum(d, 1).astype(F32) / exact)
    large = exact + (logd / math.log(RPE_MAX_DIST / exact) * (RPE_BUCKETS - exact)).astype(jnp.int32)
    large = jnp.minimum(large, RPE_BUCKETS - 1)
    return jnp.where(d < exact, d, large)


def causal_dwconv(x, w):
    k = w.shape[0]
    return lax.conv_general_dilated(x, w[:, None, :], window_strides=(1,), padding=[(k - 1, 0)],
                                    dimension_numbers=('NWC', 'WIO', 'NWC'),
                                    feature_group_count=x.shape[-1])


def moba_attention(q, k, v, rpe_bias):
    B, S, H, Dh = q.shape
    nkb = -(-S // MOBA_BLOCK)
    nqb = S // MOBA_QBLOCK
    kk = min(MOBA_TOPK, nkb)
    pad = nkb * MOBA_BLOCK - S

    def blocks(t):
        t = jnp.pad(t, ((0, 0), (0, pad), (0, 0), (0, 0)))
        return t.reshape(B, nkb, MOBA_BLOCK, H, Dh).transpose(0, 3, 1, 2, 4)

    kb, vb = blocks(k), blocks(v)
    k_mean = jnp.mean(kb.astype(F32), axis=3)
    qb = q.reshape(B, nqb, MOBA_QBLOCK, H, Dh).transpose(1, 0, 3, 2, 4)
    rpe_hb = rpe_bias.T.astype(F32)
    scale = Dh ** -0.5
    b_idx = jnp.arange(B)[:, None, None, None]
    h_idx = jnp.arange(H)[None, :, None, None]
    offs = jnp.arange(MOBA_BLOCK)

    def one_block(args):
        qi, qblk = args
        q0 = qi * MOBA_QBLOCK
        qpos = q0 + jnp.arange(MOBA_QBLOCK)
        cur = q0 // MOBA_BLOCK
        gate = jnp.einsum('bhqd,bhnd->bhqn', qblk.astype(F32), k_mean)
        gate = jnp.where(jnp.arange(nkb) < cur, gate, -jnp.inf)
        _, sel = lax.top_k(gate, kk)
        valid = jnp.arange(kk) < cur
        k_sel = kb[b_idx, h_idx, sel]
        v_sel = vb[b_idx, h_idx, sel]
        kpos = sel[..., None] * MOBA_BLOCK + offs
        bias_sel = rpe_hb[h_idx[..., None], rpe_bucket(qpos[:, None, None] - kpos)]
        l_sel = jnp.einsum('bhqd,bhqrtd->bhqrt', qblk, k_sel).astype(F32) * scale + bias_sel
        l_sel = jnp.where(valid[:, None], l_sel, -jnp.inf)
        k_own = lax.dynamic_index_in_dim(kb, cur, axis=2, keepdims=False)
        v_own = lax.dynamic_index_in_dim(vb, cur, axis=2, keepdims=False)
        dist = qpos[:, None] - (cur * MOBA_BLOCK + offs)[None, :]
        l_own = jnp.einsum('bhqd,bhtd->bhqt', qblk, k_own).astype(F32) * scale + rpe_hb[:, rpe_bucket(dist)]
        l_own = jnp.where(dist >= 0, l_own, -jnp.inf)
        logits = jnp.concatenate([l_sel.reshape(B, H, MOBA_QBLOCK, kk * MOBA_BLOCK), l_own], axis=-1)
        p = jax.nn.softmax(logits, axis=-1).astype(v.dtype)
        p_sel = p[..., :kk * MOBA_BLOCK].reshape(B, H, MOBA_QBLOCK, kk, MOBA_BLOCK)
        p_own = p[..., kk * MOBA_BLOCK:]
        return (jnp.einsum('bhqrt,bhqrtd->bhqd', p_sel, v_sel)
                + jnp.einsum('bhqt,bhtd->bhqd', p_own, v_own))

    out = lax.map(one_block, (jnp.arange(nqb), qb))
    return out.transpose(1, 0, 3, 2, 4).reshape(B, S, H, Dh)


def gla_chunked(q, k, v, g):
    B, S, H, DK = q.shape
    DV = v.shape[-1]
    n = S // GLA_CHUNK

    def chunks(t):
        return t.astype(F32).reshape(B, n, GLA_CHUNK, H, t.shape[-1]).transpose(1, 0, 3, 2, 4)

    qc = chunks(q) * DK ** -0.5
    kc, vc, gc = chunks(k), chunks(v), chunks(g)
    b = jnp.cumsum(gc, axis=3)
    b_last = b[:, :, :, -1:, :]
    q_e = qc * jnp.exp(b)
    k_e = kc * jnp.exp(-b)
    k_end = kc * jnp.exp(b_last - b)
    causal = jnp.tril(jnp.ones((GLA_CHUNK, GLA_CHUNK), F32))
    o_intra = jnp.einsum('nbhij,nbhjv->nbhiv', jnp.einsum('nbhid,nbhjd->nbhij', q_e, k_e) * causal, vc)

    def step(state, xs):
        q_i, k_i, v_i, d_i = xs
        o_i = jnp.einsum('bhid,bhdv->bhiv', q_i, state)
        state = state * d_i[..., None] + jnp.einsum('bhjd,bhjv->bhdv', k_i, v_i)
        return state, o_i

    s0 = jnp.zeros((B, H, DK, DV), F32)
    _, o_inter = lax.scan(step, s0, (q_e, k_end, vc, jnp.exp(b_last[:, :, :, 0, :])))
    return (o_intra + o_inter).transpose(1, 0, 3, 2, 4).reshape(B, S, H, DV)


def gated_delta_chunked(q, k, v, g, beta):
    B, S, H, DK = q.shape
    DV = v.shape[-1]
    C = GDN_CHUNK
    n = S // C

    def chunks(t):
        return t.astype(F32).reshape(B, n, C, H, t.shape[-1]).transpose(1, 0, 3, 2, 4)

    qc = chunks(q) * DK ** -0.5
    kc, vc = chunks(k), chunks(v)
    gc = jnp.cumsum(chunks(g[..., None])[..., 0], axis=-1)
    bc = chunks(beta[..., None])
    incl = jnp.tril(jnp.ones((C, C), bool))
    strict = jnp.tril(jnp.ones((C, C), bool), -1)
    diff = gc[..., :, None] - gc[..., None, :]
    decay = jnp.where(incl, jnp.exp(jnp.where(incl, diff, 0.0)), 0.0)
    k_beta = kc * bc
    v_beta = vc * bc
    a = jnp.where(strict, jnp.einsum('nbhid,nbhjd->nbhij', k_beta, kc) * decay, 0.0)
    eye = jnp.eye(C, dtype=F32)
    t_mat = lax.linalg.triangular_solve(a + eye, jnp.broadcast_to(eye, a.shape), left_side=True,
                                        lower=True, unit_diagonal=True)
    w_val = t_mat @ v_beta
    k_cum = t_mat @ (k_beta * jnp.exp(gc)[..., None])
    attn = jnp.einsum('nbhid,nbhjd->nbhij', qc, kc) * decay
    q_g = qc * jnp.exp(gc)[..., None]
    g_last = gc[..., -1:]
    k_end = kc * jnp.exp(g_last - gc)[..., None]
    d_last = jnp.exp(g_last[..., 0])

    def step(state, xs):
        q_i, kc_i, w_i, at_i, ke_i, dl_i = xs
        v_new = w_i - jnp.einsum('bhcd,bhdv->bhcv', kc_i, state)
        o_i = jnp.einsum('bhcd,bhdv->bhcv', q_i, state) + jnp.einsum('bhij,bhjv->bhiv', at_i, v_new)
        state = state * dl_i[..., None, None] + jnp.einsum('bhcd,bhcv->bhdv', ke_i, v_new)
        return state, o_i

    s0 = jnp.zeros((B, H, DK, DV), F32)
    _, o = lax.scan(step, s0, (q_g, k_cum, w_val, attn, k_end, d_last))
    return o.transpose(1, 0, 3, 2, 4).reshape(B, S, H, DV)


def moba_gla_mixer(h, rpe_bias, w_in, gk_w2, gk_b, o_norm, w_out):
    B, S, _ = h.shape
    mq, mk, mv, gq, gk, gv, gg, glr = _split(h @ w_in, EVEN_SPLITS)
    o_a = moba_attention(mq.reshape(B, S, MOBA_HEADS, MOBA_HEAD_DIM),
                         mk.reshape(B, S, MOBA_HEADS, MOBA_HEAD_DIM),
                         mv.reshape(B, S, MOBA_HEADS, MOBA_HEAD_DIM), rpe_bias)
    log_gate = jax.nn.log_sigmoid((glr @ gk_w2 + gk_b).astype(F32)) / GLA_GATE_NORM
    o_b = gla_chunked(gq.reshape(B, S, GLA_HEADS, GLA_DK), gk.reshape(B, S, GLA_HEADS, GLA_DK),
                      gv.reshape(B, S, GLA_HEADS, GLA_DV), log_gate.reshape(B, S, GLA_HEADS, GLA_DK))
    o_b = gated_rmsnorm(o_b, gg.reshape(B, S, GLA_HEADS, GLA_DV), o_norm).astype(h.dtype)
    mixed = jnp.concatenate([o_a.reshape(B, S, MOBA_W), o_b.reshape(B, S, GLA_V_W)], axis=-1)
    return mixed @ w_out


def gated_deltanet_mixer(h, w_in, conv_w, a_log, dt_bias, o_norm, w_out):
    B, S, _ = h.shape
    proj = h @ w_in
    qkv = jax.nn.silu(causal_dwconv(proj[..., :GDN_QKV_W], conv_w))
    q, k, v = _split(qkv, (GDN_QK_W, GDN_QK_W, GDN_V_W))
    gate, b_raw, a_raw = _split(proj[..., GDN_QKV_W:], (GDN_V_W, GDN_HEADS, GDN_HEADS))
    q = l2norm(q.reshape(B, S, GDN_HEADS, GDN_DK))
    k = l2norm(k.reshape(B, S, GDN_HEADS, GDN_DK))
    beta = jax.nn.sigmoid(b_raw.astype(F32))
    g = -jnp.exp(a_log.astype(F32)) * jax.nn.softplus(a_raw.astype(F32) + dt_bias.astype(F32))
    o = gated_delta_chunked(q, k, v.reshape(B, S, GDN_HEADS, GDN_DV), g, beta)
    o = gated_rmsnorm(o, gate.reshape(B, S, GDN_HEADS, GDN_DV), o_norm).astype(h.dtype)
    return o.reshape(B, S, GDN_V_W) @ w_out


def routed_experts(xf, topi, topw, w_gate, w_up, w_down):
    T, D = xf.shape
    M = T * TOP_K
    eid = topi.reshape(M)
    tok = jnp.arange(M, dtype=jnp.int32) // TOP_K
    wts = topw.reshape(M)
    order = jnp.argsort(eid)
    eid_s, tok_s, w_s = eid[order], tok[order], wts[order]
    counts = jnp.bincount(eid, length=N_EXPERTS)
    padded = (counts + MOE_BLOCK - 1) // MOE_BLOCK * MOE_BLOCK
    pad_end = jnp.cumsum(padded)
    pad_start = pad_end - padded
    start = jnp.cumsum(counts) - counts
    dest = pad_start[eid_s] + jnp.arange(M) - start[eid_s]
    nblk = (M + N_EXPERTS * (MOE_BLOCK - 1) + MOE_BLOCK - 1) // MOE_BLOCK
    P = nblk * MOE_BLOCK
    row_tok = jnp.zeros((P,), jnp.int32).at[dest].set(tok_s)
    row_w = jnp.zeros((P,), F32).at[dest].set(w_s)
    blk_exp = jnp.minimum(jnp.searchsorted(pad_end, jnp.arange(nblk) * MOE_BLOCK, side='right'),
                          N_EXPERTS - 1)

    def step(acc, xs):
        e, rt, rw = xs
        xb = xf[rt]
        hb = jax.nn.silu(xb @ w_gate[e]) * (xb @ w_up[e])
        yb = (hb @ w_down[e]).astype(F32) * rw[:, None]
        return acc.at[rt].add(yb), None

    out, _ = lax.scan(step, jnp.zeros((T, D), F32),
                      (blk_exp, row_tok.reshape(nblk, MOE_BLOCK), row_w.reshape(nblk, MOE_BLOCK)))
    return out.astype(xf.dtype)


def moe_ffn(h, router_w, router_b, w_gate, w_up, w_down, sh_gate, sh_up, sh_down):
    B, S, D = h.shape
    xf = h.reshape(B * S, D)
    scores = jax.nn.sigmoid((xf @ router_w).astype(F32))
    sel_scores = scores + router_b.astype(F32)
    grp = sel_scores.reshape(-1, N_GROUPS, N_EXPERTS // N_GROUPS)
    grp_score = jnp.sum(lax.top_k(grp, 2)[0], axis=-1)
    _, top_g = lax.top_k(grp_score, TOPK_GROUPS)
    gmask = jnp.any(top_g[..., None] == jnp.arange(N_GROUPS), axis=1)
    emask = jnp.repeat(gmask, N_EXPERTS // N_GROUPS, axis=1)
    _, topi = lax.top_k(jnp.where(emask, sel_scores, -jnp.inf), TOP_K)
    topw = jnp.take_along_axis(scores, topi, axis=1)
    topw = topw / (jnp.sum(topw, -1, keepdims=True) + 1e-20) * ROUTED_SCALE
    routed = routed_experts(xf, topi, topw, w_gate, w_up, w_down)
    shared = (jax.nn.silu(xf @ sh_gate) * (xf @ sh_up)) @ sh_down
    return (routed + shared).reshape(B, S, D)


def setup_inputs(seed: int = 0) -> dict:
    key = jax.random.key(seed)
    ks = iter(jax.random.split(key, 32))

    def nrm(shape, s):
        return jax.random.normal(next(ks), shape, F32) * s

    D = D_MODEL
    u_a = jax.random.uniform(next(ks), (N_ODD, GDN_HEADS), F32, 1.0, 16.0)
    u_dt = jax.random.uniform(next(ks), (N_ODD, GDN_HEADS), F32)
    dt = jnp.exp(u_dt * (math.log(0.1) - math.log(0.001)) + math.log(0.001))
    return {
        "x": nrm((BATCH, SEQ, D), 1.0),
        "c": nrm((BATCH, D), 1.0),
        "rpe_bias": nrm((RPE_BUCKETS, MOBA_HEADS), 0.5),
        "ada_w": nrm((DEPTH, D, 6 * D), 0.5 * D ** -0.5),
        "ada_b": nrm((DEPTH, 6 * D), 0.02),
        "ln_mix_g": 1.0 + nrm((DEPTH, D), 0.02),
        "ln_mix_b": nrm((DEPTH, D), 0.02),
        "ln_ffn_g": 1.0 + nrm((DEPTH, D), 0.02),
        "ln_ffn_b": nrm((DEPTH, D), 0.02),
        "ev_w_in": nrm((N_EVEN, D, EVEN_IN), D ** -0.5),
        "ev_gk_w2": nrm((N_EVEN, GLA_GATE_RANK, GLA_QK_W), GLA_GATE_RANK ** -0.5),
        "ev_gk_b": nrm((N_EVEN, GLA_QK_W), 0.1),
        "ev_norm": 1.0 + nrm((N_EVEN, GLA_DV), 0.02),
        "ev_w_out": nrm((N_EVEN, EVEN_MIX, D), EVEN_MIX ** -0.5 * DEEPNORM_BETA),
        "od_w_in": nrm((N_ODD, D, ODD_IN), D ** -0.5),
        "od_conv_w": nrm((N_ODD, GDN_CONV, GDN_QKV_W), GDN_CONV ** -0.5),
        "od_a_log": jnp.log(u_a),
        "od_dt_bias": dt + jnp.log(-jnp.expm1(-dt)),
        "od_norm": 1.0 + nrm((N_ODD, GDN_DV), 0.02),
        "od_w_out": nrm((N_ODD, ODD_MIX, D), ODD_MIX ** -0.5 * DEEPNORM_BETA),
        "moe_router_w": nrm((DEPTH, D, N_EXPERTS), D ** -0.5),
        "moe_router_b": nrm((DEPTH, N_EXPERTS), 0.01),
        "moe_w_gate": nrm((DEPTH, N_EXPERTS, D, D_EXPERT), D ** -0.5),
        "moe_w_up": nrm((DEPTH, N_EXPERTS, D, D_EXPERT), D ** -0.5),
        "moe_w_down": nrm((DEPTH, N_EXPERTS, D_EXPERT, D), D_EXPERT ** -0.5 * DEEPNORM_BETA),
        "sh_w_gate": nrm((DEPTH, D, D_SHARED), D ** -0.5),
        "sh_w_up": nrm((DEPTH, D, D_SHARED), D ** -0.5),
        "sh_w_down": nrm((DEPTH, D_SHARED, D), D_SHARED ** -0.5 * DEEPNORM_BETA),
    }


def reference(x, c, rpe_bias, ada_w, ada_b, ln_mix_g, ln_mix_b, ln_ffn_g, ln_ffn_b,
              ev_w_in, ev_gk_w2, ev_gk_b, ev_norm, ev_w_out,
              od_w_in, od_conv_w, od_a_log, od_dt_bias, od_norm, od_w_out,
              moe_router_w, moe_router_b, moe_w_gate, moe_w_up, moe_w_down,
              sh_w_gate, sh_w_up, sh_w_down):
    c_act = jax.nn.silu(c)
    for layer in range(DEPTH):
        mod = c_act @ ada_w[layer] + ada_b[layer]
        sh1, sc1, g1, sh2, sc2, g2 = jnp.split(mod[:, None, :], 6, axis=-1)
        h = x * (1.0 + sc1) + sh1
        i = layer // 2
        if layer % 2 == 0:
            y = moba_gla_mixer(h, rpe_bias, ev_w_in[i], ev_gk_w2[i], ev_gk_b[i], ev_norm[i], ev_w_out[i])
        else:
            y = gated_deltanet_mixer(h, od_w_in[i], od_conv_w[i], od_a_log[i], od_dt_bias[i],
                                     od_norm[i], od_w_out[i])
        x = layer_norm(DEEPNORM_ALPHA * x + g1 * y, ln_mix_g[layer], ln_mix_b[layer])
        h = x * (1.0 + sc2) + sh2
        y = moe_ffn(h, moe_router_w[layer], moe_router_b[layer], moe_w_gate[layer], moe_w_up[layer],
                    moe_w_down[layer], sh_w_gate[layer], sh_w_up[layer], sh_w_down[layer])
        x = layer_norm(DEEPNORM_ALPHA * x + g2 * y, ln_ffn_g[layer], ln_ffn_b[layer])
    return x
```

```python
import math
import numpy as np
import concourse.bass as bass
import concourse.mybir as mybir
from concourse.bass_utils import run_bass_kernel_spmd
from contextlib import ExitStack

F32 = mybir.dt.float32
BF16 = mybir.dt.bfloat16
AF = mybir.ActivationFunctionType
ALU = mybir.AluOpType
AX = mybir.AxisListType

ENGS = ['tensor', 'vector', 'scalar', 'gpsimd', 'sync']
DMAQ = ('sync', 'gpsimd')


class Dep:
    __slots__ = ('w', 'r', 'rd')

    def __init__(self):
        self.w = None
        self.r = {}
        self.rd = []


class Op:
    __slots__ = ('eng', 'fn', 'waits', 'signal', 'value', 'dma', 'slot', 'idx')


class Prog:
    NDMA = 12
    uid = 0

    def __init__(self, nc, es):
        self.nc = nc
        self.es = es
        self.ops = {e: [] for e in ENGS}
        Prog.uid += 1
        u = Prog.uid
        self.sem = {e: nc.alloc_semaphore(name='s%d_%s' % (u, e)) for e in ENGS}
        self.dsem = {q: [nc.alloc_semaphore(name='d%d_%s%d' % (u, q, i)) for i in range(self.NDMA)]
                     for q in DMAQ}
        self.dcount = {q: 0 for q in DMAQ}
        self.dlast = {q: [None] * self.NDMA for q in DMAQ}
        self.n = 0
        self.out_dmas = []
        self.bg_sem = nc.alloc_semaphore(name='bg%d' % u)
        self.bg_count = 0

    def dep(self):
        return Dep()

    def deps(self, n):
        return [Dep() for _ in range(n)]

    def op(self, eng, fn, reads=(), writes=(), dma=False, is_out=False):
        o = Op()
        o.eng = eng
        o.fn = fn
        o.dma = dma
        o.signal = False
        o.value = None
        o.slot = None
        o.idx = self.n
        self.n += 1
        raw = []
        other = []
        for d in reads:
            if d.w is not None:
                raw.append(d.w)
        for d in writes:
            if d.w is not None:
                raw.append(d.w)
            other.extend(d.r.values())
            other.extend(d.rd)
        if dma:
            j = self.dcount[eng]
            self.dcount[eng] += 1
            s = j % self.NDMA
            o.slot = s
            o.value = 16 * (j // self.NDMA + 1)
            prev = self.dlast[eng][s]
            if prev is not None:
                raw.append(prev)
            self.dlast[eng][s] = o
        waits = {}
        for p in raw:
            if p.dma or p.eng != eng or eng != 'tensor':
                waits[p.idx] = p
        for p in other:
            if p.dma or p.eng != eng or eng == 'gpsimd':
                waits[p.idx] = p
        if dma:
            for p in other:
                waits[p.idx] = p
        o.waits = list(waits.values())
        for p in o.waits:
            p.signal = True
        for d in reads:
            if dma:
                d.rd.append(o)
            else:
                d.r[eng] = o
        for d in writes:
            d.w = o
            d.r = {}
            d.rd = []
        self.ops[eng].append(o)
        if is_out:
            self.out_dmas.append(o)
        return o

    def bg_dma(self, eng, fn):
        o = Op()
        o.eng = eng
        o.fn = fn
        o.dma = 'bg'
        o.signal = False
        o.value = None
        o.slot = None
        o.idx = self.n
        self.n += 1
        o.waits = []
        self.bg_count += 1
        self.ops[eng].append(o)
        return o

    def finish(self):
        o = Op()
        o.eng = 'sync'
        o.fn = None
        o.dma = False
        o.signal = False
        o.value = None
        o.slot = None
        o.idx = self.n
        self.n += 1
        o.waits = list(self.out_dmas)
        self.final_op = o
        self.ops['sync'].append(o)
        for e in ENGS:
            c = 0
            for o in self.ops[e]:
                if o.dma:
                    continue
                if o.signal:
                    c += 1
                    o.value = c

    def emit(self):
        self.finish()
        nc = self.nc
        with nc.Block() as block:
            for e in ENGS:
                getattr(block, e)(self._body(e))

    def _body(self, e):
        def body(eng):
            waited = {}
            for o in self.ops[e]:
                need = {}
                for p in o.waits:
                    if p.dma:
                        key = ('d', p.eng, p.slot)
                        sem = self.dsem[p.eng][p.slot]
                    else:
                        key = ('c', p.eng)
                        sem = self.sem[p.eng]
                    v = p.value
                    assert v is not None
                    if key not in need or need[key][1] < v:
                        need[key] = (sem, v)
                for key, (sem, v) in need.items():
                    if waited.get(key, 0) >= v:
                        continue
                    eng.wait_ge(sem, v)
                    waited[key] = v
                if o.fn is None:
                    if o is self.final_op and self.bg_count:
                        eng.wait_ge(self.bg_sem, 16 * self.bg_count)
                    continue
                inst = o.fn(eng)
                if o.dma == 'bg':
                    inst.then_inc(self.bg_sem, 16)
                elif o.dma:
                    inst.then_inc(self.dsem[e][o.slot], 16)
                elif o.signal:
                    inst.then_inc(self.sem[e], 1)
        return body


D = 1024
NE = 64
ALPHA = float(4 ** 0.25)
LN_EPS = 1e-5


class Ctx:
    def __init__(self, nc, es):
        self.nc = nc
        self.es = es
        self.P = Prog(nc, es)

    k = 0

    def sb(self, shape, dt, name=None):
        Ctx.k += 1
        return self.es.enter_context(self.nc.sbuf_tensor(name or "sb%d" % Ctx.k, shape, dt))

    def ps(self, shape, dt, name=None):
        Ctx.k += 1
        return self.es.enter_context(self.nc.psum_tensor(name or "ps%d" % Ctx.k, shape, dt))


def dram_in(nc, name, shape, dt=F32):
    return nc.dram_tensor(name, list(shape), dt, kind="ExternalInput").ap()


def dram_out(nc, name, shape, dt=F32):
    return nc.dram_tensor(name, list(shape), dt, kind="ExternalOutput").ap()


def emit_bg(P, bg, n, NB):
    if not bg:
        return
    for fn in bg[len(bg) * n // NB:len(bg) * (n + 1) // NB]:
        P.bg_dma('gpsimd', fn)


def emit_consts(cx, a):
    P = cx.P
    sb = cx.sb
    cx.d_c = P.dep()
    cx.ident = sb([128, 128], F32)
    P.op('sync', lambda e: e.dma_start(out=cx.ident[:], in_=a['ident']), writes=[cx.d_c], dma=True)
    cx.eps_col = sb([128, 1], F32)
    P.op('vector', lambda e: e.memset(cx.eps_col[:], LN_EPS), writes=[cx.d_c])
    cx.ones = sb([128, 128], F32)
    P.op('vector', lambda e: e.memset(cx.ones[:], 1.0), writes=[cx.d_c])
    ccol = sb([128, 8], F32)
    P.op('sync', lambda e: e.dma_start(out=ccol[:], in_=a['ccol']), writes=[cx.d_c], dma=True)
    cx.cact = sb([128, 8], F32)
    P.op('scalar', lambda e: e.activation(out=cx.cact[:], in_=ccol[:], func=AF.Silu), reads=[cx.d_c], writes=[cx.d_c])
    cx.cact2 = sb([128, 8, 2], F32)
    for r in range(2):
        P.op('vector', lambda e, r=r: e.tensor_copy(out=cx.cact2[:, :, r], in_=cx.cact[:]), reads=[cx.d_c], writes=[cx.d_c])


def emit_modcols(cx, a, secs, adaw, d_adaw, pm, d_pm, d_out):
    P = cx.P
    adab_col = cx.sb([128, 48], F32)
    P.op('sync', lambda e: e.dma_start(out=adab_col[:], in_=a['ada_b_col']), writes=[d_out], dma=True)
    modcol = cx.sb([128, len(secs), 8], F32)
    for i, sec in enumerate(secs):
        P.op('sync', lambda e, sec=sec: e.dma_start(out=adaw[:], in_=a['ada_w'][:, sec * D:(sec + 1) * D].rearrange("(j p) n -> p j n", p=128)),
             writes=[d_adaw], dma=True)
        for jc in range(8):
            for j in range(8):
                P.op('tensor', lambda e, jc=jc, j=j: e.matmul(pm[:, jc * 2:jc * 2 + 2], lhsT=adaw[:, j, jc * 128:(jc + 1) * 128],
                                                             rhs=cx.cact2[:, j, :], start=(j == 0), stop=(j == 7)),
                     reads=[cx.d_c, d_adaw], writes=[d_pm])
        P.op('vector', lambda e, i=i, sec=sec: e.tensor_tensor(out=modcol[:, i, :], in0=pm[:, 0:16].rearrange("p (j r) -> p j r", r=2)[:, :, 0],
                                                              in1=adab_col[:, sec * 8:(sec + 1) * 8], op=ALU.add),
             reads=[d_pm, d_out], writes=[d_out])
    return modcol


def emit_hT_block(cx, a_x, t0, ntile, xt, d_xt, pts, d_pts, modcol, d_mod, hT, d_hT, hT_off=0):
    P = cx.P
    for i in range(ntile):
        b = i % 2
        P.op('sync', lambda e, b=b, i=i: e.dma_start(out=xt[b][:], in_=a_x[t0 + i * 128:t0 + (i + 1) * 128, :]), writes=[d_xt[b]], dma=True)
        for half in range(2):
            pt = pts[half]
            for j4 in range(4):
                j = half * 4 + j4
                P.op('tensor', lambda e, b=b, pt=pt, j=j, j4=j4: e.transpose(pt[:, j4 * 128:(j4 + 1) * 128], xt[b][:, j * 128:(j + 1) * 128], cx.ident[:]),
                     reads=[d_xt[b], cx.d_c], writes=[d_pts[half]])
            for j4 in range(4):
                j = half * 4 + j4
                P.op('vector', lambda e, pt=pt, j=j, j4=j4, i=i: e.tensor_scalar(out=hT[:, j, hT_off + i * 128:hT_off + (i + 1) * 128],
                                                                                in0=pt[:, j4 * 128:(j4 + 1) * 128],
                                                                                scalar1=modcol[:, 1, j:j + 1], scalar2=modcol[:, 0, j:j + 1],
                                                                                op0=ALU.mult, op1=ALU.add),
                     reads=[d_pts[half], d_mod], writes=[d_hT])


def layer_norm_tile(cx, v, d_v, out, d_out, grow, brow, d_rows, st, d_st):
    P = cx.P
    stats, mv, rstd = st
    for h in range(2):
        P.op('vector', lambda e, h=h: e.bn_stats(out=stats[:, h * 6:(h + 1) * 6], in_=v[:, h * 512:(h + 1) * 512]),
             reads=[d_v], writes=[d_st])
    P.op('vector', lambda e: e.bn_aggr(out=mv[:], in_=stats[:]), reads=[d_st], writes=[d_st])
    P.op('scalar', lambda e: e.activation(out=rstd[:], in_=mv[:, 1:2], func=AF.Sqrt, bias=cx.eps_col[:], scale=1.0),
         reads=[d_st], writes=[d_st])
    P.op('vector', lambda e: e.reciprocal(out=rstd[:], in_=rstd[:]), reads=[d_st], writes=[d_st])
    P.op('vector', lambda e: e.tensor_scalar(out=out[:], in0=v[:], scalar1=mv[:, 0:1], scalar2=rstd[:, 0:1],
                                             op0=ALU.subtract, op1=ALU.mult), reads=[d_v, d_st], writes=[d_out])
    P.op('gpsimd', lambda e: e.tensor_tensor(out=out[:], in0=out[:], in1=grow, op=ALU.mult),
         reads=[d_out, d_rows], writes=[d_out])
    P.op('gpsimd', lambda e: e.tensor_tensor(out=out[:], in0=out[:], in1=brow, op=ALU.add),
         reads=[d_out, d_rows], writes=[d_out])


def emit_stage_c(nc, es, T, a, PASS=2048):
    cx = Ctx(nc, es)
    P = cx.P
    sb, ps = cx.sb, cx.ps
    PASS = min(PASS, T)
    NT = PASS // 128
    NTT = PASS // 512
    npass = T // PASS

    ident = sb([128, 128], F32); d_c = P.dep()
    P.op('sync', lambda e: e.dma_start(out=ident[:], in_=a['ident']), writes=[d_c], dma=True)
    cx.eps_col = sb([128, 1], F32)
    P.op('vector', lambda e: e.memset(cx.eps_col[:], LN_EPS), writes=[d_c])
    ones = sb([128, 128], F32)
    P.op('vector', lambda e: e.memset(ones[:], 1.0), writes=[d_c])
    ccol = sb([128, 8], F32)
    P.op('sync', lambda e: e.dma_start(out=ccol[:], in_=a['ccol']), writes=[d_c], dma=True)
    cact = sb([128, 8], F32)
    P.op('scalar', lambda e: e.activation(out=cact[:], in_=ccol[:], func=AF.Silu), reads=[d_c], writes=[d_c])
    h2f = [sb([128, 8, 128], F32)]; d_h2f = P.deps(1)
    crep = h2f[0]
    for j in range(8):
        P.op('vector', lambda e, j=j: e.tensor_scalar(out=crep[:, j, :], in0=ones[:], scalar1=cact[:, j:j + 1], scalar2=None,
                                                      op0=ALU.mult), reads=[d_c], writes=[d_h2f[0]])
    cact2 = sb([128, 8, 2], F32)
    for r in range(2):
        P.op('vector', lambda e, r=r: e.tensor_copy(out=cact2[:, :, r], in_=cact[:]), reads=[d_c], writes=[d_c])
    lnrows = sb([128, 4, D], F32); d_rows = P.dep()
    for i in range(4):
        P.op('sync', lambda e, i=i: e.dma_start(out=lnrows[:, i, :], in_=a['ln'][i:i + 1, :].partition_broadcast(128)),
             writes=[d_rows], dma=True)
    grow = sb([128, 2, D], F32)
    for i, sec in enumerate((2, 5)):
        P.op('sync', lambda e, i=i, sec=sec: e.dma_start(out=grow[:, i, :],
                                                         in_=a['ada_b_row'][0:1, sec * D:(sec + 1) * D].partition_broadcast(128)),
             writes=[d_rows], dma=True)
    adab_col = sb([128, 48], F32)
    P.op('sync', lambda e: e.dma_start(out=adab_col[:], in_=a['ada_b_col']), writes=[d_rows], dma=True)
    modcol = sb([128, 2, 8], F32)
    acc = sb([128, max(NT, 8), D], F32); d_acc = P.deps(max(NT, 8))
    d_adaw8 = d_acc[0:8]
    adaw = acc[:, 0:8, :]
    pmod = [ps([128, 512], F32), ps([128, 512], F32)]; d_pmod = [P.dep(), P.dep()]
    for i, sec in enumerate((2, 5)):
        P.op('sync', lambda e, sec=sec: e.dma_start(out=adaw[:], in_=a['ada_w'][:, sec * D:(sec + 1) * D].rearrange("(j p) n -> p j n", p=128)),
             writes=d_adaw8, dma=True)
        for h in range(2):
            for j in range(8):
                P.op('tensor', lambda e, h=h, j=j: e.matmul(pmod[h][:], lhsT=crep[:, j, :], rhs=adaw[:, j, h * 512:(h + 1) * 512],
                                                           start=(j == 0), stop=(j == 7)),
                     reads=[d_c, d_h2f[0]] + d_adaw8, writes=[d_pmod[h]])
            P.op('vector', lambda e, h=h, i=i: e.tensor_tensor(out=grow[:, i, h * 512:(h + 1) * 512], in0=pmod[h][:],
                                                              in1=grow[:, i, h * 512:(h + 1) * 512], op=ALU.add),
                 reads=[d_pmod[h], d_rows], writes=[d_rows])
    for i, sec in enumerate((3, 4)):
        P.op('sync', lambda e, sec=sec: e.dma_start(out=adaw[:], in_=a['ada_w'][:, sec * D:(sec + 1) * D].rearrange("(j p) n -> p j n", p=128)),
             writes=d_adaw8, dma=True)
        for jc in range(8):
            for j in range(8):
                P.op('tensor', lambda e, jc=jc, j=j: e.matmul(pmod[0][:, jc * 2:jc * 2 + 2], lhsT=adaw[:, j, jc * 128:(jc + 1) * 128],
                                                             rhs=cact2[:, j, :], start=(j == 0), stop=(j == 7)),
                     reads=[d_c] + d_adaw8, writes=[d_pmod[0]])
        P.op('vector', lambda e, i=i, sec=sec: e.tensor_tensor(out=modcol[:, i, :], in0=pmod[0][:, 0:16].rearrange("p (j r) -> p j r", r=2)[:, :, 0],
                                                              in1=adab_col[:, sec * 8:(sec + 1) * 8], op=ALU.add),
             reads=[d_pmod[0], d_rows], writes=[d_rows])
    P.op('vector', lambda e: e.tensor_scalar(out=modcol[:, 1, :], in0=modcol[:, 1, :], scalar1=1.0, scalar2=None, op0=ALU.add),
         reads=[d_rows], writes=[d_rows])

    wout = sb([128, 8, D], BF16); d_wout = P.dep()
    P.op('gpsimd', lambda e: e.dma_start(out=wout[:], in_=a['w_out'].rearrange("(j p) n -> p j n", p=128)), writes=[d_wout], dma=True)
    rw = sb([128, 8, NE], F32)
    P.op('sync', lambda e: e.dma_start(out=rw[:], in_=a['router_w'].rearrange("(j p) n -> p j n", p=128)), writes=[d_wout], dma=True)
    rb = sb([128, NE], F32)
    P.op('sync', lambda e: e.dma_start(out=rb[:], in_=a['router_b'][0:1, :].partition_broadcast(128)), writes=[d_wout], dma=True)

    h2T = sb([128, 8, PASS], BF16); d_h2T = P.deps(NTT)
    gw = sb([128, NT, NE + 1], F32); d_gw = P.deps(NT)
    wall = [sb([128, 6144], BF16) for _ in range(2)]
    wgt = [w_[:, 0:2048].rearrange("p (j n) -> p j n", n=256) for w_ in wall]
    wut = [w_[:, 2048:4096].rearrange("p (j n) -> p j n", n=256) for w_ in wall]
    wdt = [w_[:, 4096:6144].rearrange("p (j n) -> p j n", n=D) for w_ in wall]
    d_w = P.deps(2)
    mixt = [sb([128, 8, 128], BF16) for _ in range(2)]; d_mixt = P.deps(2)
    xt = [sb([128, D], F32) for _ in range(2)]; d_xt = P.deps(2)
    vt = [sb([128, D], F32)] * 2; d_vt = [P.dep()] * 2
    x1t = [sb([128, D], F32)] * 2; d_x1t = [P.dep()] * 2
    st = [(sb([128, 12], F32), sb([128, 2], F32), sb([128, 1], F32)) for _ in range(2)]; d_st = P.deps(2)
    rt = [dict(sc=sb([128, NE], F32), sel=sb([128, NE], F32), m8=sb([128, 8, 8], F32), gs=sb([128, 8], F32),
               g8=sb([128, 8], F32), gm=sb([128, 8], F32), gneg=sb([128, 8], F32), selm=sb([128, NE], F32),
               e8=sb([128, 8], F32), em=sb([128, NE], F32), den=sb([128, 1], F32))] * 2
    d_rt = [P.dep()] * 2
    sg = [sb([128, 512], BF16) for _ in range(2)]; d_sg = P.deps(2)
    ytmp = [sb([128, D], F32) for _ in range(2)]; d_ytmp = P.deps(2)
    hw = [[sb([128, 512], BF16) for _ in range(2)] for _ in range(2)]; d_hw = [P.deps(2) for _ in range(2)]
    pg = [ps([128, 512], F32) for _ in range(2)]; d_pg = P.deps(2)
    pu = [pmod[0], pmod[1]]; d_pu = d_pmod
    py = [ps([128, D], F32) for _ in range(2)]; d_py = P.deps(2)

    Ctx.k += 1
    x1s = nc.dram_tensor("x1_scratch%d" % Ctx.k, [T, D], F32, kind="Internal").ap()
    d_x1s = P.deps(T // 128)


    def load_expert(e_i, buf):
        P.op('sync', lambda e: e.dma_start(out=wall[buf][:], in_=a['wbf'][e_i]), writes=[d_w[buf]], dma=True)

    for pa in range(npass):
        tok_base = pa * PASS
        for t in range(NT):
            b = t % 2
            t0 = tok_base + t * 128
            gt = t0 // 128
            P.op('gpsimd', lambda e, b=b, t0=t0: e.dma_start(out=mixt[b][:], in_=a['mixT'][:, t0:t0 + 128].rearrange("(j p) t -> p j t", p=128)),
                 writes=[d_mixt[b]], dma=True)
            P.op('sync', lambda e, b=b, t0=t0: e.dma_start(out=xt[b][:], in_=a['x'][t0:t0 + 128, :]), writes=[d_xt[b]], dma=True)
            for h in range(2):
                for j in range(8):
                    P.op('tensor', lambda e, b=b, h=h, j=j: e.matmul(py[b][:, h * 512:(h + 1) * 512], lhsT=mixt[b][:, j, :],
                                                                    rhs=wout[:, j, h * 512:(h + 1) * 512], start=(j == 0), stop=(j == 7)),
                         reads=[d_mixt[b], d_wout], writes=[d_py[b]])
            P.op('vector', lambda e, b=b: e.tensor_tensor(out=vt[b][:], in0=py[b][:], in1=grow[:, 0, :], op=ALU.mult),
                 reads=[d_py[b], d_rows], writes=[d_vt[b]])
            P.op('vector', lambda e, b=b: e.scalar_tensor_tensor(out=vt[b][:], in0=xt[b][:], scalar=ALPHA, in1=vt[b][:],
                                                                 op0=ALU.mult, op1=ALU.add),
                 reads=[d_xt[b], d_vt[b]], writes=[d_vt[b]])
            layer_norm_tile(cx, vt[b], d_vt[b], x1t[b], d_x1t[b], lnrows[:, 0, :], lnrows[:, 1, :], d_rows, st[b], d_st[b])
            P.op('sync', lambda e, b=b, t0=t0: e.dma_start(out=x1s[t0:t0 + 128, :], in_=x1t[b][:]), reads=[d_x1t[b]],
                 writes=[d_x1s[gt]], dma=True)
            for half in range(2):
                pt = pg[half]
                for j4 in range(4):
                    j = half * 4 + j4
                    P.op('tensor', lambda e, b=b, pt=pt, j=j, j4=j4: e.transpose(pt[:, j4 * 128:(j4 + 1) * 128], x1t[b][:, j * 128:(j + 1) * 128], ident[:]),
                         reads=[d_x1t[b], d_c], writes=[d_pg[half]])
                for j4 in range(4):
                    j = half * 4 + j4
                    P.op('vector', lambda e, b=b, pt=pt, j=j, j4=j4: e.tensor_scalar(out=h2f[0][:, j, :], in0=pt[:, j4 * 128:(j4 + 1) * 128],
                                                                                    scalar1=modcol[:, 1, j:j + 1], scalar2=modcol[:, 0, j:j + 1],
                                                                                    op0=ALU.mult, op1=ALU.add),
                         reads=[d_pg[half], d_rows], writes=[d_h2f[0]])
            tt = t // 4
            P.op('gpsimd', lambda e, b=b, t=t: e.tensor_copy(out=h2T[:, :, t * 128:(t + 1) * 128], in_=h2f[0][:]),
                 reads=[d_h2f[0]], writes=[d_h2T[tt]])
            pr = pu[b]
            for j in range(8):
                P.op('tensor', lambda e, b=b, j=j, pr=pr: e.matmul(pr[:, 0:NE], lhsT=h2f[0][:, j, :], rhs=rw[:, j, :], start=(j == 0), stop=(j == 7)),
                     reads=[d_h2f[0], d_wout], writes=[d_pu[b]])
            r = rt[b]
            dr = d_rt[b]
            P.op('scalar', lambda e, r=r, pr=pr: e.activation(out=r['sc'][:], in_=pr[:, 0:NE], func=AF.Sigmoid), reads=[d_pu[b]], writes=[dr])
            P.op('vector', lambda e, r=r: e.tensor_tensor(out=r['sel'][:], in0=r['sc'][:], in1=rb[:], op=ALU.add), reads=[dr, d_wout], writes=[dr])
            for g in range(8):
                P.op('vector', lambda e, r=r, g=g: e.max(out=r['m8'][:, g, :], in_=r['sel'][:, g * 8:(g + 1) * 8]), reads=[dr], writes=[dr])
            P.op('vector', lambda e, r=r: e.tensor_tensor(out=r['gs'][:], in0=r['m8'][:, :, 0], in1=r['m8'][:, :, 1], op=ALU.add), reads=[dr], writes=[dr])
            P.op('vector', lambda e, r=r: e.max(out=r['g8'][:], in_=r['gs'][:]), reads=[dr], writes=[dr])
            P.op('vector', lambda e, r=r: e.tensor_scalar(out=r['gm'][:], in0=r['gs'][:], scalar1=r['g8'][:, 3:4], scalar2=None, op0=ALU.is_ge),
                 reads=[dr], writes=[dr])
            P.op('vector', lambda e, r=r: e.tensor_scalar(out=r['gneg'][:], in0=r['gm'][:], scalar1=-1.0, scalar2=1e9, op0=ALU.add, op1=ALU.mult),
                 reads=[dr], writes=[dr])
            P.op('vector', lambda e, r=r: e.tensor_tensor(out=r['selm'][:].rearrange("p (g k) -> p g k", k=8),
                                                          in0=r['sel'][:].rearrange("p (g k) -> p g k", k=8),
                                                          in1=r['gm'][:].unsqueeze(2).broadcast_to([128, 8, 8]), op=ALU.mult), reads=[dr], writes=[dr])
            P.op('vector', lambda e, r=r: e.tensor_tensor(out=r['selm'][:].rearrange("p (g k) -> p g k", k=8),
                                                          in0=r['selm'][:].rearrange("p (g k) -> p g k", k=8),
                                                          in1=r['gneg'][:].unsqueeze(2).broadcast_to([128, 8, 8]), op=ALU.add), reads=[dr], writes=[dr])
            P.op('vector', lambda e, r=r: e.max(out=r['e8'][:], in_=r['selm'][:]), reads=[dr], writes=[dr])
            P.op('vector', lambda e, r=r: e.tensor_scalar(out=r['em'][:], in0=r['selm'][:], scalar1=r['e8'][:, 5:6], scalar2=None, op0=ALU.is_ge),
                 reads=[dr], writes=[dr])
            P.op('vector', lambda e, r=r: e.tensor_tensor(out=r['em'][:], in0=r['em'][:], in1=r['sc'][:], op=ALU.mult), reads=[dr], writes=[dr])
            P.op('vector', lambda e, r=r: e.tensor_reduce(out=r['den'][:], in_=r['em'][:], axis=AX.X, op=ALU.add), reads=[dr], writes=[dr])
            P.op('vector', lambda e, r=r: e.tensor_scalar(out=r['den'][:], in0=r['den'][:], scalar1=1e-20, scalar2=None, op0=ALU.add), reads=[dr], writes=[dr])
            P.op('vector', lambda e, r=r: e.reciprocal(out=r['den'][:], in_=r['den'][:]), reads=[dr], writes=[dr])
            P.op('vector', lambda e, r=r, t=t: e.tensor_scalar(out=gw[:, t, 0:NE], in0=r['em'][:], scalar1=r['den'][:, 0:1], scalar2=2.5,
                                                               op0=ALU.mult, op1=ALU.mult), reads=[dr], writes=[d_gw[t]])
            P.op('vector', lambda e, t=t: e.memset(gw[:, t, NE:NE + 1], 1.0), writes=[d_gw[t]])

        def emit_gu_group(k, ei, tt, c, which):
            buf = ei % 2
            hb = k % 2
            pp, dpp, wt_ = ((pg[c], d_pg[c], wgt[buf]), (pu[c], d_pu[c], wut[buf]))[which]
            for j in range(8):
                P.op('tensor', lambda e, pp=pp, wt_=wt_, c=c, j=j, tt=tt: e.matmul(pp[:], lhsT=wt_[:, j, c * 128:(c + 1) * 128],
                                                                                   rhs=h2T[:, j, tt * 512:(tt + 1) * 512],
                                                                                   start=(j == 0), stop=(j == 7)),
                     reads=[d_w[buf], d_h2T[tt]], writes=[dpp])
            if which == 0:
                P.op('scalar', lambda e, c=c: e.activation(out=sg[c][:], in_=pg[c][:], func=AF.Silu), reads=[d_pg[c]], writes=[d_sg[c]])
            else:
                P.op('vector', lambda e, c=c, hb=hb: e.tensor_tensor(out=hw[hb][c][:], in0=pu[c][:], in1=sg[c][:], op=ALU.mult),
                     reads=[d_pu[c], d_sg[c]], writes=[d_hw[hb][c]])

        def emit_down_tile(k, ei, tt, t4):
            buf = ei % 2
            hb = k % 2
            t = tt * 4 + t4
            pb = t % 2
            for h in range(2):
                for c in range(2):
                    P.op('tensor', lambda e, pb=pb, h=h, c=c, hb=hb, t4=t4, buf=buf: e.matmul(py[pb][:, h * 512:(h + 1) * 512],
                                                                                     lhsT=hw[hb][c][:, t4 * 128:(t4 + 1) * 128],
                                                                                     rhs=wdt[buf][:, c, h * 512:(h + 1) * 512],
                                                                                     start=(c == 0), stop=(c == 1)),
                         reads=[d_hw[hb][c], d_w[buf]], writes=[d_py[pb]])
            if t4 % 2 == 1:
                if ei == 0:
                    P.op('scalar', lambda e, pb=pb, t=t, ei=ei: e.activation(out=acc[:, t, :], in_=py[pb][:], func=AF.Identity, scale=gw[:, t, ei:ei + 1]),
                         reads=[d_gw[t]], writes=[d_py[pb], d_acc[t]])
                else:
                    tb = (t4 // 2) % 2
                    P.op('scalar', lambda e, pb=pb, t=t, ei=ei, tb=tb: e.activation(out=ytmp[tb][:], in_=py[pb][:], func=AF.Identity, scale=gw[:, t, ei:ei + 1]),
                         reads=[d_gw[t]], writes=[d_py[pb], d_ytmp[tb]])
                    P.op('gpsimd', lambda e, t=t, tb=tb: e.tensor_tensor(out=acc[:, t, :], in0=acc[:, t, :], in1=ytmp[tb][:], op=ALU.add),
                         reads=[d_ytmp[tb]], writes=[d_acc[t]])
            elif ei == 0:
                P.op('vector', lambda e, pb=pb, t=t, ei=ei: e.tensor_scalar(out=acc[:, t, :], in0=py[pb][:], scalar1=gw[:, t, ei:ei + 1],
                                                                             scalar2=None, op0=ALU.mult),
                     reads=[d_gw[t]], writes=[d_py[pb], d_acc[t]])
            else:
                P.op('vector', lambda e, pb=pb, t=t, ei=ei: e.scalar_tensor_tensor(out=acc[:, t, :], in0=py[pb][:], scalar=gw[:, t, ei:ei + 1],
                                                                                    in1=acc[:, t, :], op0=ALU.mult, op1=ALU.add),
                     reads=[d_gw[t]], writes=[d_py[pb], d_acc[t]])

        items = [(ei, tt) for ei in range(NE + 1) for tt in range(NTT)]
        groups = [(0, 0), (0, 1), (1, 0), (1, 1)]
        load_expert(0, 0)
        load_expert(1, 1)
        for (c, which) in groups:
            emit_gu_group(0, items[0][0], items[0][1], c, which)
        for k, (ei, tt) in enumerate(items):
            nxt = items[k + 1] if k + 1 < len(items) else None
            for t4 in range(4):
                if nxt is not None:
                    emit_gu_group(k + 1, nxt[0], nxt[1], groups[t4][0], groups[t4][1])
                emit_down_tile(k, ei, tt, t4)
            if tt == NTT - 1 and ei + 2 <= NE:
                load_expert(ei + 2, ei % 2)

        for t in range(NT):
            b = t % 2
            t0 = tok_base + t * 128
            gt = t0 // 128
            P.op('sync', lambda e, b=b, t0=t0: e.dma_start(out=xt[b][:], in_=x1s[t0:t0 + 128, :]), reads=[d_x1s[gt]], writes=[d_xt[b]], dma=True)
            P.op('gpsimd', lambda e, b=b, t=t: e.tensor_tensor(out=vt[b][:], in0=acc[:, t, :], in1=grow[:, 1, :], op=ALU.mult),
                 reads=[d_acc[t], d_rows], writes=[d_vt[b]])
            P.op('vector', lambda e, b=b: e.scalar_tensor_tensor(out=vt[b][:], in0=xt[b][:], scalar=ALPHA, in1=vt[b][:],
                                                                 op0=ALU.mult, op1=ALU.add),
                 reads=[d_xt[b], d_vt[b]], writes=[d_vt[b]])
            layer_norm_tile(cx, vt[b], d_vt[b], x1t[b], d_x1t[b], lnrows[:, 2, :], lnrows[:, 3, :], d_rows, st[b], d_st[b])
            P.op('sync', lambda e, b=b, t0=t0: e.dma_start(out=a['out'][t0:t0 + 128, :], in_=x1t[b][:]), reads=[d_x1t[b]], dma=True, is_out=True)
    P.emit()


MB = 256
RW = 1920
NEAR = 1664
NEG = -30000.0


def rpe_bucket_np(dist):
    exact = 16
    d = np.maximum(dist, 0)
    logd = np.log(np.maximum(d, 1).astype(np.float32) / np.float32(exact))
    large = exact + (logd / np.float32(math.log(2048 / exact)) * np.float32(32 - exact)).astype(np.int32)
    large = np.minimum(large, 31)
    return np.where(d < exact, d, large)


def moba_host_tables(rpe_bias, heads):
    p = np.arange(128)[:, None]
    m = np.arange(RW)[None, :]
    dist = m - p - 128
    bk = rpe_bucket_np(dist)
    out = np.empty((len(heads), 128, RW), np.float32)
    for i, h in enumerate(heads):
        out[i] = np.where(dist >= 0, rpe_bias[bk, h], np.float32(NEG))
    return out


def emit_stage_moba(nc, es, S, a, bg=None):
    cx = Ctx(nc, es)
    P = cx.P
    sb, ps = cx.sb, cx.ps
    NB = S // MB
    emit_consts(cx, a)
    ones_bf = sb([128, 128], BF16)
    P.op('vector', lambda e: e.memset(ones_bf[:], 1.0), writes=[cx.d_c])
    eoh = sb([128, 32 * 128], BF16)
    eohf = sb([32, 32 * 128], F32)
    P.op('sync', lambda e: e.dma_start(out=eohf[:], in_=a['eoh']), writes=[cx.d_c], dma=True)
    P.op('vector', lambda e: e.memset(eoh[:], 0.0), writes=[cx.d_c])
    P.op('vector', lambda e: e.tensor_copy(out=eoh[0:32, :], in_=eohf[:]), reads=[cx.d_c], writes=[cx.d_c])
    R = sb([128, 2, RW], F32)
    for h in range(2):
        P.op('sync', lambda e, h=h: e.dma_start(out=R[:, h, :], in_=a['rtab'][h]), writes=[cx.d_c], dma=True)
    Rb = sb([128, 2, RW], BF16)
    P.op('vector', lambda e: e.tensor_copy(out=Rb[:], in_=R[:]), reads=[cx.d_c], writes=[cx.d_c])
    identb = sb([128, 128], BF16)
    P.op('vector', lambda e: e.tensor_copy(out=identb[:], in_=cx.ident[:]), reads=[cx.d_c], writes=[cx.d_c])

    NL = 4
    pl = [ps([128, 512], F32) for _ in range(NL)]; d_pl = P.deps(NL)
    po = ps([128, 512], F32); d_po = P.dep()
    pd = ps([128, 512], F32); d_pd = P.dep()
    pa = [ps([128, 512], F32) for _ in range(2)]; d_pa = P.deps(2)
    pgt = pa[0]; d_pgt = d_pa[0]

    adaw = sb([128, 8, D], F32); d_adaw = P.dep()
    d_mod = P.dep()
    modcol = emit_modcols(cx, a, (0, 1), adaw, d_adaw, pgt, d_pgt, d_mod)
    P.op('vector', lambda e: e.tensor_scalar(out=modcol[:, 1, :], in0=modcol[:, 1, :], scalar1=1.0, scalar2=None, op0=ALU.add),
         reads=[d_mod], writes=[d_mod])

    wq = sb([128, 8, 256], BF16); wk = sb([128, 8, 256], BF16); wv = sb([128, 8, 256], BF16); d_w = P.dep()
    for wt_, nm in ((wq, 'w_q'), (wk, 'w_k'), (wv, 'w_v')):
        P.op('gpsimd', lambda e, wt_=wt_, nm=nm: e.dma_start(out=wt_[:], in_=a[nm].rearrange("(j p) n -> p j n", p=128)), writes=[d_w], dma=True)

    kT = sb([128, 2, S], BF16); d_kT = P.deps(NB)
    V = sb([128, S // 128, 256], BF16); d_V = P.deps(NB)
    kmean = sb([128, 2, 32], F32); d_km = P.dep()
    P.op('vector', lambda e: e.memset(kmean[:], 0.0), writes=[d_km])
    G = [sb([128, 32], F32) for _ in range(2)]; d_G = P.deps(2)
    for g_ in range(2):
        P.op('vector', lambda e, g_=g_: e.memset(G[g_][:], -1e30), writes=[d_G[g_]])
    M8 = [sb([128, 8], F32) for _ in range(2)]
    mb = [sb([128, 32], F32) for _ in range(2)]
    mbT = [sb([128, 256], BF16) for _ in range(2)]; d_mbT = P.deps(2)
    for h_ in range(2):
        P.op('vector', lambda e, h_=h_: e.memset(mbT[h_][:], 0.0), writes=[d_mbT[h_]])

    xt = [sb([128, D], F32) for _ in range(2)]; d_xt = P.deps(2)
    hT = [sb([128, 8, MB], BF16) for _ in range(2)]; d_hT = P.deps(2)
    qTb = [[sb([128, MB], BF16) for _ in range(2)] for _ in range(2)]; d_qTb = [P.deps(2) for _ in range(2)]
    qTf = [[sb([128, MB], F32) for _ in range(2)] for _ in range(2)]; d_qTf = [P.deps(2) for _ in range(2)]
    Pm = [sb([128, MB], BF16) for _ in range(NL)]; d_Pm = P.deps(NL)
    rden = [sb([128, MB], F32) for _ in range(2)]; d_rden = P.deps(2)
    ot = [sb([128, MB], F32) for _ in range(2)]; d_ot = P.deps(2)

    scale = 128 ** -0.5
    cnt = 0
    for n in range(NB):
        emit_bg(P, bg, n, NB)
        q0 = n * MB
        hb = n % 2
        emit_hT_block(cx, a['x'], q0, 2, xt, d_xt, pa, d_pa, modcol, d_mod, hT[hb], d_hT[hb])
        for h in range(2):
            pq = pa[0]
            for j in range(8):
                P.op('tensor', lambda e, h=h, j=j, hb=hb, pq=pq: e.matmul(pq[:, 0:MB], lhsT=wq[:, j, h * 128:(h + 1) * 128], rhs=hT[hb][:, j, :],
                                                                       start=(j == 0), stop=(j == 7)), reads=[d_w, d_hT[hb]], writes=[d_pa[0]])
            P.op('scalar', lambda e, h=h, hb=hb, pq=pq: e.activation(out=qTb[hb][h][:], in_=pq[:, 0:MB], func=AF.Identity, scale=scale),
                 writes=[d_pa[0], d_qTb[hb][h]])
            P.op('vector', lambda e, h=h, hb=hb, pq=pq: e.tensor_scalar(out=qTf[hb][h][:], in0=pq[:, 0:MB], scalar1=scale, scalar2=None, op0=ALU.mult),
                 writes=[d_pa[0], d_qTf[hb][h]])
            pk = pa[1]
            for j in range(8):
                P.op('tensor', lambda e, h=h, j=j, hb=hb, pk=pk: e.matmul(pk[:, 0:MB], lhsT=wk[:, j, h * 128:(h + 1) * 128], rhs=hT[hb][:, j, :],
                                                                       start=(j == 0), stop=(j == 7)), reads=[d_w, d_hT[hb]], writes=[d_pa[1]])
            P.op('scalar', lambda e, h=h, pk=pk, q0=q0: e.activation(out=kT[:, h, q0:q0 + MB], in_=pk[:, 0:MB], func=AF.Identity),
                 writes=[d_pa[1], d_kT[n]])
            P.op('vector', lambda e, h=h, pk=pk, n=n: e.tensor_reduce(out=kmean[:, h, n:n + 1], in_=pk[:, 0:MB], axis=AX.X, op=ALU.add),
                 writes=[d_pa[1], d_km])
        P.op('vector', lambda e, n=n: e.tensor_scalar(out=kmean[:, :, n:n + 1], in0=kmean[:, :, n:n + 1], scalar1=1.0 / MB, scalar2=None, op0=ALU.mult),
             reads=[d_km], writes=[d_km])
        for i in range(2):
            pv = pa[i]
            for j in range(8):
                P.op('tensor', lambda e, i=i, j=j, hb=hb, pv=pv: e.matmul(pv[:, 0:256], lhsT=hT[hb][:, j, i * 128:(i + 1) * 128], rhs=wv[:, j, :],
                                                                       start=(j == 0), stop=(j == 7)), reads=[d_w, d_hT[hb]], writes=[d_pa[i]])
            P.op('scalar', lambda e, i=i, pv=pv, n=n: e.activation(out=V[:, 2 * n + i, :], in_=pv[:, 0:256], func=AF.Identity),
                 reads=[d_pa[i]], writes=[d_V[n]])
        if n >= 1:
            for h in range(2):
                for i in range(2):
                    gb = i
                    P.op('tensor', lambda e, h=h, i=i, hb=hb: e.matmul(pgt[:, i * 32:i * 32 + 32], lhsT=qTf[hb][h][:, i * 128:(i + 1) * 128], rhs=kmean[:, h, :],
                                                                    start=True, stop=True), reads=[d_qTf[hb][h], d_km], writes=[d_pgt])
                    P.op('vector', lambda e, i=i, gb=gb, n=n: e.tensor_copy(out=G[gb][:, 0:n], in_=pgt[:, i * 32:i * 32 + n]), reads=[d_pgt], writes=[d_G[gb]])
                    P.op('vector', lambda e, gb=gb: e.max(out=M8[gb][:], in_=G[gb][:]), reads=[d_G[gb]], writes=[d_G[gb]])
                    P.op('vector', lambda e, gb=gb: e.tensor_scalar(out=mb[gb][:], in0=G[gb][:], scalar1=M8[gb][:, 2:3], scalar2=NEG,
                                                                    op0=ALU.is_lt, op1=ALU.mult), reads=[d_G[gb]], writes=[d_G[gb]])
                    P.op('tensor', lambda e, i=i, gb=gb: e.transpose(pgt[0:32, 256 + i * 128:256 + (i + 1) * 128], mb[gb][:], cx.ident[:]),
                         reads=[d_G[gb], cx.d_c], writes=[d_pgt])
                P.op('vector', lambda e, h=h: e.tensor_copy(out=mbT[h][0:32, :], in_=pgt[0:32, 256:512]), reads=[d_pgt], writes=[d_mbT[h]])
        tiles = [(h, t) for h in range(2) for t in range(2 * n + 2)]
        nt = 2 * n + 2

        def emit_qk(idx):
            h, t = tiles[idx]
            lb = (cnt + idx) % NL
            past = t < 2 * n
            delta = q0 - t * 128
            near = delta < NEAR
            P.op('tensor', lambda e, h=h, t=t, lb=lb, hb=hb, past=past, near=near: e.matmul(pl[lb][:, 0:MB], lhsT=kT[:, h, t * 128:(t + 1) * 128], rhs=qTb[hb][h][:],
                                                                                         start=True, stop=(not past and not near)),
                 reads=[d_kT[t // 2], d_qTb[hb][h]], writes=[d_pl[lb]])
            if past:
                P.op('tensor', lambda e, h=h, t=t, lb=lb, near=near: e.matmul(pl[lb][:, 0:MB], lhsT=eoh[:, (t // 2) * 128:(t // 2 + 1) * 128], rhs=mbT[h][:],
                                                                           start=False, stop=(not near)),
                     reads=[cx.d_c, d_mbT[h]], writes=[d_pl[lb]])
            if near:
                s0 = delta + 128
                P.op('tensor', lambda e, h=h, lb=lb, s0=s0: e.matmul(pl[lb][:, 0:MB], lhsT=identb[:], rhs=Rb[:, h, s0:s0 + MB], start=False, stop=True),
                     reads=[cx.d_c], writes=[d_pl[lb]])
                P.op('scalar', lambda e, lb=lb: e.activation(out=Pm[lb][:], in_=pl[lb][:, 0:MB], func=AF.Exp), writes=[d_pl[lb], d_Pm[lb]])
            else:
                P.op('scalar', lambda e, h=h, lb=lb: e.activation(out=Pm[lb][:], in_=pl[lb][:, 0:MB], func=AF.Exp, bias=R[:, h, RW - 1:RW], scale=1.0),
                     reads=[cx.d_c], writes=[d_pl[lb], d_Pm[lb]])

        def emit_pv(idx):
            h, t = tiles[idx]
            lb = (cnt + idx) % NL
            P.op('tensor', lambda e, h=h, t=t, lb=lb, nt=nt: e.matmul(po[:, 0:MB], lhsT=V[:, t, h * 128:(h + 1) * 128], rhs=Pm[lb][:],
                                                            start=(t == 0), stop=(t == nt - 1)),
                 reads=[d_V[t // 2], d_Pm[lb]], writes=[d_po])
            P.op('tensor', lambda e, t=t, lb=lb, nt=nt: e.matmul(pd[:, 0:MB], lhsT=ones_bf[:], rhs=Pm[lb][:], start=(t == 0), stop=(t == nt - 1)),
                 reads=[cx.d_c, d_Pm[lb]], writes=[d_pd])
            if t == nt - 1:
                ob = h
                P.op('vector', lambda e, ob=ob: e.reciprocal(out=rden[ob][:], in_=pd[:, 0:MB]), writes=[d_pd, d_rden[ob]])
                P.op('vector', lambda e, ob=ob: e.tensor_tensor(out=ot[ob][:], in0=po[:, 0:MB], in1=rden[ob][:], op=ALU.mult),
                     reads=[d_rden[ob]], writes=[d_po, d_ot[ob]])
                P.op('sync', lambda e, ob=ob, h=h, q0=q0: e.dma_start(out=a['oT'][h * 128:(h + 1) * 128, q0:q0 + MB], in_=ot[ob][:]),
                     reads=[d_ot[ob]], dma=True, is_out=True)

        LOOK = NL - 1
        for idx in range(min(LOOK, len(tiles))):
            emit_qk(idx)
        for idx in range(len(tiles)):
            emit_pv(idx)
            if idx + LOOK < len(tiles):
                emit_qk(idx + LOOK)
        cnt += len(tiles)
    P.emit()


GB = 256
NORM_EPS = 1e-6


def gla_host_consts():
    t = np.arange(GB)
    reset = np.broadcast_to((t % 64 != 0).astype(np.float32)[None, :], (128, GB)).copy()
    j = np.arange(128)[:, None]
    i = np.arange(128)[None, :]
    maskT = ((j // 64 == i // 64) & (j <= i)).astype(np.float32)
    return reset, maskT


def emit_stage_gla(nc, es, S, a, bg=None):
    cx = Ctx(nc, es)
    P = cx.P
    sb, ps = cx.sb, cx.ps
    NB = S // GB
    emit_consts(cx, a)
    reset = sb([128, GB], F32); maskT = sb([128, 128], F32)
    P.op('sync', lambda e: e.dma_start(out=reset[:], in_=a['reset']), writes=[cx.d_c], dma=True)
    P.op('sync', lambda e: e.dma_start(out=maskT[:], in_=a['maskT']), writes=[cx.d_c], dma=True)
    negb = sb([128, 1], F32); wcol = sb([128, 1], F32); neps = sb([128, 1], F32)
    P.op('vector', lambda e: e.memset(neps[:], NORM_EPS), writes=[cx.d_c])
    P.op('sync', lambda e: e.dma_start(out=negb[:], in_=a['gk_b_col']), writes=[cx.d_c], dma=True)
    P.op('vector', lambda e: e.tensor_scalar(out=negb[:], in0=negb[:], scalar1=-1.0, scalar2=None, op0=ALU.mult), reads=[cx.d_c], writes=[cx.d_c])
    P.op('sync', lambda e: e.dma_start(out=wcol[:], in_=a['norm_col']), writes=[cx.d_c], dma=True)

    pa = [ps([128, 512], F32) for _ in range(2)]; d_pa = P.deps(2)
    pA = ps([128, 512], F32); d_pA = P.dep()
    pO = ps([128, 512], F32); d_pO = P.dep()
    pS = ps([128, 512], F32); d_pS = P.dep()
    pss = ps([128, 512], F32); d_pss = P.dep()
    pz = ps([128, 512], F32); d_pz = P.dep()
    ptr = ps([128, 512], F32); d_ptr = P.dep()

    adaw = sb([128, 8, D], F32); d_adaw = P.dep()
    d_mod = P.dep()
    modcol = emit_modcols(cx, a, (0, 1), adaw, d_adaw, pz, d_pz, d_mod)
    P.op('vector', lambda e: e.tensor_scalar(out=modcol[:, 1, :], in0=modcol[:, 1, :], scalar1=1.0, scalar2=None, op0=ALU.add),
         reads=[d_mod], writes=[d_mod])

    wq = sb([128, 8, 128], BF16); wk = sb([128, 8, 128], BF16); wv = sb([128, 8, 256], BF16); wg = sb([128, 8, 256], BF16)
    wlr = sb([128, 8, 16], BF16); d_w = P.dep()
    for wt_, nm in ((wq, 'w_gq'), (wk, 'w_gk'), (wv, 'w_gv'), (wg, 'w_gg'), (wlr, 'w_glr')):
        P.op('gpsimd', lambda e, wt_=wt_, nm=nm: e.dma_start(out=wt_[:], in_=a[nm].rearrange("(j p) n -> p j n", p=128)), writes=[d_w], dma=True)
    w2f = sb([16, 128], F32); w2 = sb([16, 128], BF16)
    P.op('sync', lambda e: e.dma_start(out=w2f[:], in_=a['gk_w2']), writes=[d_w], dma=True)
    P.op('vector', lambda e: e.tensor_copy(out=w2[:], in_=w2f[:]), reads=[d_w], writes=[d_w])

    Sf = sb([128, 128], F32); d_Sf = P.dep()
    P.op('vector', lambda e: e.memset(Sf[:], 0.0), writes=[d_Sf])
    Sb = [sb([128, 128], BF16) for _ in range(3)]; d_Sb = P.deps(3)
    P.op('vector', lambda e: e.memset(Sb[0][:], 0.0), writes=[d_Sb[0]])

    xt = [sb([128, D], F32) for _ in range(2)]; d_xt = P.deps(2)
    hT = [sb([128, 8, GB], BF16) for _ in range(2)]; d_hT = P.deps(2)
    glr = sb([16, GB], BF16); d_glr = P.dep()
    e1 = sb([128, GB], F32); g_ = sb([128, GB], F32); bcs = sb([128, GB], F32); d2 = sb([128, GB], F32); d_gt = P.dep()
    eb = [sb([128, GB], F32) for _ in range(2)]; d_eb = P.deps(2)
    enb = sb([128, GB], F32); ekend = sb([128, GB], F32); d_en = P.dep()
    qe = [sb([128, GB], BF16) for _ in range(2)]; ke = [sb([128, GB], BF16) for _ in range(2)]; d_qk = P.deps(2)
    kendT = sb([128, GB], F32); d_kendT = P.dep()
    kend = [sb([128, 2, 128], BF16) for _ in range(2)]; d_kend = P.deps(2)
    Vt = [sb([128, 2, 256], BF16) for _ in range(2)]; d_Vt = P.deps(2)
    sgate = [sb([128, 2, GB], F32) for _ in range(2)]; d_sg = P.deps(2)
    ATm = [sb([128, 128], BF16) for _ in range(2)]; d_AT = P.deps(2)
    osb = [sb([128, 128], F32) for _ in range(2)]; osq = [sb([128, 128], F32) for _ in range(2)]; d_o = P.deps(2)
    rstd = [sb([128, 128], F32) for _ in range(2)]; d_rs = P.deps(2)
    yt = [sb([128, 128], F32) for _ in range(2)]; d_yt = P.deps(2)

    sbi = 0
    cnt = 0
    for n in range(NB):
        emit_bg(P, bg, n, NB)
        t0 = n * GB
        hb = n % 2
        emit_hT_block(cx, a['x'], t0, 2, xt, d_xt, pa, d_pa, modcol, d_mod, hT[hb], d_hT[hb])
        for j in range(8):
            P.op('tensor', lambda e, j=j, hb=hb: e.matmul(pz[0:16, 0:GB], lhsT=wlr[:, j, :], rhs=hT[hb][:, j, :], start=(j == 0), stop=(j == 7)),
                 reads=[d_w, d_hT[hb]], writes=[d_pz])
        P.op('vector', lambda e: e.tensor_copy(out=glr[:], in_=pz[0:16, 0:GB]), writes=[d_pz, d_glr])
        P.op('tensor', lambda e: e.matmul(pz[:, GB:2 * GB], lhsT=w2[:], rhs=glr[:], start=True, stop=True), reads=[d_w, d_glr], writes=[d_pz])
        P.op('scalar', lambda e: e.activation(out=e1[:], in_=pz[:, GB:2 * GB], func=AF.Exp, bias=negb[:], scale=-1.0), reads=[cx.d_c], writes=[d_pz, d_gt])
        P.op('scalar', lambda e: e.activation(out=e1[:], in_=e1[:], func=AF.Ln, bias=cx.ones[:, 0:1], scale=1.0), reads=[cx.d_c], writes=[d_gt])
        P.op('vector', lambda e: e.tensor_scalar(out=g_[:], in0=e1[:], scalar1=-1.0 / 16.0, scalar2=None, op0=ALU.mult), reads=[d_gt], writes=[d_gt])
        P.op('vector', lambda e: e.tensor_tensor_scan(out=bcs[:], data0=reset[:], data1=g_[:], initial=0.0, op0=ALU.mult, op1=ALU.add),
             reads=[d_gt, cx.d_c], writes=[d_gt])
        P.op('vector', lambda e: e.tensor_tensor(out=d2[:].rearrange("p (c t) -> p c t", t=64),
                                                 in0=bcs[:].rearrange("p (c t) -> p c t", t=64)[:, :, 63:64].broadcast_to([128, 4, 64]),
                                                 in1=bcs[:].rearrange("p (c t) -> p c t", t=64), op=ALU.subtract), reads=[d_gt], writes=[d_gt])
        P.op('scalar', lambda e, hb=hb: e.activation(out=eb[hb][:], in_=bcs[:], func=AF.Exp), reads=[d_gt], writes=[d_eb[hb]])
        P.op('scalar', lambda e: e.activation(out=enb[:], in_=bcs[:], func=AF.Exp, scale=-1.0), reads=[d_gt], writes=[d_en])
        P.op('scalar', lambda e: e.activation(out=ekend[:], in_=d2[:], func=AF.Exp), reads=[d_gt], writes=[d_en])
        for j in range(8):
            P.op('tensor', lambda e, j=j, hb=hb: e.matmul(pa[0][:, 0:GB], lhsT=wq[:, j, :], rhs=hT[hb][:, j, :], start=(j == 0), stop=(j == 7)),
                 reads=[d_w, d_hT[hb]], writes=[d_pa[0]])
        P.op('vector', lambda e, hb=hb: e.scalar_tensor_tensor(out=qe[hb][:], in0=pa[0][:, 0:GB], scalar=0.125, in1=eb[hb][:], op0=ALU.mult, op1=ALU.mult),
             reads=[d_eb[hb]], writes=[d_pa[0], d_qk[hb]])
        for j in range(8):
            P.op('tensor', lambda e, j=j, hb=hb: e.matmul(pa[1][:, 0:GB], lhsT=wk[:, j, :], rhs=hT[hb][:, j, :], start=(j == 0), stop=(j == 7)),
                 reads=[d_w, d_hT[hb]], writes=[d_pa[1]])
        P.op('vector', lambda e, hb=hb: e.tensor_tensor(out=ke[hb][:], in0=pa[1][:, 0:GB], in1=enb[:], op=ALU.mult), reads=[d_en], writes=[d_pa[1], d_qk[hb]])
        P.op('vector', lambda e: e.tensor_tensor(out=kendT[:], in0=pa[1][:, 0:GB], in1=ekend[:], op=ALU.mult), reads=[d_en], writes=[d_pa[1], d_kendT])
        for i in range(2):
            P.op('tensor', lambda e, i=i: e.transpose(ptr[:, i * 128:(i + 1) * 128], kendT[:, i * 128:(i + 1) * 128], cx.ident[:]),
                 reads=[d_kendT, cx.d_c], writes=[d_ptr])
        P.op('scalar', lambda e, hb=hb: e.activation(out=kend[hb][:].rearrange("p i d -> p (i d)"), in_=ptr[:, 0:256], func=AF.Identity), writes=[d_ptr, d_kend[hb]])
        for h in range(2):
            for j in range(8):
                P.op('tensor', lambda e, j=j, hb=hb, h=h: e.matmul(pa[h][:, 0:GB], lhsT=wg[:, j, h * 128:(h + 1) * 128], rhs=hT[hb][:, j, :], start=(j == 0), stop=(j == 7)),
                     reads=[d_w, d_hT[hb]], writes=[d_pa[h]])
            P.op('scalar', lambda e, hb=hb, h=h: e.activation(out=sgate[hb][:, h, :], in_=pa[h][:, 0:GB], func=AF.Silu), writes=[d_pa[h], d_sg[hb]])
        for i in range(2):
            for j in range(8):
                P.op('tensor', lambda e, i=i, j=j, hb=hb: e.matmul(pa[i][:, 0:256], lhsT=hT[hb][:, j, i * 128:(i + 1) * 128], rhs=wv[:, j, :], start=(j == 0), stop=(j == 7)),
                     reads=[d_w, d_hT[hb]], writes=[d_pa[i]])
            P.op('vector', lambda e, i=i, hb=hb: e.tensor_copy(out=Vt[hb][:, i, :], in_=pa[i][:, 0:256]), writes=[d_pa[i], d_Vt[hb]])
        for i in range(2):
            c0 = i * 128
            for h in range(2):
                hs = slice(h * 64, (h + 1) * 64)
                P.op('tensor', lambda e, hs=hs, hb=hb, c0=c0: e.matmul(pA[:, 0:128], lhsT=ke[hb][hs, c0:c0 + 128], rhs=qe[hb][hs, c0:c0 + 128], start=True, stop=True),
                     reads=[d_qk[hb]], writes=[d_pA])
                P.op('vector', lambda e, h=h: e.tensor_tensor(out=ATm[h][:], in0=pA[:, 0:128], in1=maskT[:], op=ALU.mult), reads=[cx.d_c], writes=[d_pA, d_AT[h]])
            sidx = [sbi, (sbi + 1) % 3, (sbi + 2) % 3]
            for c2 in range(2):
                cs = c0 + c2 * 64
                rows = slice(c2 * 64, (c2 + 1) * 64)
                nxt = sidx[c2 + 1]
                for h in range(2):
                    hs = slice(h * 64, (h + 1) * 64)
                    P.op('tensor', lambda e, h=h, hs=hs, hb=hb, i=i, rows=rows: e.matmul(pS[hs, 0:128], lhsT=kend[hb][rows, i, hs], rhs=Vt[hb][rows, i, h * 128:(h + 1) * 128],
                                                                                      start=True, stop=True), reads=[d_kend[hb], d_Vt[hb]], writes=[d_pS])
                P.op('vector', lambda e, hb=hb, cs=cs: e.scalar_tensor_tensor(out=Sf[:], in0=Sf[:], scalar=eb[hb][:, cs + 63:cs + 64], in1=pS[:, 0:128],
                                                                              op0=ALU.mult, op1=ALU.add), reads=[d_eb[hb]], writes=[d_pS, d_Sf])
                P.op('gpsimd', lambda e, nxt=nxt: e.tensor_copy(out=Sb[nxt][:], in_=Sf[:]), reads=[d_Sf], writes=[d_Sb[nxt]])
            sbi = sidx[2]
            for h in range(2):
                hs = slice(h * 64, (h + 1) * 64)
                P.op('tensor', lambda e, h=h, hb=hb, i=i: e.matmul(pO[:, 0:128], lhsT=Vt[hb][:, i, h * 128:(h + 1) * 128], rhs=ATm[h][:],
                                                                 start=True, stop=False), reads=[d_Vt[hb], d_AT[h]], writes=[d_pO])
                for c2 in range(2):
                    cs = c0 + c2 * 64
                    cur = sidx[c2]
                    P.op('tensor', lambda e, hs=hs, hb=hb, cs=cs, c2=c2, cur=cur: e.matmul(pO[:, c2 * 64:(c2 + 1) * 64], lhsT=Sb[cur][hs, :],
                                                                                         rhs=qe[hb][hs, cs:cs + 64], start=False, stop=(c2 == 1)),
                         reads=[d_Sb[cur], d_qk[hb]], writes=[d_pO])
                ob = h
                P.op('scalar', lambda e, ob=ob: e.activation(out=osb[ob][:], in_=pO[:, 0:128], func=AF.Identity), writes=[d_pO, d_o[ob]])
                P.op('scalar', lambda e, ob=ob: e.activation(out=osq[ob][:], in_=osb[ob][:], func=AF.Square), reads=[d_o[ob]], writes=[d_o[ob]])
                P.op('tensor', lambda e, ob=ob: e.matmul(pss[:, 0:128], lhsT=cx.ones[:], rhs=osq[ob][:], start=True, stop=True), reads=[cx.d_c, d_o[ob]], writes=[d_pss])
                P.op('scalar', lambda e, ob=ob: e.activation(out=rstd[ob][:], in_=pss[:, 0:128], func=AF.Ln, bias=neps[:], scale=1.0 / 128.0),
                     reads=[cx.d_c], writes=[d_pss, d_rs[ob]])
                P.op('scalar', lambda e, ob=ob: e.activation(out=rstd[ob][:], in_=rstd[ob][:], func=AF.Exp, scale=-0.5), writes=[d_rs[ob]])
                P.op('vector', lambda e, ob=ob: e.scalar_tensor_tensor(out=yt[ob][:], in0=osb[ob][:], scalar=wcol[:, 0:1], in1=rstd[ob][:], op0=ALU.mult, op1=ALU.mult),
                     reads=[d_o[ob], d_rs[ob], cx.d_c], writes=[d_yt[ob]])
                P.op('vector', lambda e, ob=ob, hb=hb, h=h, c0=c0: e.tensor_tensor(out=yt[ob][:], in0=yt[ob][:], in1=sgate[hb][:, h, c0:c0 + 128], op=ALU.mult),
                     reads=[d_sg[hb]], writes=[d_yt[ob]])
                P.op('sync', lambda e, ob=ob, h=h, t0=t0, c0=c0: e.dma_start(out=a['oT'][h * 128:(h + 1) * 128, t0 + c0:t0 + c0 + 128], in_=yt[ob][:]),
                     reads=[d_yt[ob]], dma=True, is_out=True)
    P.emit()


DB = 256
NORM_EPS = 1e-6
NEGM = -30000.0


def gdn_host_consts():
    t = np.arange(DB)
    reset = np.broadcast_to((t % 64 != 0).astype(np.float32)[None, :], (4, DB)).copy()
    i = np.arange(128)[:, None]
    j = np.arange(128)[None, :]
    same = (i // 64 == j // 64)
    m_incl = np.where(same & (j <= i), 0.0, NEGM).astype(np.float32)
    m_inclT = np.where(same & (i <= j), 0.0, NEGM).astype(np.float32)
    s01 = (same & (j < i)).astype(np.float32)
    s01T = (same & (i < j)).astype(np.float32)
    masks = np.stack([m_incl, m_inclT, s01, s01T]).astype(np.float32)
    sel = np.zeros((4, 4, 128), np.float32)
    for h in range(4):
        sel[h, h, :] = 1.0
    return reset, masks, sel


def emit_stage_gdn(nc, es, S, a, bg=None):
    bgq = bg
    cx = Ctx(nc, es)
    P = cx.P
    sb, ps = cx.sb, cx.ps
    NB = S // DB
    emit_consts(cx, a)
    d_c = cx.d_c
    reset = sb([4, DB], F32); masks = sb([128, 4, 128], F32); sel = sb([128, 4, 128], F32)
    P.op('vector', lambda e: e.memset(sel[:], 0.0), writes=[d_c])
    P.op('sync', lambda e: e.dma_start(out=reset[:], in_=a['reset']), writes=[d_c], dma=True)
    P.op('sync', lambda e: e.dma_start(out=masks[:], in_=a['masks'].rearrange("m p f -> p m f")), writes=[d_c], dma=True)
    P.op('sync', lambda e: e.dma_start(out=sel[0:4], in_=a['sel']), writes=[d_c], dma=True)
    identb = sb([128, 128], BF16)
    P.op('vector', lambda e: e.tensor_copy(out=identb[:], in_=cx.ident[:]), reads=[d_c], writes=[d_c])
    neps = sb([128, 1], F32); wcol = sb([128, 1], F32)
    P.op('vector', lambda e: e.memset(neps[:], NORM_EPS), writes=[d_c])
    P.op('sync', lambda e: e.dma_start(out=wcol[:], in_=a['norm_col']), writes=[d_c], dma=True)
    dtb = sb([4, 1], F32); negA = sb([4, 1], F32)
    P.op('sync', lambda e: e.dma_start(out=dtb[:], in_=a['dt_bias_col']), writes=[d_c], dma=True)
    P.op('sync', lambda e: e.dma_start(out=negA[:], in_=a['a_log_col']), writes=[d_c], dma=True)
    P.op('scalar', lambda e: e.activation(out=negA[:], in_=negA[:], func=AF.Exp), reads=[d_c], writes=[d_c])
    P.op('vector', lambda e: e.tensor_scalar(out=negA[:], in0=negA[:], scalar1=-1.0, scalar2=None, op0=ALU.mult), reads=[d_c], writes=[d_c])
    convw = sb([128, 12, 4], F32)
    P.op('sync', lambda e: e.dma_start(out=convw[:], in_=a['convw_col']), writes=[d_c], dma=True)

    pa = [ps([128, 512], F32) for _ in range(2)]; d_pa = P.deps(2)
    b1 = ps([128, 512], F32); b2 = ps([128, 512], F32); b3 = ps([128, 512], F32)
    d_b1, d_b2, d_b3 = P.deps(3)
    bg = ps([128, 512], F32); d_bg = P.dep()
    pO = ps([128, 512], F32); d_pO = P.dep()
    pV = ps([128, 512], F32); d_pV = P.dep()

    adaw = sb([128, 8, D], F32); d_adaw = P.dep()
    d_mod = P.dep()
    modcol = emit_modcols(cx, a, (0, 1), adaw, d_adaw, bg, d_bg, d_mod)
    P.op('vector', lambda e: e.tensor_scalar(out=modcol[:, 1, :], in0=modcol[:, 1, :], scalar1=1.0, scalar2=None, op0=ALU.add),
         reads=[d_mod], writes=[d_mod])

    wqkv = sb([128, 8, 1536], BF16); wgt = sb([128, 8, 512], BF16); wba = sb([128, 8, 2, 128], BF16); d_w = P.dep()
    P.op('vector', lambda e: e.memset(wba[:], 0.0), writes=[d_w])
    for k3, nm in enumerate(('w_q', 'w_k', 'w_v')):
        P.op('gpsimd', lambda e, k3=k3, nm=nm: e.dma_start(out=wqkv[:, :, k3 * 512:(k3 + 1) * 512], in_=a[nm].rearrange("(j p) n -> p j n", p=128)),
             writes=[d_w], dma=True)
    P.op('gpsimd', lambda e: e.dma_start(out=wgt[:], in_=a['w_gate'].rearrange("(j p) n -> p j n", p=128)), writes=[d_w], dma=True)
    for r_ in range(2):
        P.op('gpsimd', lambda e, r_=r_: e.dma_start(out=wba[:, :, r_, 0:4], in_=a['w_ba'][:, r_ * 4:(r_ + 1) * 4].rearrange("(j p) n -> p j n", p=128)), writes=[d_w], dma=True)

    Sf = sb([128, 4, 128], F32); Sb = sb([128, 4, 128], BF16); d_S = P.deps(4)
    for h in range(4):
        P.op('vector', lambda e, h=h: e.memset(Sf[:, h, :], 0.0), writes=[d_S[h]])
        P.op('vector', lambda e, h=h: e.memset(Sb[:, h, :], 0.0), writes=[d_S[h]])
    praw = sb([128, 12, 3 + DB], F32); d_praw = P.dep()
    P.op('vector', lambda e: e.memset(praw[:], 0.0), writes=[d_praw])

    xt = [sb([128, D], F32) for _ in range(2)]; d_xt = P.deps(2)
    hT = sb([128, 8, DB], BF16); d_hT = P.dep()
    cv = sb([128, 12, DB], F32); d_cv = P.dep(); d_cvp = P.dep()
    ctmp = sb([128, DB], F32); d_ctmp = P.dep()
    sq = sb([128, 8, DB], BF16); rn = sb([128, 8, DB], F32); d_rn = P.dep()
    onesb = sb([128, 128], BF16)
    P.op('vector', lambda e: e.memset(onesb[:], 1.0), writes=[d_c])
    qn = sb([128, 4, DB], F32); kn = sb([128, 4, DB], F32); d_qk = P.dep()
    sgate = sb([128, 4, DB], F32); d_sg = P.dep()
    RS = sb([128, 5, DB], F32); d_RS = P.dep()
    P.op('vector', lambda e: e.memset(RS[:], 0.0), writes=[d_RS])
    rtmp = sb([4, DB], F32)
    cols = sb([128, 2, 5, 4], F32); d_cols = P.dep()
    dl = sb([128, 4, 4], F32); d_dl = P.dep()
    kTb = sb([128, 4, DB], BF16); kbTb = sb([128, 4, DB], BF16); qTb = sb([128, 4, DB], BF16); qgTb = sb([128, 4, DB], BF16); d_fm = P.dep()
    vbeta = sb([128, 2, 4, 128], BF16); kbg = sb([128, 2, 4, 128], BF16); kend = sb([128, 2, 4, 128], BF16); d_tm = P.deps(2)
    dtmp = sb([128, 4, 128], F32); Dm = sb([128, 4, 128], F32); DmT = sb([128, 4, 128], F32); Ds = sb([128, 4, 128], F32); DsT = sb([128, 4, 128], F32)
    d_D = P.dep()
    X = [sb([128, 4, 128], BF16) for _ in range(2)]; XT = [sb([128, 4, 128], BF16) for _ in range(2)]; TT = [sb([128, 4, 128], BF16) for _ in range(2)]
    d_X = P.deps(2); d_XT = P.deps(2); d_TT = P.deps(2)
    attnT = sb([128, 4, 128], BF16); d_at = P.dep()
    wsb = sb([128, 4, 128], F32); kcT = sb([128, 4, 128], BF16); d_wk = P.dep()
    vnew = [[sb([128, 128], BF16) for _ in range(4)] for _ in range(2)]; d_vn = [P.deps(4) for _ in range(2)]
    for c2_ in range(2):
        for h_ in range(4):
            P.op('vector', lambda e, c2_=c2_, h_=h_: e.memset(vnew[c2_][h_][:], 0.0), writes=[d_vn[c2_][h_]])
    osb = [sb([128, 128], F32) for _ in range(2)]; osq = [sb([128, 128], BF16) for _ in range(2)]; d_o = P.deps(2)
    rstd = [sb([128, 128], F32) for _ in range(2)]; d_rs = P.deps(2)
    yt = [sb([128, 128], F32) for _ in range(2)]; d_yt = P.deps(2)
    osb4 = sb([128, 512], F32); osq4 = sb([128, 512], BF16); rstd4 = sb([128, 512], F32); yt4 = sb([128, 512], F32)
    d_o4, d_rs4, d_yt4 = P.deps(3)

    def v3(t):
        return t[:].rearrange("p (h f) -> p h f", h=4)

    for n in range(NB):
        emit_bg(P, bgq, n, NB)
        t0 = n * DB
        emit_hT_block(cx, a['x'], t0, 2, xt, d_xt, pa, d_pa, modcol, d_mod, hT, d_hT)
        for ct in range(12):
            pb = ct % 2
            for j in range(8):
                P.op('tensor', lambda e, ct=ct, j=j, pb=pb: e.matmul(pa[pb][:, 0:DB], lhsT=wqkv[:, j, ct * 128:(ct + 1) * 128], rhs=hT[:, j, :],
                                                                   start=(j == 0), stop=(j == 7)), reads=[d_w, d_hT], writes=[d_pa[pb]])
            P.op('scalar', lambda e, ct=ct, pb=pb: e.activation(out=praw[:, ct, 3:3 + DB], in_=pa[pb][:, 0:DB], func=AF.Identity),
                 writes=[d_pa[pb], d_praw])
        for h in range(4):
            pb = h % 2
            for j in range(8):
                P.op('tensor', lambda e, h=h, j=j, pb=pb: e.matmul(pa[pb][:, 0:DB], lhsT=wgt[:, j, h * 128:(h + 1) * 128], rhs=hT[:, j, :],
                                                                 start=(j == 0), stop=(j == 7)), reads=[d_w, d_hT], writes=[d_pa[pb]])
            P.op('scalar', lambda e, h=h, pb=pb: e.activation(out=sgate[:, h, :], in_=pa[pb][:, 0:DB], func=AF.Silu), writes=[d_pa[pb], d_sg])
        for r in range(2):
            for j in range(8):
                P.op('tensor', lambda e, r=r, j=j: e.matmul(bg[:, r * DB:(r + 1) * DB], lhsT=wba[:, j, r, :], rhs=hT[:, j, :],
                                                          start=(j == 0), stop=(j == 7)), reads=[d_w, d_hT], writes=[d_bg])
        P.op('scalar', lambda e: e.activation(out=RS[0:4, 0, :], in_=bg[0:4, 0:DB], func=AF.Sigmoid), writes=[d_bg, d_RS])
        P.op('scalar', lambda e: e.activation(out=rtmp[:], in_=bg[0:4, DB:2 * DB], func=AF.Exp, bias=dtb[:], scale=1.0), reads=[d_c], writes=[d_bg, d_RS])
        P.op('scalar', lambda e: e.activation(out=rtmp[:], in_=rtmp[:], func=AF.Ln, bias=cx.ones[0:4, 0:1], scale=1.0), reads=[d_c], writes=[d_RS])
        P.op('vector', lambda e: e.tensor_scalar(out=rtmp[:], in0=rtmp[:], scalar1=negA[:, 0:1], scalar2=None, op0=ALU.mult), reads=[d_c], writes=[d_RS])
        P.op('vector', lambda e: e.tensor_tensor_scan(out=RS[0:4, 1, :], data0=reset[:], data1=rtmp[:], initial=0.0, op0=ALU.mult, op1=ALU.add),
             reads=[d_c], writes=[d_RS])
        P.op('scalar', lambda e: e.activation(out=RS[0:4, 2, :], in_=RS[0:4, 1, :], func=AF.Exp), writes=[d_RS])
        P.op('vector', lambda e: e.tensor_tensor(out=RS[0:4, 3, :], in0=RS[0:4, 0, :], in1=RS[0:4, 2, :], op=ALU.mult), writes=[d_RS])
        P.op('vector', lambda e: e.tensor_tensor(out=rtmp[:].rearrange("p (c t) -> p c t", t=64),
                                                 in0=RS[0:4, 1, :].rearrange("p (c t) -> p c t", t=64)[:, :, 63:64].broadcast_to([4, 4, 64]),
                                                 in1=RS[0:4, 1, :].rearrange("p (c t) -> p c t", t=64), op=ALU.subtract), writes=[d_RS])
        P.op('scalar', lambda e: e.activation(out=RS[0:4, 4, :], in_=rtmp[:], func=AF.Exp), writes=[d_RS])
        for i in range(2):
            for q in range(5):
                P.op('tensor', lambda e, i=i, q=q: e.matmul(bg[:, (i * 5 + q) * 4:(i * 5 + q) * 4 + 4], lhsT=RS[:, q, i * 128:(i + 1) * 128], rhs=cx.ident[:, 0:4], start=True, stop=True),
                     reads=[d_RS, d_c], writes=[d_bg])
        P.op('vector', lambda e: e.tensor_copy(out=cols[:].rearrange("p i q h -> p (i q h)"), in_=bg[:, 0:40]), writes=[d_bg, d_cols])
        for ct in range(12):
            if ct >= 8:
                P.op('gpsimd', lambda e, ct=ct: e.tensor_scalar(out=cv[:, ct, :], in0=praw[:, ct, 0:DB], scalar1=convw[:, ct, 0:1], scalar2=None, op0=ALU.mult),
                     reads=[d_praw, d_c], writes=[d_cvp])
                for tap in range(1, 4):
                    P.op('gpsimd', lambda e, ct=ct, tap=tap: e.tensor_scalar(out=ctmp[:], in0=praw[:, ct, tap:tap + DB], scalar1=convw[:, ct, tap:tap + 1], scalar2=None, op0=ALU.mult),
                         reads=[d_praw, d_c], writes=[d_ctmp])
                    P.op('gpsimd', lambda e, ct=ct: e.tensor_tensor(out=cv[:, ct, :], in0=cv[:, ct, :], in1=ctmp[:], op=ALU.add), reads=[d_ctmp], writes=[d_cvp])
                continue
            P.op('vector', lambda e, ct=ct: e.tensor_scalar(out=cv[:, ct, :], in0=praw[:, ct, 0:DB], scalar1=convw[:, ct, 0:1], scalar2=None, op0=ALU.mult),
                 reads=[d_praw, d_c], writes=[d_cv])
            for tap in range(1, 4):
                P.op('vector', lambda e, ct=ct, tap=tap: e.scalar_tensor_tensor(out=cv[:, ct, :], in0=praw[:, ct, tap:tap + DB], scalar=convw[:, ct, tap:tap + 1],
                                                                                 in1=cv[:, ct, :], op0=ALU.mult, op1=ALU.add),
                     reads=[d_praw, d_c], writes=[d_cv])
        P.op('gpsimd', lambda e: e.tensor_copy(out=praw[:, :, 0:3], in_=praw[:, :, DB:DB + 3]), writes=[d_praw])
        P.op('scalar', lambda e: e.activation(out=cv[:].rearrange("p c t -> p (c t)"), in_=cv[:].rearrange("p c t -> p (c t)"), func=AF.Silu), writes=[d_cv, d_cvp])
        P.op('scalar', lambda e: e.activation(out=sq[:].rearrange("p c t -> p (c t)"), in_=cv[:, 0:8, :].rearrange("p c t -> p (c t)"), func=AF.Square),
             reads=[d_cv], writes=[d_rn])
        for pr in range(4):
            bank, d_bank = ((b1, d_b1), (b2, d_b2))[pr % 2]
            for u in range(2):
                ct = pr * 2 + u
                P.op('tensor', lambda e, bank=bank, u=u, ct=ct: e.matmul(bank[:, u * DB:(u + 1) * DB], lhsT=onesb[:], rhs=sq[:, ct, :], start=True, stop=True),
                     reads=[d_c, d_rn], writes=[d_bank])
            P.op('scalar', lambda e, bank=bank, pr=pr: e.activation(out=rn[:, pr * 2:pr * 2 + 2, :].rearrange("p c t -> p (c t)"), in_=bank[:], func=AF.Ln,
                                                                    bias=neps[:], scale=1.0), reads=[d_c], writes=[d_bank, d_rn])
        P.op('scalar', lambda e: e.activation(out=rn[:].rearrange("p c t -> p (c t)"), in_=rn[:].rearrange("p c t -> p (c t)"), func=AF.Exp, scale=-0.5),
             writes=[d_rn])
        P.op('vector', lambda e: e.scalar_tensor_tensor(out=qn[:], in0=cv[:, 0:4, :], scalar=128 ** -0.5, in1=rn[:, 0:4, :], op0=ALU.mult, op1=ALU.mult),
             reads=[d_cv, d_rn], writes=[d_qk])
        P.op('vector', lambda e: e.tensor_tensor(out=kn[:], in0=cv[:, 4:8, :], in1=rn[:, 4:8, :], op=ALU.mult), reads=[d_cv, d_rn], writes=[d_qk])
        P.op('gpsimd', lambda e: e.tensor_copy(out=kTb[:], in_=kn[:]), reads=[d_qk], writes=[d_fm])
        P.op('gpsimd', lambda e: e.tensor_copy(out=qTb[:], in_=qn[:]), reads=[d_qk], writes=[d_fm])
        for h in range(4):
            for q2, qi in enumerate((0, 2)):
                P.op('tensor', lambda e, h=h, q2=q2, qi=qi: e.matmul(bg[:, q2 * DB:(q2 + 1) * DB], lhsT=sel[:, h, :], rhs=RS[:, qi, :], start=True, stop=True),
                     reads=[d_c, d_RS], writes=[d_bg])
            P.op('vector', lambda e, h=h: e.tensor_tensor(out=kbTb[:, h, :], in0=kn[:, h, :], in1=bg[:, 0:DB], op=ALU.mult), reads=[d_qk], writes=[d_bg, d_fm])
            P.op('vector', lambda e, h=h: e.tensor_tensor(out=qgTb[:, h, :], in0=qn[:, h, :], in1=bg[:, DB:2 * DB], op=ALU.mult), reads=[d_qk], writes=[d_bg, d_fm])
            P.op('vector', lambda e, h=h: e.tensor_copy(out=dl[:, h, :], in_=bg[:, DB:2 * DB].rearrange("p (c t) -> p c t", t=64)[:, :, 63]),
                 writes=[d_bg, d_dl])
        for i in range(2):
            c0 = i * 128
            for h in range(4):
                P.op('tensor', lambda e, h=h, c0=c0: e.transpose(b1[:, h * 128:(h + 1) * 128], cv[:, 8 + h, c0:c0 + 128], cx.ident[:]),
                     reads=[d_cv, d_cvp, d_c], writes=[d_b1])
                P.op('tensor', lambda e, h=h, c0=c0: e.transpose(b2[:, h * 128:(h + 1) * 128], kn[:, h, c0:c0 + 128], cx.ident[:]),
                     reads=[d_qk, d_c], writes=[d_b2])
            P.op('vector', lambda e, i=i: e.tensor_tensor(out=vbeta[:, i, :, :], in0=v3(b1), in1=cols[:, i, 0, :].unsqueeze(2).broadcast_to([128, 4, 128]), op=ALU.mult),
                 reads=[d_cols], writes=[d_b1, d_tm[i]])
            P.op('vector', lambda e, i=i: e.tensor_tensor(out=kbg[:, i, :, :], in0=v3(b2), in1=cols[:, i, 3, :].unsqueeze(2).broadcast_to([128, 4, 128]), op=ALU.mult),
                 reads=[d_cols], writes=[d_b2, d_tm[i]])
            P.op('vector', lambda e, i=i: e.tensor_tensor(out=kend[:, i, :, :], in0=v3(b2), in1=cols[:, i, 4, :].unsqueeze(2).broadcast_to([128, 4, 128]), op=ALU.mult),
                 reads=[d_cols], writes=[d_b2, d_tm[i]])
            for h in range(4):
                P.op('tensor', lambda e, h=h, c0=c0: e.matmul(bg[:, h * 128:(h + 1) * 128], lhsT=sel[:, h, :], rhs=RS[:, 1, c0:c0 + 128], start=True, stop=True),
                     reads=[d_c, d_RS], writes=[d_bg])
            gcol = cols[:, i, 1, :].unsqueeze(2).broadcast_to([128, 4, 128])
            P.op('vector', lambda e, gcol=gcol: e.tensor_tensor(out=dtmp[:], in0=gcol, in1=v3(bg), op=ALU.subtract), reads=[d_cols], writes=[d_bg, d_D])
            P.op('gpsimd', lambda e: e.tensor_tensor(out=dtmp[:], in0=dtmp[:], in1=masks[:, 0, :].unsqueeze(1).broadcast_to([128, 4, 128]), op=ALU.add),
                 reads=[d_c], writes=[d_D])
            P.op('scalar', lambda e: e.activation(out=Dm[:].rearrange("p h f -> p (h f)"), in_=dtmp[:].rearrange("p h f -> p (h f)"), func=AF.Exp), writes=[d_D])
            P.op('gpsimd', lambda e: e.tensor_tensor(out=Ds[:], in0=Dm[:], in1=masks[:, 2, :].unsqueeze(1).broadcast_to([128, 4, 128]), op=ALU.mult),
                 reads=[d_c], writes=[d_D])
            P.op('vector', lambda e, gcol=gcol: e.tensor_tensor(out=dtmp[:], in0=v3(bg), in1=gcol, op=ALU.subtract), reads=[d_cols], writes=[d_bg, d_D])
            P.op('gpsimd', lambda e: e.tensor_tensor(out=dtmp[:], in0=dtmp[:], in1=masks[:, 1, :].unsqueeze(1).broadcast_to([128, 4, 128]), op=ALU.add),
                 reads=[d_c], writes=[d_D])
            P.op('scalar', lambda e: e.activation(out=DmT[:].rearrange("p h f -> p (h f)"), in_=dtmp[:].rearrange("p h f -> p (h f)"), func=AF.Exp), writes=[d_D])
            P.op('gpsimd', lambda e: e.tensor_tensor(out=DsT[:], in0=DmT[:], in1=masks[:, 3, :].unsqueeze(1).broadcast_to([128, 4, 128]), op=ALU.mult),
                 reads=[d_c], writes=[d_D])
            for h in range(4):
                P.op('tensor', lambda e, h=h, c0=c0: e.matmul(b1[:, h * 128:(h + 1) * 128], lhsT=kbTb[:, h, c0:c0 + 128], rhs=kTb[:, h, c0:c0 + 128], start=True, stop=True),
                     reads=[d_fm], writes=[d_b1])
                P.op('tensor', lambda e, h=h, c0=c0: e.matmul(b2[:, h * 128:(h + 1) * 128], lhsT=kTb[:, h, c0:c0 + 128], rhs=kbTb[:, h, c0:c0 + 128], start=True, stop=True),
                     reads=[d_fm], writes=[d_b2])
                P.op('tensor', lambda e, h=h, c0=c0: e.matmul(b3[:, h * 128:(h + 1) * 128], lhsT=kTb[:, h, c0:c0 + 128], rhs=qTb[:, h, c0:c0 + 128], start=True, stop=True),
                     reads=[d_fm], writes=[d_b3])
            P.op('vector', lambda e: e.scalar_tensor_tensor(out=X[0][:], in0=v3(b1), scalar=-1.0, in1=Ds[:], op0=ALU.mult, op1=ALU.mult), reads=[d_D], writes=[d_b1, d_X[0]])
            P.op('vector', lambda e: e.scalar_tensor_tensor(out=XT[0][:], in0=v3(b2), scalar=-1.0, in1=DsT[:], op0=ALU.mult, op1=ALU.mult), reads=[d_D], writes=[d_b2, d_XT[0]])
            P.op('vector', lambda e: e.tensor_tensor(out=attnT[:], in0=v3(b3), in1=DmT[:], op=ALU.mult), reads=[d_D], writes=[d_b3, d_at])
            P.op('gpsimd', lambda e: e.tensor_tensor(out=TT[0][:], in0=XT[0][:], in1=identb[:].unsqueeze(1).broadcast_to([128, 4, 128]), op=ALU.add),
                 reads=[d_XT[0], d_c], writes=[d_TT[0]])
            cur = 0
            for k in range(1, 6):
                nx = 1 - cur
                for h in range(4):
                    P.op('tensor', lambda e, h=h, cur=cur: e.matmul(b1[:, h * 128:(h + 1) * 128], lhsT=XT[cur][:, h, :], rhs=X[cur][:, h, :], start=True, stop=True),
                         reads=[d_X[cur], d_XT[cur]], writes=[d_b1])
                if k < 5:
                    for h in range(4):
                        P.op('tensor', lambda e, h=h, cur=cur: e.matmul(b2[:, h * 128:(h + 1) * 128], lhsT=X[cur][:, h, :], rhs=XT[cur][:, h, :], start=True, stop=True),
                             reads=[d_X[cur], d_XT[cur]], writes=[d_b2])
                P.op('scalar', lambda e, nx=nx: e.activation(out=X[nx][:].rearrange("p h f -> p (h f)"), in_=b1[:], func=AF.Identity), writes=[d_b1, d_X[nx]])
                if k < 5:
                    P.op('vector', lambda e, nx=nx: e.tensor_copy(out=XT[nx][:].rearrange("p h f -> p (h f)"), in_=b2[:]), writes=[d_b2, d_XT[nx]])
                for h in range(4):
                    P.op('tensor', lambda e, h=h, cur=cur, nx=nx: e.matmul(b3[:, h * 128:(h + 1) * 128], lhsT=X[nx][:, h, :], rhs=TT[cur][:, h, :], start=True, stop=True),
                         reads=[d_X[nx], d_TT[cur]], writes=[d_b3])
                P.op('vector', lambda e, cur=cur, nx=nx: e.tensor_tensor(out=TT[nx][:], in0=v3(b3), in1=TT[cur][:], op=ALU.add), reads=[d_TT[cur]], writes=[d_b3, d_TT[nx]])
                cur = nx
            tt = cur
            for h in range(4):
                P.op('tensor', lambda e, h=h, i=i, tt=tt: e.matmul(b2[:, h * 128:(h + 1) * 128], lhsT=TT[tt][:, h, :], rhs=vbeta[:, i, h, :], start=True, stop=True),
                     reads=[d_TT[tt], d_tm[i]], writes=[d_b2])
                P.op('tensor', lambda e, h=h, i=i, tt=tt: e.matmul(b3[:, h * 128:(h + 1) * 128], lhsT=kbg[:, i, h, :], rhs=TT[tt][:, h, :], start=True, stop=True),
                     reads=[d_TT[tt], d_tm[i]], writes=[d_b3])
            P.op('scalar', lambda e: e.activation(out=wsb[:].rearrange("p h f -> p (h f)"), in_=b2[:], func=AF.Identity), writes=[d_b2, d_wk])
            P.op('vector', lambda e: e.tensor_copy(out=kcT[:].rearrange("p h f -> p (h f)"), in_=b3[:]), writes=[d_b3, d_wk])
            for c2 in range(2):
                cs = c0 + c2 * 64
                rows = slice(c2 * 64, (c2 + 1) * 64)
                cl = slice(c2 * 64, (c2 + 1) * 64)
                ci = i * 2 + c2
                for h in range(4):
                    P.op('tensor', lambda e, h=h, rows=rows, cl=cl: e.matmul(pV[:, h * 128:(h + 1) * 128], lhsT=kcT[:, h, :], rhs=Sb[:, h, :], start=True, stop=True),
                         reads=[d_wk, d_S[h]], writes=[d_pV])
                    P.op('vector', lambda e, h=h, rows=rows, c2=c2: e.tensor_tensor(out=vnew[c2][h][rows, :], in0=wsb[rows, h, :], in1=pV[rows, h * 128:(h + 1) * 128], op=ALU.subtract),
                         reads=[d_wk], writes=[d_pV, d_vn[c2][h]])
                for h in range(4):
                    P.op('tensor', lambda e, h=h, cs=cs, cl=cl: e.matmul(pO[:, h * 128 + c2 * 64:h * 128 + (c2 + 1) * 64] if False else pO[:, h * 128 + cl.start:h * 128 + cl.stop],
                                                                       lhsT=Sb[:, h, :], rhs=qgTb[:, h, cs:cs + 64], start=True, stop=False),
                         reads=[d_S[h], d_fm], writes=[d_pO])
                    P.op('tensor', lambda e, h=h, rows=rows, cl=cl, c2=c2: e.matmul(pO[:, h * 128 + cl.start:h * 128 + cl.stop], lhsT=vnew[c2][h][:], rhs=attnT[:, h, cl],
                                                                           start=False, stop=True), reads=[d_vn[c2][h], d_at], writes=[d_pO])
                    P.op('tensor', lambda e, h=h, c2=c2, i=i: e.matmul(b1[:, h * 128:(h + 1) * 128], lhsT=kend[:, i, h, :], rhs=vnew[c2][h][:], start=True, stop=True),
                         reads=[d_vn[c2][h], d_tm[i]], writes=[d_b1])
                for h in range(4):
                    P.op('vector', lambda e, h=h, ci=ci: e.scalar_tensor_tensor(out=Sf[:, h, :], in0=Sf[:, h, :], scalar=dl[:, h, ci:ci + 1], in1=b1[:, h * 128:(h + 1) * 128],
                                                                                 op0=ALU.mult, op1=ALU.add), reads=[d_dl], writes=[d_b1, d_S[h]])
                    P.op('gpsimd', lambda e, h=h: e.tensor_copy(out=Sb[:, h, :], in_=Sf[:, h, :]), writes=[d_S[h]])
            P.op('scalar', lambda e: e.activation(out=osb4[:], in_=pO[:], func=AF.Identity), writes=[d_pO, d_o4])
            P.op('scalar', lambda e: e.activation(out=osq4[:], in_=osb4[:], func=AF.Square), writes=[d_o4])
            P.op('tensor', lambda e: e.matmul(bg[:], lhsT=onesb[:], rhs=osq4[:], start=True, stop=True), reads=[d_c, d_o4], writes=[d_bg])
            P.op('scalar', lambda e: e.activation(out=rstd4[:], in_=bg[:], func=AF.Ln, bias=neps[:], scale=1.0 / 128.0), reads=[d_c], writes=[d_bg, d_rs4])
            P.op('scalar', lambda e: e.activation(out=rstd4[:], in_=rstd4[:], func=AF.Exp, scale=-0.5), writes=[d_rs4])
            P.op('vector', lambda e: e.scalar_tensor_tensor(out=yt4[:], in0=osb4[:], scalar=wcol[:, 0:1], in1=rstd4[:], op0=ALU.mult, op1=ALU.mult),
                 reads=[d_o4, d_rs4, d_c], writes=[d_yt4])
            P.op('vector', lambda e, c0=c0: e.tensor_tensor(out=yt4[:].rearrange("p (h f) -> p h f", h=4), in0=yt4[:].rearrange("p (h f) -> p h f", h=4),
                                                            in1=sgate[:, :, c0:c0 + 128], op=ALU.mult), reads=[d_sg], writes=[d_yt4])
            for h in range(4):
                P.op('sync', lambda e, h=h, t0=t0, c0=c0: e.dma_start(out=a['oT'][h * 128:(h + 1) * 128, t0 + c0:t0 + c0 + 128], in_=yt4[:, h * 128:(h + 1) * 128]),
                     reads=[d_yt4], dma=True, is_out=True)
    P.emit()

SEQ = 8192
NB_ = 4
NCORES = 4


def _col(v, n):
    return np.ascontiguousarray(np.asarray(v, np.float32).reshape(n, 128).T)


_NC_CACHE = {}


def _build_fused(S=SEQ):
    if S in _NC_CACHE:
        return _NC_CACHE[S]
    nc = bass.Bass("TRN2", target_bir_lowering=False)
    di = lambda name, shape: dram_in(nc, name, shape)
    I = dict(x=di("x", [S, D]), ccol=di("ccol", [128, 8]), ada_w=di("ada_w", [2, D, 6 * D]), ada_b_col=di("ada_b_col", [2, 128, 48]),
             ada_b_row=di("ada_b_row", [2, 1, 6 * D]), ident=di("ident", [128, 128]), ln=di("ln", [2, 4, D]),
             ev_w_in=di("ev_w_in", [D, 3088]), rtab=di("rtab", [4, 128, RW]), eoh=di("eoh", [32, 32 * 128]),
             gk_w2=di("gk_w2", [16, 256]), gk_b_col=di("gk_b_col", [2, 128, 1]), ev_norm_col=di("ev_norm_col", [128, 1]),
             reset_g=di("reset_g", [128, GB]), maskT_g=di("maskT_g", [128, 128]), ev_w_out=di("ev_w_out", [D, D]),
             od_w_in=di("od_w_in", [D, 4112]), w_ba=di("w_ba", [2, D, 8]), convw_col=di("convw_col", [2, 128, 12, 4]),
             dt_bias_col=di("dt_bias_col", [2, 4, 1]), a_log_col=di("a_log_col", [2, 4, 1]), od_norm_col=di("od_norm_col", [128, 1]),
             reset_d=di("reset_d", [4, DB]), masks_d=di("masks_d", [4, 128, 128]), sel_d=di("sel_d", [4, 4, 128]), od_w_out=di("od_w_out", [D, D]),
             router_w=di("router_w", [2, D, NE]), router_b=di("router_b", [2, 1, NE]),
             wg=di("wg", [2, NE, D, 256]), wu=di("wu", [2, NE, D, 256]), wd=di("wd", [2, NE, 256, D]),
             shg=di("shg", [2, D, 256]), shu=di("shu", [2, D, 256]), shd=di("shd", [2, 256, D]))
    y = dram_out(nc, "y", [S, D])
    mixT = nc.dram_tensor("mixT_scratch", [D, S], F32, kind="Internal").ap()
    xmid = nc.dram_tensor("xmid_scratch", [S, D], F32, kind="Internal").ap()

    def stage(fn, *args, **kw):
        with nc.cleanup_on_exit():
            with ExitStack() as es:
                fn(nc, es, *args, **kw)

    wbf = [nc.dram_tensor("wbf_scratch%d" % l, [NE + 1, 128, 6144], BF16, kind="Internal").ap() for l in range(2)]

    def conv_thunks(layer, e_lo, e_hi):
        th = []
        for e_i in range(e_lo, e_hi):
            if e_i < NE:
                srcs = (I['wg'][layer][e_i], I['wu'][layer][e_i], I['wd'][layer][e_i])
            else:
                srcs = (I['shg'][layer], I['shu'][layer], I['shd'][layer])
            dst = wbf[layer][e_i]
            for k3, (lo, n_) in enumerate(((0, 256), (2048, 256), (4096, D))):
                th.append(lambda e, s_=srcs[k3], d_=dst, lo=lo, n_=n_: e.dma_start(
                    out=d_[:, lo:lo + 2048].rearrange("p (j n) -> p j n", n=n_), in_=s_.rearrange("(j p) n -> p j n", p=128)))
        return th

    def com(layer, xin):
        return dict(x=xin, ccol=I['ccol'], ada_w=I['ada_w'][layer], ada_b_col=I['ada_b_col'][layer], ident=I['ident'])

    w = I['ev_w_in']
    for g in range(2):
        hs = slice(g * 256, (g + 1) * 256)
        stage(emit_stage_moba, S, dict(com(0, I['x']), w_q=w[:, hs], w_k=w[:, 512 + g * 256:512 + (g + 1) * 256],
                                       w_v=w[:, 1024 + g * 256:1024 + (g + 1) * 256], rtab=I['rtab'][2 * g:2 * g + 2], eoh=I['eoh'],
                                       oT=mixT[g * 256:(g + 1) * 256, :]), bg=conv_thunks(0, (0, 23)[g], (23, 46)[g]))
    for g in range(2):
        ks = slice(g * 128, (g + 1) * 128)
        stage(emit_stage_gla, S, dict(com(0, I['x']), w_gq=w[:, 1536 + g * 128:1536 + (g + 1) * 128], w_gk=w[:, 1792 + g * 128:1792 + (g + 1) * 128],
                                      w_gv=w[:, 2048 + g * 256:2048 + (g + 1) * 256], w_gg=w[:, 2560 + g * 256:2560 + (g + 1) * 256],
                                      w_glr=w[:, 3072:3088], gk_w2=I['gk_w2'][:, ks], gk_b_col=I['gk_b_col'][g], norm_col=I['ev_norm_col'],
                                      reset=I['reset_g'], maskT=I['maskT_g'], oT=mixT[512 + g * 256:512 + (g + 1) * 256, :]),
              bg=conv_thunks(0, (46, 56)[g], (56, 65)[g]))

    def cargs(layer, xin, w_out, out):
        return dict(mixT=mixT, x=xin, w_out=w_out, ccol=I['ccol'], ada_w=I['ada_w'][layer], ada_b_row=I['ada_b_row'][layer],
                    ada_b_col=I['ada_b_col'][layer], ln=I['ln'][layer], router_w=I['router_w'][layer], router_b=I['router_b'][layer],
                    wbf=wbf[layer],
                    ident=I['ident'], out=out)

    stage(emit_stage_c, S, cargs(0, I['x'], I['ev_w_out'], xmid))
    w = I['od_w_in']
    for g in range(2):
        stage(emit_stage_gdn, S, dict(com(1, xmid), w_q=w[:, g * 512:(g + 1) * 512], w_k=w[:, 1024 + g * 512:1024 + (g + 1) * 512],
                                      w_v=w[:, 2048 + g * 512:2048 + (g + 1) * 512], w_gate=w[:, 3072 + g * 512:3072 + (g + 1) * 512],
                                      w_ba=I['w_ba'][g], convw_col=I['convw_col'][g], dt_bias_col=I['dt_bias_col'][g], a_log_col=I['a_log_col'][g],
                                      norm_col=I['od_norm_col'], reset=I['reset_d'], masks=I['masks_d'], sel=I['sel_d'],
                                      oT=mixT[g * 512:(g + 1) * 512, :]), bg=conv_thunks(1, (0, 33)[g], (33, 65)[g]))
    stage(emit_stage_c, S, cargs(1, xmid, I['od_w_out'], y))
    _NC_CACHE[S] = nc
    return nc


def host_inputs(x, c, rpe_bias, ada_w, ada_b, ln_mix_g, ln_mix_b, ln_ffn_g, ln_ffn_b,
                ev_w_in, ev_gk_w2, ev_gk_b, ev_norm, ev_w_out,
                od_w_in, od_conv_w, od_a_log, od_dt_bias, od_norm, od_w_out,
                moe_router_w, moe_router_b, moe_w_gate, moe_w_up, moe_w_down,
                sh_w_gate, sh_w_up, sh_w_down):
    f = lambda t: np.ascontiguousarray(np.asarray(t, dtype=np.float32))
    ada_b = f(ada_b)
    eoh = np.zeros((32, 32, 128), np.float32)
    for j in range(32):
        eoh[j, j, :] = 1.0
    reset_g, maskT_g = gla_host_consts()
    reset_d, masks_d, sel_d = gdn_host_consts()
    w_in1 = f(od_w_in)[0]
    convw = f(od_conv_w)[0]
    w_ba, cwc = [], []
    for g in range(2):
        w_ba.append(np.concatenate([w_in1[:, 4096 + g * 4:4096 + (g + 1) * 4], w_in1[:, 4104 + g * 4:4104 + (g + 1) * 4]], axis=1))
        cw = np.concatenate([convw[:, g * 512:(g + 1) * 512], convw[:, 1024 + g * 512:1024 + (g + 1) * 512],
                             convw[:, 2048 + g * 512:2048 + (g + 1) * 512]], axis=1)
        cwc.append(cw.reshape(4, 12, 128).transpose(2, 1, 0))
    return dict(
        ada_w=f(ada_w), ada_b_col=f(np.stack([_col(ada_b[l], 48) for l in range(2)])), ada_b_row=f(ada_b.reshape(2, 1, -1)),
        ident=np.eye(128, dtype=np.float32),
        ln=f(np.stack([np.stack([f(ln_mix_g)[l], f(ln_mix_b)[l], f(ln_ffn_g)[l], f(ln_ffn_b)[l]]) for l in range(2)])),
        ev_w_in=f(ev_w_in)[0], rtab=moba_host_tables(f(rpe_bias), [0, 1, 2, 3]), eoh=eoh.reshape(32, -1),
        gk_w2=f(ev_gk_w2)[0], gk_b_col=f(f(ev_gk_b)[0].reshape(2, 128, 1)), ev_norm_col=f(f(ev_norm)[0].reshape(128, 1)),
        reset_g=reset_g, maskT_g=maskT_g, ev_w_out=f(ev_w_out)[0],
        od_w_in=w_in1, w_ba=f(np.stack(w_ba)), convw_col=f(np.stack(cwc)),
        dt_bias_col=f(f(od_dt_bias)[0].reshape(2, 4, 1)), a_log_col=f(f(od_a_log)[0].reshape(2, 4, 1)), od_norm_col=f(f(od_norm)[0].reshape(128, 1)),
        reset_d=reset_d, masks_d=masks_d, sel_d=sel_d, od_w_out=f(od_w_out)[0],
        router_w=f(moe_router_w), router_b=f(f(moe_router_b).reshape(2, 1, -1)),
        wg=f(moe_w_gate), wu=f(moe_w_up), wd=f(moe_w_down), shg=f(sh_w_gate), shu=f(sh_w_up), shd=f(sh_w_down))


def kernel(x, c, **kw):
    f = lambda t: np.ascontiguousarray(np.asarray(t, dtype=np.float32))
    x = f(x); c = f(c)
    shared = host_inputs(x, c, **kw)
    nc = _build_fused()
    in_maps = [dict(shared, x=x[b], ccol=_col(c[b], 8)) for b in range(NCORES)]
    res = run_bass_kernel_spmd(nc, in_maps, core_ids=list(range(NCORES)))
    return np.stack([res.results[b]["y"] for b in range(NCORES)])
```

```python
import math
import numpy as np
import concourse.bass as bass
import concourse.mybir as mybir
from concourse.bass_utils import run_bass_kernel_spmd
from contextlib import ExitStack

F32 = mybir.dt.float32
BF16 = mybir.dt.bfloat16
AF = mybir.ActivationFunctionType
ALU = mybir.AluOpType
AX = mybir.AxisListType

ENGS = ['tensor', 'vector', 'scalar', 'gpsimd', 'sync']
DMAQ = ('sync', 'gpsimd')


class Dep:
    __slots__ = ('w', 'r', 'rd')

    def __init__(self):
        self.w = None
        self.r = {}
        self.rd = []


class Op:
    __slots__ = ('eng', 'fn', 'waits', 'signal', 'value', 'dma', 'slot', 'idx')


class Prog:
    NDMA = 12
    uid = 0

    def __init__(self, nc, es):
        self.nc = nc
        self.es = es
        self.ops = {e: [] for e in ENGS}
        Prog.uid += 1
        u = Prog.uid
        self.sem = {e: nc.alloc_semaphore(name='s%d_%s' % (u, e)) for e in ENGS}
        self.dsem = {q: [nc.alloc_semaphore(name='d%d_%s%d' % (u, q, i)) for i in range(self.NDMA)]
                     for q in DMAQ}
        self.dcount = {q: 0 for q in DMAQ}
        self.dlast = {q: [None] * self.NDMA for q in DMAQ}
        self.n = 0
        self.out_dmas = []
        self.bg_sem = nc.alloc_semaphore(name='bg%d' % u)
        self.bg_count = 0

    def dep(self):
        return Dep()

    def deps(self, n):
        return [Dep() for _ in range(n)]

    def op(self, eng, fn, reads=(), writes=(), dma=False, is_out=False):
        o = Op()
        o.eng = eng
        o.fn = fn
        o.dma = dma
        o.signal = False
        o.value = None
        o.slot = None
        o.idx = self.n
        self.n += 1
        raw = []
        other = []
        for d in reads:
            if d.w is not None:
                raw.append(d.w)
        for d in writes:
            if d.w is not None:
                raw.append(d.w)
            other.extend(d.r.values())
            other.extend(d.rd)
        if dma:
            j = self.dcount[eng]
            self.dcount[eng] += 1
            s = j % self.NDMA
            o.slot = s
            o.value = 16 * (j // self.NDMA + 1)
            prev = self.dlast[eng][s]
            if prev is not None:
                raw.append(prev)
            self.dlast[eng][s] = o
        waits = {}
        for p in raw:
            if p.dma or p.eng != eng or eng != 'tensor':
                waits[p.idx] = p
        for p in other:
            if p.dma or p.eng != eng:
                waits[p.idx] = p
        if dma:
            for p in other:
                waits[p.idx] = p
        o.waits = list(waits.values())
        for p in o.waits:
            p.signal = True
        for d in reads:
            if dma:
                d.rd.append(o)
            else:
                d.r[eng] = o
        for d in writes:
            d.w = o
            d.r = {}
            d.rd = []
        self.ops[eng].append(o)
        if is_out:
            self.out_dmas.append(o)
        return o

    def bg_dma(self, eng, fn):
        o = Op()
        o.eng = eng
        o.fn = fn
        o.dma = 'bg'
        o.signal = False
        o.value = None
        o.slot = None
        o.idx = self.n
        self.n += 1
        o.waits = []
        self.bg_count += 1
        self.ops[eng].append(o)
        return o

    def finish(self):
        o = Op()
        o.eng = 'sync'
        o.fn = None
        o.dma = False
        o.signal = False
        o.value = None
        o.slot = None
        o.idx = self.n
        self.n += 1
        o.waits = list(self.out_dmas)
        self.final_op = o
        self.ops['sync'].append(o)
        for e in ENGS:
            c = 0
            for o in self.ops[e]:
                if o.dma:
                    continue
                if o.signal:
                    c += 1
                    o.value = c

    def emit(self):
        self.finish()
        nc = self.nc
        with nc.Block() as block:
            for e in ENGS:
                getattr(block, e)(self._body(e))

    def _body(self, e):
        def body(eng):
            waited = {}
            for o in self.ops[e]:
                need = {}
                for p in o.waits:
                    if p.dma:
                        key = ('d', p.eng, p.slot)
                        sem = self.dsem[p.eng][p.slot]
                    else:
                        key = ('c', p.eng)
                        sem = self.sem[p.eng]
                    v = p.value
                    assert v is not None
                    if key not in need or need[key][1] < v:
                        need[key] = (sem, v)
                for key, (sem, v) in need.items():
                    if waited.get(key, 0) >= v:
                        continue
                    eng.wait_ge(sem, v)
                    waited[key] = v
                if o.fn is None:
                    if o is self.final_op and self.bg_count:
                        eng.wait_ge(self.bg_sem, 16 * self.bg_count)
                    continue
                inst = o.fn(eng)
                if o.dma == 'bg':
                    inst.then_inc(self.bg_sem, 16)
                elif o.dma:
                    inst.then_inc(self.dsem[e][o.slot], 16)
                elif o.signal:
                    inst.then_inc(self.sem[e], 1)
        return body


D = 1024
NE = 64
ALPHA = float(4 ** 0.25)
LN_EPS = 1e-5


class Ctx:
    def __init__(self, nc, es):
        self.nc = nc
        self.es = es
        self.P = Prog(nc, es)

    k = 0

    def sb(self, shape, dt, name=None):
        Ctx.k += 1
        return self.es.enter_context(self.nc.sbuf_tensor(name or "sb%d" % Ctx.k, shape, dt))

    def ps(self, shape, dt, name=None):
        Ctx.k += 1
        return self.es.enter_context(self.nc.psum_tensor(name or "ps%d" % Ctx.k, shape, dt))


def dram_in(nc, name, shape, dt=F32):
    return nc.dram_tensor(name, list(shape), dt, kind="ExternalInput").ap()


def dram_out(nc, name, shape, dt=F32):
    return nc.dram_tensor(name, list(shape), dt, kind="ExternalOutput").ap()


def emit_bg(P, bg, n, NB):
    if not bg:
        return
    for fn in bg[len(bg) * n // NB:len(bg) * (n + 1) // NB]:
        P.bg_dma('gpsimd', fn)


def emit_consts(cx, a):
    P = cx.P
    sb = cx.sb
    cx.d_c = P.dep()
    cx.ident = sb([128, 128], F32)
    P.op('sync', lambda e: e.dma_start(out=cx.ident[:], in_=a['ident']), writes=[cx.d_c], dma=True)
    cx.eps_col = sb([128, 1], F32)
    P.op('vector', lambda e: e.memset(cx.eps_col[:], LN_EPS), writes=[cx.d_c])
    cx.ones = sb([128, 128], F32)
    P.op('vector', lambda e: e.memset(cx.ones[:], 1.0), writes=[cx.d_c])
    ccol = sb([128, 8], F32)
    P.op('sync', lambda e: e.dma_start(out=ccol[:], in_=a['ccol']), writes=[cx.d_c], dma=True)
    cx.cact = sb([128, 8], F32)
    P.op('scalar', lambda e: e.activation(out=cx.cact[:], in_=ccol[:], func=AF.Silu), reads=[cx.d_c], writes=[cx.d_c])
    cx.cact2 = sb([128, 8, 2], F32)
    for r in range(2):
        P.op('vector', lambda e, r=r: e.tensor_copy(out=cx.cact2[:, :, r], in_=cx.cact[:]), reads=[cx.d_c], writes=[cx.d_c])


def emit_modcols(cx, a, secs, adaw, d_adaw, pm, d_pm, d_out):
    P = cx.P
    adab_col = cx.sb([128, 48], F32)
    P.op('sync', lambda e: e.dma_start(out=adab_col[:], in_=a['ada_b_col']), writes=[d_out], dma=True)
    modcol = cx.sb([128, len(secs), 8], F32)
    for i, sec in enumerate(secs):
        P.op('sync', lambda e, sec=sec: e.dma_start(out=adaw[:], in_=a['ada_w'][:, sec * D:(sec + 1) * D].rearrange("(j p) n -> p j n", p=128)),
             writes=[d_adaw], dma=True)
        for jc in range(8):
            for j in range(8):
                P.op('tensor', lambda e, jc=jc, j=j: e.matmul(pm[:, jc * 2:jc * 2 + 2], lhsT=adaw[:, j, jc * 128:(jc + 1) * 128],
                                                             rhs=cx.cact2[:, j, :], start=(j == 0), stop=(j == 7)),
                     reads=[cx.d_c, d_adaw], writes=[d_pm])
        P.op('vector', lambda e, i=i, sec=sec: e.tensor_tensor(out=modcol[:, i, :], in0=pm[:, 0:16].rearrange("p (j r) -> p j r", r=2)[:, :, 0],
                                                              in1=adab_col[:, sec * 8:(sec + 1) * 8], op=ALU.add),
             reads=[d_pm, d_out], writes=[d_out])
    return modcol


def emit_hT_block(cx, a_x, t0, ntile, xt, d_xt, pts, d_pts, modcol, d_mod, hT, d_hT, hT_off=0):
    P = cx.P
    for i in range(ntile):
        b = i % 2
        P.op('sync', lambda e, b=b, i=i: e.dma_start(out=xt[b][:], in_=a_x[t0 + i * 128:t0 + (i + 1) * 128, :]), writes=[d_xt[b]], dma=True)
        for half in range(2):
            pt = pts[half]
            for j4 in range(4):
                j = half * 4 + j4
                P.op('tensor', lambda e, b=b, pt=pt, j=j, j4=j4: e.transpose(pt[:, j4 * 128:(j4 + 1) * 128], xt[b][:, j * 128:(j + 1) * 128], cx.ident[:]),
                     reads=[d_xt[b], cx.d_c], writes=[d_pts[half]])
            for j4 in range(4):
                j = half * 4 + j4
                P.op('vector', lambda e, pt=pt, j=j, j4=j4, i=i: e.tensor_scalar(out=hT[:, j, hT_off + i * 128:hT_off + (i + 1) * 128],
                                                                                in0=pt[:, j4 * 128:(j4 + 1) * 128],
                                                                                scalar1=modcol[:, 1, j:j + 1], scalar2=modcol[:, 0, j:j + 1],
                                                                                op0=ALU.mult, op1=ALU.add),
                     reads=[d_pts[half], d_mod], writes=[d_hT])


def layer_norm_tile(cx, v, d_v, out, d_out, grow, brow, d_rows, st, d_st):
    P = cx.P
    stats, mv, rstd = st
    for h in range(2):
        P.op('vector', lambda e, h=h: e.bn_stats(out=stats[:, h * 6:(h + 1) * 6], in_=v[:, h * 512:(h + 1) * 512]),
             reads=[d_v], writes=[d_st])
    P.op('vector', lambda e: e.bn_aggr(out=mv[:], in_=stats[:]), reads=[d_st], writes=[d_st])
    P.op('scalar', lambda e: e.activation(out=rstd[:], in_=mv[:, 1:2], func=AF.Sqrt, bias=cx.eps_col[:], scale=1.0),
         reads=[d_st], writes=[d_st])
    P.op('vector', lambda e: e.reciprocal(out=rstd[:], in_=rstd[:]), reads=[d_st], writes=[d_st])
    P.op('vector', lambda e: e.tensor_scalar(out=out[:], in0=v[:], scalar1=mv[:, 0:1], scalar2=rstd[:, 0:1],
                                             op0=ALU.subtract, op1=ALU.mult), reads=[d_v, d_st], writes=[d_out])
    P.op('gpsimd', lambda e: e.tensor_tensor(out=out[:], in0=out[:], in1=grow, op=ALU.mult),
         reads=[d_out, d_rows], writes=[d_out])
    P.op('gpsimd', lambda e: e.tensor_tensor(out=out[:], in0=out[:], in1=brow, op=ALU.add),
         reads=[d_out, d_rows], writes=[d_out])


def emit_stage_c(nc, es, T, a, PASS=2048):
    cx = Ctx(nc, es)
    P = cx.P
    sb, ps = cx.sb, cx.ps
    PASS = min(PASS, T)
    NT = PASS // 128
    NTT = PASS // 512
    npass = T // PASS

    ident = sb([128, 128], F32); d_c = P.dep()
    P.op('sync', lambda e: e.dma_start(out=ident[:], in_=a['ident']), writes=[d_c], dma=True)
    cx.eps_col = sb([128, 1], F32)
    P.op('vector', lambda e: e.memset(cx.eps_col[:], LN_EPS), writes=[d_c])
    ones = sb([128, 128], F32)
    P.op('vector', lambda e: e.memset(ones[:], 1.0), writes=[d_c])
    ccol = sb([128, 8], F32)
    P.op('sync', lambda e: e.dma_start(out=ccol[:], in_=a['ccol']), writes=[d_c], dma=True)
    cact = sb([128, 8], F32)
    P.op('scalar', lambda e: e.activation(out=cact[:], in_=ccol[:], func=AF.Silu), reads=[d_c], writes=[d_c])
    h2f = [sb([128, 8, 128], F32)]; d_h2f = P.deps(1)
    crep = h2f[0]
    for j in range(8):
        P.op('vector', lambda e, j=j: e.tensor_scalar(out=crep[:, j, :], in0=ones[:], scalar1=cact[:, j:j + 1], scalar2=None,
                                                      op0=ALU.mult), reads=[d_c], writes=[d_h2f[0]])
    cact2 = sb([128, 8, 2], F32)
    for r in range(2):
        P.op('vector', lambda e, r=r: e.tensor_copy(out=cact2[:, :, r], in_=cact[:]), reads=[d_c], writes=[d_c])
    lnrows = sb([128, 4, D], F32); d_rows = P.dep()
    for i in range(4):
        P.op('sync', lambda e, i=i: e.dma_start(out=lnrows[:, i, :], in_=a['ln'][i:i + 1, :].partition_broadcast(128)),
             writes=[d_rows], dma=True)
    grow = sb([128, 2, D], F32)
    for i, sec in enumerate((2, 5)):
        P.op('sync', lambda e, i=i, sec=sec: e.dma_start(out=grow[:, i, :],
                                                         in_=a['ada_b_row'][0:1, sec * D:(sec + 1) * D].partition_broadcast(128)),
             writes=[d_rows], dma=True)
    adab_col = sb([128, 48], F32)
    P.op('sync', lambda e: e.dma_start(out=adab_col[:], in_=a['ada_b_col']), writes=[d_rows], dma=True)
    modcol = sb([128, 2, 8], F32)
    acc = sb([128, max(NT, 8), D], F32); d_acc = P.deps(max(NT, 8))
    d_adaw8 = d_acc[0:8]
    adaw = acc[:, 0:8, :]
    pmod = [ps([128, 512], F32), ps([128, 512], F32)]; d_pmod = [P.dep(), P.dep()]
    for i, sec in enumerate((2, 5)):
        P.op('sync', lambda e, sec=sec: e.dma_start(out=adaw[:], in_=a['ada_w'][:, sec * D:(sec + 1) * D].rearrange("(j p) n -> p j n", p=128)),
             writes=d_adaw8, dma=True)
        for h in range(2):
            for j in range(8):
                P.op('tensor', lambda e, h=h, j=j: e.matmul(pmod[h][:], lhsT=crep[:, j, :], rhs=adaw[:, j, h * 512:(h + 1) * 512],
                                                           start=(j == 0), stop=(j == 7)),
                     reads=[d_c, d_h2f[0]] + d_adaw8, writes=[d_pmod[h]])
            P.op('vector', lambda e, h=h, i=i: e.tensor_tensor(out=grow[:, i, h * 512:(h + 1) * 512], in0=pmod[h][:],
                                                              in1=grow[:, i, h * 512:(h + 1) * 512], op=ALU.add),
                 reads=[d_pmod[h], d_rows], writes=[d_rows])
    for i, sec in enumerate((3, 4)):
        P.op('sync', lambda e, sec=sec: e.dma_start(out=adaw[:], in_=a['ada_w'][:, sec * D:(sec + 1) * D].rearrange("(j p) n -> p j n", p=128)),
             writes=d_adaw8, dma=True)
        for jc in range(8):
            for j in range(8):
                P.op('tensor', lambda e, jc=jc, j=j: e.matmul(pmod[0][:, jc * 2:jc * 2 + 2], lhsT=adaw[:, j, jc * 128:(jc + 1) * 128],
                                                             rhs=cact2[:, j, :], start=(j == 0), stop=(j == 7)),
                     reads=[d_c] + d_adaw8, writes=[d_pmod[0]])
        P.op('vector', lambda e, i=i, sec=sec: e.tensor_tensor(out=modcol[:, i, :], in0=pmod[0][:, 0:16].rearrange("p (j r) -> p j r", r=2)[:, :, 0],
                                                              in1=adab_col[:, sec * 8:(sec + 1) * 8], op=ALU.add),
             reads=[d_pmod[0], d_rows], writes=[d_rows])
    P.op('vector', lambda e: e.tensor_scalar(out=modcol[:, 1, :], in0=modcol[:, 1, :], scalar1=1.0, scalar2=None, op0=ALU.add),
         reads=[d_rows], writes=[d_rows])

    wout = sb([128, 8, D], BF16); d_wout = P.dep()
    P.op('gpsimd', lambda e: e.dma_start(out=wout[:], in_=a['w_out'].rearrange("(j p) n -> p j n", p=128)), writes=[d_wout], dma=True)
    rw = sb([128, 8, NE], F32)
    P.op('sync', lambda e: e.dma_start(out=rw[:], in_=a['router_w'].rearrange("(j p) n -> p j n", p=128)), writes=[d_wout], dma=True)
    rb = sb([128, NE], F32)
    P.op('sync', lambda e: e.dma_start(out=rb[:], in_=a['router_b'][0:1, :].partition_broadcast(128)), writes=[d_wout], dma=True)

    h2T = sb([128, 8, PASS], BF16); d_h2T = P.deps(NTT)
    gw = sb([128, NT, NE + 1], F32); d_gw = P.deps(NT)
    wall = [sb([128, 6144], BF16) for _ in range(2)]
    wgt = [w_[:, 0:2048].rearrange("p (j n) -> p j n", n=256) for w_ in wall]
    wut = [w_[:, 2048:4096].rearrange("p (j n) -> p j n", n=256) for w_ in wall]
    wdt = [w_[:, 4096:6144].rearrange("p (j n) -> p j n", n=D) for w_ in wall]
    d_w = P.deps(2)
    mixt = [sb([128, 8, 128], BF16) for _ in range(2)]; d_mixt = P.deps(2)
    xt = [sb([128, D], F32) for _ in range(2)]; d_xt = P.deps(2)
    vt = [sb([128, D], F32)] * 2; d_vt = [P.dep()] * 2
    x1t = [sb([128, D], F32)] * 2; d_x1t = [P.dep()] * 2
    st = [(sb([128, 12], F32), sb([128, 2], F32), sb([128, 1], F32)) for _ in range(2)]; d_st = P.deps(2)
    rt = [dict(sc=sb([128, NE], F32), sel=sb([128, NE], F32), m8=sb([128, 8, 8], F32), gs=sb([128, 8], F32),
               g8=sb([128, 8], F32), gm=sb([128, 8], F32), gneg=sb([128, 8], F32), selm=sb([128, NE], F32),
               e8=sb([128, 8], F32), em=sb([128, NE], F32), den=sb([128, 1], F32))] * 2
    d_rt = [P.dep()] * 2
    sg = [sb([128, 512], BF16) for _ in range(2)]; d_sg = P.deps(2)
    ytmp = [sb([128, D], F32) for _ in range(2)]; d_ytmp = P.deps(2)
    hw = [[sb([128, 512], BF16) for _ in range(2)] for _ in range(2)]; d_hw = [P.deps(2) for _ in range(2)]
    pg = [ps([128, 512], F32) for _ in range(2)]; d_pg = P.deps(2)
    pu = [pmod[0], pmod[1]]; d_pu = d_pmod
    py = [ps([128, D], F32) for _ in range(2)]; d_py = P.deps(2)

    Ctx.k += 1
    x1s = nc.dram_tensor("x1_scratch%d" % Ctx.k, [T, D], F32, kind="Internal").ap()
    d_x1s = P.deps(T // 128)


    def load_expert(e_i, buf):
        P.op('sync', lambda e: e.dma_start(out=wall[buf][:], in_=a['wbf'][e_i]), writes=[d_w[buf]], dma=True)

    for pa in range(npass):
        tok_base = pa * PASS
        for t in range(NT):
            b = t % 2
            t0 = tok_base + t * 128
            gt = t0 // 128
            P.op('gpsimd', lambda e, b=b, t0=t0: e.dma_start(out=mixt[b][:], in_=a['mixT'][:, t0:t0 + 128].rearrange("(j p) t -> p j t", p=128)),
                 writes=[d_mixt[b]], dma=True)
            P.op('sync', lambda e, b=b, t0=t0: e.dma_start(out=xt[b][:], in_=a['x'][t0:t0 + 128, :]), writes=[d_xt[b]], dma=True)
            for h in range(2):
                for j in range(8):
                    P.op('tensor', lambda e, b=b, h=h, j=j: e.matmul(py[b][:, h * 512:(h + 1) * 512], lhsT=mixt[b][:, j, :],
                                                                    rhs=wout[:, j, h * 512:(h + 1) * 512], start=(j == 0), stop=(j == 7)),
                         reads=[d_mixt[b], d_wout], writes=[d_py[b]])
            P.op('vector', lambda e, b=b: e.tensor_tensor(out=vt[b][:], in0=py[b][:], in1=grow[:, 0, :], op=ALU.mult),
                 reads=[d_py[b], d_rows], writes=[d_vt[b]])
            P.op('vector', lambda e, b=b: e.scalar_tensor_tensor(out=vt[b][:], in0=xt[b][:], scalar=ALPHA, in1=vt[b][:],
                                                                 op0=ALU.mult, op1=ALU.add),
                 reads=[d_xt[b], d_vt[b]], writes=[d_vt[b]])
            layer_norm_tile(cx, vt[b], d_vt[b], x1t[b], d_x1t[b], lnrows[:, 0, :], lnrows[:, 1, :], d_rows, st[b], d_st[b])
            P.op('sync', lambda e, b=b, t0=t0: e.dma_start(out=x1s[t0:t0 + 128, :], in_=x1t[b][:]), reads=[d_x1t[b]],
                 writes=[d_x1s[gt]], dma=True)
            for half in range(2):
                pt = pg[half]
                for j4 in range(4):
                    j = half * 4 + j4
                    P.op('tensor', lambda e, b=b, pt=pt, j=j, j4=j4: e.transpose(pt[:, j4 * 128:(j4 + 1) * 128], x1t[b][:, j * 128:(j + 1) * 128], ident[:]),
                         reads=[d_x1t[b], d_c], writes=[d_pg[half]])
                for j4 in range(4):
                    j = half * 4 + j4
                    P.op('vector', lambda e, b=b, pt=pt, j=j, j4=j4: e.tensor_scalar(out=h2f[0][:, j, :], in0=pt[:, j4 * 128:(j4 + 1) * 128],
                                                                                    scalar1=modcol[:, 1, j:j + 1], scalar2=modcol[:, 0, j:j + 1],
                                                                                    op0=ALU.mult, op1=ALU.add),
                         reads=[d_pg[half], d_rows], writes=[d_h2f[0]])
            tt = t // 4
            P.op('gpsimd', lambda e, b=b, t=t: e.tensor_copy(out=h2T[:, :, t * 128:(t + 1) * 128], in_=h2f[0][:]),
                 reads=[d_h2f[0]], writes=[d_h2T[tt]])
            pr = pu[b]
            for j in range(8):
                P.op('tensor', lambda e, b=b, j=j, pr=pr: e.matmul(pr[:, 0:NE], lhsT=h2f[0][:, j, :], rhs=rw[:, j, :], start=(j == 0), stop=(j == 7)),
                     reads=[d_h2f[0], d_wout], writes=[d_pu[b]])
            r = rt[b]
            dr = d_rt[b]
            P.op('scalar', lambda e, r=r, pr=pr: e.activation(out=r['sc'][:], in_=pr[:, 0:NE], func=AF.Sigmoid), reads=[d_pu[b]], writes=[dr])
            P.op('vector', lambda e, r=r: e.tensor_tensor(out=r['sel'][:], in0=r['sc'][:], in1=rb[:], op=ALU.add), reads=[dr, d_wout], writes=[dr])
            for g in range(8):
                P.op('vector', lambda e, r=r, g=g: e.max(out=r['m8'][:, g, :], in_=r['sel'][:, g * 8:(g + 1) * 8]), reads=[dr], writes=[dr])
            P.op('vector', lambda e, r=r: e.tensor_tensor(out=r['gs'][:], in0=r['m8'][:, :, 0], in1=r['m8'][:, :, 1], op=ALU.add), reads=[dr], writes=[dr])
            P.op('vector', lambda e, r=r: e.max(out=r['g8'][:], in_=r['gs'][:]), reads=[dr], writes=[dr])
            P.op('vector', lambda e, r=r: e.tensor_scalar(out=r['gm'][:], in0=r['gs'][:], scalar1=r['g8'][:, 3:4], scalar2=None, op0=ALU.is_ge),
                 reads=[dr], writes=[dr])
            P.op('vector', lambda e, r=r: e.tensor_scalar(out=r['gneg'][:], in0=r['gm'][:], scalar1=-1.0, scalar2=1e9, op0=ALU.add, op1=ALU.mult),
                 reads=[dr], writes=[dr])
            P.op('vector', lambda e, r=r: e.tensor_tensor(out=r['selm'][:].rearrange("p (g k) -> p g k", k=8),
                                                          in0=r['sel'][:].rearrange("p (g k) -> p g k", k=8),
                                                          in1=r['gm'][:].unsqueeze(2).broadcast_to([128, 8, 8]), op=ALU.mult), reads=[dr], writes=[dr])
            P.op('vector', lambda e, r=r: e.tensor_tensor(out=r['selm'][:].rearrange("p (g k) -> p g k", k=8),
                                                          in0=r['selm'][:].rearrange("p (g k) -> p g k", k=8),
                                                          in1=r['gneg'][:].unsqueeze(2).broadcast_to([128, 8, 8]), op=ALU.add), reads=[dr], writes=[dr])
            P.op('vector', lambda e, r=r: e.max(out=r['e8'][:], in_=r['selm'][:]), reads=[dr], writes=[dr])
            P.op('vector', lambda e, r=r: e.tensor_scalar(out=r['em'][:], in0=r['selm'][:], scalar1=r['e8'][:, 5:6], scalar2=None, op0=ALU.is_ge),
                 reads=[dr], writes=[dr])
            P.op('vector', lambda e, r=r: e.tensor_tensor(out=r['em'][:], in0=r['em'][:], in1=r['sc'][:], op=ALU.mult), reads=[dr], writes=[dr])
            P.op('vector', lambda e, r=r: e.tensor_reduce(out=r['den'][:], in_=r['em'][:], axis=AX.X, op=ALU.add), reads=[dr], writes=[dr])
            P.op('vector', lambda e, r=r: e.tensor_scalar(out=r['den'][:], in0=r['den'][:], scalar1=1e-20, scalar2=None, op0=ALU.add), reads=[dr], writes=[dr])
            P.op('vector', lambda e, r=r: e.reciprocal(out=r['den'][:], in_=r['den'][:]), reads=[dr], writes=[dr])
            P.op('vector', lambda e, r=r, t=t: e.tensor_scalar(out=gw[:, t, 0:NE], in0=r['em'][:], scalar1=r['den'][:, 0:1], scalar2=2.5,
                                                               op0=ALU.mult, op1=ALU.mult), reads=[dr], writes=[d_gw[t]])
            P.op('vector', lambda e, t=t: e.memset(gw[:, t, NE:NE + 1], 1.0), writes=[d_gw[t]])

        def emit_gu_group(k, ei, tt, c, which):
            buf = ei % 2
            hb = k % 2
            pp, dpp, wt_ = ((pg[c], d_pg[c], wgt[buf]), (pu[c], d_pu[c], wut[buf]))[which]
            for j in range(8):
                P.op('tensor', lambda e, pp=pp, wt_=wt_, c=c, j=j, tt=tt: e.matmul(pp[:], lhsT=wt_[:, j, c * 128:(c + 1) * 128],
                                                                                   rhs=h2T[:, j, tt * 512:(tt + 1) * 512],
                                                                                   start=(j == 0), stop=(j == 7)),
                     reads=[d_w[buf], d_h2T[tt]], writes=[dpp])
            if which == 0:
                P.op('scalar', lambda e, c=c: e.activation(out=sg[c][:], in_=pg[c][:], func=AF.Silu), reads=[d_pg[c]], writes=[d_sg[c]])
            else:
                P.op('vector', lambda e, c=c, hb=hb: e.tensor_tensor(out=hw[hb][c][:], in0=pu[c][:], in1=sg[c][:], op=ALU.mult),
                     reads=[d_pu[c], d_sg[c]], writes=[d_hw[hb][c]])

        def emit_down_tile(k, ei, tt, t4):
            buf = ei % 2
            hb = k % 2
            t = tt * 4 + t4
            pb = t % 2
            for h in range(2):
                for c in range(2):
                    P.op('tensor', lambda e, pb=pb, h=h, c=c, hb=hb, t4=t4, buf=buf: e.matmul(py[pb][:, h * 512:(h + 1) * 512],
                                                                                     lhsT=hw[hb][c][:, t4 * 128:(t4 + 1) * 128],
                                                                                     rhs=wdt[buf][:, c, h * 512:(h + 1) * 512],
                                                                                     start=(c == 0), stop=(c == 1)),
                         reads=[d_hw[hb][c], d_w[buf]], writes=[d_py[pb]])
            if t4 % 2 == 1:
                if ei == 0:
                    P.op('scalar', lambda e, pb=pb, t=t, ei=ei: e.activation(out=acc[:, t, :], in_=py[pb][:], func=AF.Identity, scale=gw[:, t, ei:ei + 1]),
                         reads=[d_gw[t]], writes=[d_py[pb], d_acc[t]])
                else:
                    tb = (t4 // 2) % 2
                    P.op('scalar', lambda e, pb=pb, t=t, ei=ei, tb=tb: e.activation(out=ytmp[tb][:], in_=py[pb][:], func=AF.Identity, scale=gw[:, t, ei:ei + 1]),
                         reads=[d_gw[t]], writes=[d_py[pb], d_ytmp[tb]])
                    P.op('gpsimd', lambda e, t=t, tb=tb: e.tensor_tensor(out=acc[:, t, :], in0=acc[:, t, :], in1=ytmp[tb][:], op=ALU.add),
                         reads=[d_ytmp[tb]], writes=[d_acc[t]])
            elif ei == 0:
                P.op('vector', lambda e, pb=pb, t=t, ei=ei: e.tensor_scalar(out=acc[:, t, :], in0=py[pb][:], scalar1=gw[:, t, ei:ei + 1],
                                                                             scalar2=None, op0=ALU.mult),
                     reads=[d_gw[t]], writes=[d_py[pb], d_acc[t]])
            else:
                P.op('vector', lambda e, pb=pb, t=t, ei=ei: e.scalar_tensor_tensor(out=acc[:, t, :], in0=py[pb][:], scalar=gw[:, t, ei:ei + 1],
                                                                                    in1=acc[:, t, :], op0=ALU.mult, op1=ALU.add),
                     reads=[d_gw[t]], writes=[d_py[pb], d_acc[t]])

        items = [(ei, tt) for ei in range(NE + 1) for tt in range(NTT)]
        groups = [(0, 0), (0, 1), (1, 0), (1, 1)]
        load_expert(0, 0)
        load_expert(1, 1)
        for (c, which) in groups:
            emit_gu_group(0, items[0][0], items[0][1], c, which)
        for k, (ei, tt) in enumerate(items):
            nxt = items[k + 1] if k + 1 < len(items) else None
            for t4 in range(4):
                if nxt is not None:
                    emit_gu_group(k + 1, nxt[0], nxt[1], groups[t4][0], groups[t4][1])
                emit_down_tile(k, ei, tt, t4)
            if tt == NTT - 1 and ei + 2 <= NE:
                load_expert(ei + 2, ei % 2)

        for t in range(NT):
            b = t % 2
            t0 = tok_base + t * 128
            gt = t0 // 128
            P.op('sync', lambda e, b=b, t0=t0: e.dma_start(out=xt[b][:], in_=x1s[t0:t0 + 128, :]), reads=[d_x1s[gt]], writes=[d_xt[b]], dma=True)
            P.op('gpsimd', lambda e, b=b, t=t: e.tensor_tensor(out=vt[b][:], in0=acc[:, t, :], in1=grow[:, 1, :], op=ALU.mult),
                 reads=[d_acc[t], d_rows], writes=[d_vt[b]])
            P.op('vector', lambda e, b=b: e.scalar_tensor_tensor(out=vt[b][:], in0=xt[b][:], scalar=ALPHA, in1=vt[b][:],
                                                                 op0=ALU.mult, op1=ALU.add),
                 reads=[d_xt[b], d_vt[b]], writes=[d_vt[b]])
            layer_norm_tile(cx, vt[b], d_vt[b], x1t[b], d_x1t[b], lnrows[:, 2, :], lnrows[:, 3, :], d_rows, st[b], d_st[b])
            P.op('sync', lambda e, b=b, t0=t0: e.dma_start(out=a['out'][t0:t0 + 128, :], in_=x1t[b][:]), reads=[d_x1t[b]], dma=True, is_out=True)
    P.emit()


MB = 256
RW = 1920
NEAR = 1664
NEG = -30000.0


def rpe_bucket_np(dist):
    exact = 16
    d = np.maximum(dist, 0)
    logd = np.log(np.maximum(d, 1).astype(np.float32) / np.float32(exact))
    large = exact + (logd / np.float32(math.log(2048 / exact)) * np.float32(32 - exact)).astype(np.int32)
    large = np.minimum(large, 31)
    return np.where(d < exact, d, large)


def moba_host_tables(rpe_bias, heads):
    p = np.arange(128)[:, None]
    m = np.arange(RW)[None, :]
    dist = m - p - 128
    bk = rpe_bucket_np(dist)
    out = np.empty((len(heads), 128, RW), np.float32)
    for i, h in enumerate(heads):
        out[i] = np.where(dist >= 0, rpe_bias[bk, h], np.float32(NEG))
    return out


def emit_stage_moba(nc, es, S, a, bg=None):
    cx = Ctx(nc, es)
    P = cx.P
    sb, ps = cx.sb, cx.ps
    NB = S // MB
    emit_consts(cx, a)
    ones_bf = sb([128, 128], BF16)
    P.op('vector', lambda e: e.memset(ones_bf[:], 1.0), writes=[cx.d_c])
    eoh = sb([128, 32 * 128], BF16)
    eohf = sb([32, 32 * 128], F32)
    P.op('sync', lambda e: e.dma_start(out=eohf[:], in_=a['eoh']), writes=[cx.d_c], dma=True)
    P.op('vector', lambda e: e.memset(eoh[:], 0.0), writes=[cx.d_c])
    P.op('vector', lambda e: e.tensor_copy(out=eoh[0:32, :], in_=eohf[:]), reads=[cx.d_c], writes=[cx.d_c])
    R = sb([128, 2, RW], F32)
    for h in range(2):
        P.op('sync', lambda e, h=h: e.dma_start(out=R[:, h, :], in_=a['rtab'][h]), writes=[cx.d_c], dma=True)
    Rb = sb([128, 2, RW], BF16)
    P.op('vector', lambda e: e.tensor_copy(out=Rb[:], in_=R[:]), reads=[cx.d_c], writes=[cx.d_c])
    identb = sb([128, 128], BF16)
    P.op('vector', lambda e: e.tensor_copy(out=identb[:], in_=cx.ident[:]), reads=[cx.d_c], writes=[cx.d_c])

    NL = 4
    pl = [ps([128, 512], F32) for _ in range(NL)]; d_pl = P.deps(NL)
    po = ps([128, 512], F32); d_po = P.dep()
    pd = ps([128, 512], F32); d_pd = P.dep()
    pa = [ps([128, 512], F32) for _ in range(2)]; d_pa = P.deps(2)
    pgt = pa[0]; d_pgt = d_pa[0]

    adaw = sb([128, 8, D], F32); d_adaw = P.dep()
    d_mod = P.dep()
    modcol = emit_modcols(cx, a, (0, 1), adaw, d_adaw, pgt, d_pgt, d_mod)
    P.op('vector', lambda e: e.tensor_scalar(out=modcol[:, 1, :], in0=modcol[:, 1, :], scalar1=1.0, scalar2=None, op0=ALU.add),
         reads=[d_mod], writes=[d_mod])

    wq = sb([128, 8, 256], BF16); wk = sb([128, 8, 256], BF16); wv = sb([128, 8, 256], BF16); d_w = P.dep()
    for wt_, nm in ((wq, 'w_q'), (wk, 'w_k'), (wv, 'w_v')):
        P.op('gpsimd', lambda e, wt_=wt_, nm=nm: e.dma_start(out=wt_[:], in_=a[nm].rearrange("(j p) n -> p j n", p=128)), writes=[d_w], dma=True)

    kT = sb([128, 2, S], BF16); d_kT = P.deps(NB)
    V = sb([128, S // 128, 256], BF16); d_V = P.deps(NB)
    kmean = sb([128, 2, 32], F32); d_km = P.dep()
    P.op('vector', lambda e: e.memset(kmean[:], 0.0), writes=[d_km])
    G = [sb([128, 32], F32) for _ in range(2)]; d_G = P.deps(2)
    for g_ in range(2):
        P.op('vector', lambda e, g_=g_: e.memset(G[g_][:], -1e30), writes=[d_G[g_]])
    M8 = [sb([128, 8], F32) for _ in range(2)]
    mb = [sb([128, 32], F32) for _ in range(2)]
    mbT = [sb([128, 256], BF16) for _ in range(2)]; d_mbT = P.deps(2)
    for h_ in range(2):
        P.op('vector', lambda e, h_=h_: e.memset(mbT[h_][:], 0.0), writes=[d_mbT[h_]])

    xt = [sb([128, D], F32) for _ in range(2)]; d_xt = P.deps(2)
    hT = [sb([128, 8, MB], BF16) for _ in range(2)]; d_hT = P.deps(2)
    qTb = [[sb([128, MB], BF16) for _ in range(2)] for _ in range(2)]; d_qTb = [P.deps(2) for _ in range(2)]
    qTf = [[sb([128, MB], F32) for _ in range(2)] for _ in range(2)]; d_qTf = [P.deps(2) for _ in range(2)]
    Pm = [sb([128, MB], BF16) for _ in range(NL)]; d_Pm = P.deps(NL)
    rden = [sb([128, MB], F32) for _ in range(2)]; d_rden = P.deps(2)
    ot = [sb([128, MB], F32) for _ in range(2)]; d_ot = P.deps(2)

    scale = 128 ** -0.5
    cnt = 0
    for n in range(NB):
        emit_bg(P, bg, n, NB)
        q0 = n * MB
        hb = n % 2
        emit_hT_block(cx, a['x'], q0, 2, xt, d_xt, pa, d_pa, modcol, d_mod, hT[hb], d_hT[hb])
        for h in range(2):
            pq = pa[0]
            for j in range(8):
                P.op('tensor', lambda e, h=h, j=j, hb=hb, pq=pq: e.matmul(pq[:, 0:MB], lhsT=wq[:, j, h * 128:(h + 1) * 128], rhs=hT[hb][:, j, :],
                                                                       start=(j == 0), stop=(j == 7)), reads=[d_w, d_hT[hb]], writes=[d_pa[0]])
            P.op('scalar', lambda e, h=h, hb=hb, pq=pq: e.activation(out=qTb[hb][h][:], in_=pq[:, 0:MB], func=AF.Identity, scale=scale),
                 writes=[d_pa[0], d_qTb[hb][h]])
            P.op('vector', lambda e, h=h, hb=hb, pq=pq: e.tensor_scalar(out=qTf[hb][h][:], in0=pq[:, 0:MB], scalar1=scale, scalar2=None, op0=ALU.mult),
                 writes=[d_pa[0], d_qTf[hb][h]])
            pk = pa[1]
            for j in range(8):
                P.op('tensor', lambda e, h=h, j=j, hb=hb, pk=pk: e.matmul(pk[:, 0:MB], lhsT=wk[:, j, h * 128:(h + 1) * 128], rhs=hT[hb][:, j, :],
                                                                       start=(j == 0), stop=(j == 7)), reads=[d_w, d_hT[hb]], writes=[d_pa[1]])
            P.op('scalar', lambda e, h=h, pk=pk, q0=q0: e.activation(out=kT[:, h, q0:q0 + MB], in_=pk[:, 0:MB], func=AF.Identity),
                 writes=[d_pa[1], d_kT[n]])
            P.op('vector', lambda e, h=h, pk=pk, n=n: e.tensor_reduce(out=kmean[:, h, n:n + 1], in_=pk[:, 0:MB], axis=AX.X, op=ALU.add),
                 writes=[d_pa[1], d_km])
        P.op('vector', lambda e, n=n: e.tensor_scalar(out=kmean[:, :, n:n + 1], in0=kmean[:, :, n:n + 1], scalar1=1.0 / MB, scalar2=None, op0=ALU.mult),
             reads=[d_km], writes=[d_km])
        for i in range(2):
            pv = pa[i]
            for j in range(8):
                P.op('tensor', lambda e, i=i, j=j, hb=hb, pv=pv: e.matmul(pv[:, 0:256], lhsT=hT[hb][:, j, i * 128:(i + 1) * 128], rhs=wv[:, j, :],
                                                                       start=(j == 0), stop=(j == 7)), reads=[d_w, d_hT[hb]], writes=[d_pa[i]])
            P.op('scalar', lambda e, i=i, pv=pv, n=n: e.activation(out=V[:, 2 * n + i, :], in_=pv[:, 0:256], func=AF.Identity),
                 reads=[d_pa[i]], writes=[d_V[n]])
        if n >= 1:
            for h in range(2):
                for i in range(2):
                    gb = i
                    P.op('tensor', lambda e, h=h, i=i, hb=hb: e.matmul(pgt[:, i * 32:i * 32 + 32], lhsT=qTf[hb][h][:, i * 128:(i + 1) * 128], rhs=kmean[:, h, :],
                                                                    start=True, stop=True), reads=[d_qTf[hb][h], d_km], writes=[d_pgt])
                    P.op('vector', lambda e, i=i, gb=gb, n=n: e.tensor_copy(out=G[gb][:, 0:n], in_=pgt[:, i * 32:i * 32 + n]), reads=[d_pgt], writes=[d_G[gb]])
                    P.op('vector', lambda e, gb=gb: e.max(out=M8[gb][:], in_=G[gb][:]), reads=[d_G[gb]], writes=[d_G[gb]])
                    P.op('vector', lambda e, gb=gb: e.tensor_scalar(out=mb[gb][:], in0=G[gb][:], scalar1=M8[gb][:, 2:3], scalar2=NEG,
                                                                    op0=ALU.is_lt, op1=ALU.mult), reads=[d_G[gb]], writes=[d_G[gb]])
                    P.op('tensor', lambda e, i=i, gb=gb: e.transpose(pgt[0:32, 256 + i * 128:256 + (i + 1) * 128], mb[gb][:], cx.ident[:]),
                         reads=[d_G[gb], cx.d_c], writes=[d_pgt])
                P.op('vector', lambda e, h=h: e.tensor_copy(out=mbT[h][0:32, :], in_=pgt[0:32, 256:512]), reads=[d_pgt], writes=[d_mbT[h]])
        tiles = [(h, t) for h in range(2) for t in range(2 * n + 2)]
        nt = 2 * n + 2

        def emit_qk(idx):
            h, t = tiles[idx]
            lb = (cnt + idx) % NL
            past = t < 2 * n
            delta = q0 - t * 128
            near = delta < NEAR
            P.op('tensor', lambda e, h=h, t=t, lb=lb, hb=hb, past=past, near=near: e.matmul(pl[lb][:, 0:MB], lhsT=kT[:, h, t * 128:(t + 1) * 128], rhs=qTb[hb][h][:],
                                                                                         start=True, stop=(not past and not near)),
                 reads=[d_kT[t // 2], d_qTb[hb][h]], writes=[d_pl[lb]])
            if past:
                P.op('tensor', lambda e, h=h, t=t, lb=lb, near=near: e.matmul(pl[lb][:, 0:MB], lhsT=eoh[:, (t // 2) * 128:(t // 2 + 1) * 128], rhs=mbT[h][:],
                                                                           start=False, stop=(not near)),
                     reads=[cx.d_c, d_mbT[h]], writes=[d_pl[lb]])
            if near:
                s0 = delta + 128
                P.op('tensor', lambda e, h=h, lb=lb, s0=s0: e.matmul(pl[lb][:, 0:MB], lhsT=identb[:], rhs=Rb[:, h, s0:s0 + MB], start=False, stop=True),
                     reads=[cx.d_c], writes=[d_pl[lb]])
                P.op('scalar', lambda e, lb=lb: e.activation(out=Pm[lb][:], in_=pl[lb][:, 0:MB], func=AF.Exp), writes=[d_pl[lb], d_Pm[lb]])
            else:
                P.op('scalar', lambda e, h=h, lb=lb: e.activation(out=Pm[lb][:], in_=pl[lb][:, 0:MB], func=AF.Exp, bias=R[:, h, RW - 1:RW], scale=1.0),
                     reads=[cx.d_c], writes=[d_pl[lb], d_Pm[lb]])

        def emit_pv(idx):
            h, t = tiles[idx]
            lb = (cnt + idx) % NL
            P.op('tensor', lambda e, h=h, t=t, lb=lb, nt=nt: e.matmul(po[:, 0:MB], lhsT=V[:, t, h * 128:(h + 1) * 128], rhs=Pm[lb][:],
                                                            start=(t == 0), stop=(t == nt - 1)),
                 reads=[d_V[t // 2], d_Pm[lb]], writes=[d_po])
            P.op('tensor', lambda e, t=t, lb=lb, nt=nt: e.matmul(pd[:, 0:MB], lhsT=ones_bf[:], rhs=Pm[lb][:], start=(t == 0), stop=(t == nt - 1)),
                 reads=[cx.d_c, d_Pm[lb]], writes=[d_pd])
            if t == nt - 1:
                ob = h
                P.op('vector', lambda e, ob=ob: e.reciprocal(out=rden[ob][:], in_=pd[:, 0:MB]), writes=[d_pd, d_rden[ob]])
                P.op('vector', lambda e, ob=ob: e.tensor_tensor(out=ot[ob][:], in0=po[:, 0:MB], in1=rden[ob][:], op=ALU.mult),
                     reads=[d_rden[ob]], writes=[d_po, d_ot[ob]])
                P.op('sync', lambda e, ob=ob, h=h, q0=q0: e.dma_start(out=a['oT'][h * 128:(h + 1) * 128, q0:q0 + MB], in_=ot[ob][:]),
                     reads=[d_ot[ob]], dma=True, is_out=True)

        LOOK = NL - 1
        for idx in range(min(LOOK, len(tiles))):
            emit_qk(idx)
        for idx in range(len(tiles)):
            emit_pv(idx)
            if idx + LOOK < len(tiles):
                emit_qk(idx + LOOK)
        cnt += len(tiles)
    P.emit()


GB = 256
NORM_EPS = 1e-6


def gla_host_consts():
    t = np.arange(GB)
    reset = np.broadcast_to((t % 64 != 0).astype(np.float32)[None, :], (128, GB)).copy()
    j = np.arange(128)[:, None]
    i = np.arange(128)[None, :]
    maskT = ((j // 64 == i // 64) & (j <= i)).astype(np.float32)
    return reset, maskT


def emit_stage_gla(nc, es, S, a, bg=None):
    cx = Ctx(nc, es)
    P = cx.P
    sb, ps = cx.sb, cx.ps
    NB = S // GB
    emit_consts(cx, a)
    reset = sb([128, GB], F32); maskT = sb([128, 128], F32)
    P.op('sync', lambda e: e.dma_start(out=reset[:], in_=a['reset']), writes=[cx.d_c], dma=True)
    P.op('sync', lambda e: e.dma_start(out=maskT[:], in_=a['maskT']), writes=[cx.d_c], dma=True)
    negb = sb([128, 1], F32); wcol = sb([128, 1], F32); neps = sb([128, 1], F32)
    P.op('vector', lambda e: e.memset(neps[:], NORM_EPS), writes=[cx.d_c])
    P.op('sync', lambda e: e.dma_start(out=negb[:], in_=a['gk_b_col']), writes=[cx.d_c], dma=True)
    P.op('vector', lambda e: e.tensor_scalar(out=negb[:], in0=negb[:], scalar1=-1.0, scalar2=None, op0=ALU.mult), reads=[cx.d_c], writes=[cx.d_c])
    P.op('sync', lambda e: e.dma_start(out=wcol[:], in_=a['norm_col']), writes=[cx.d_c], dma=True)

    pa = [ps([128, 512], F32) for _ in range(2)]; d_pa = P.deps(2)
    pA = ps([128, 512], F32); d_pA = P.dep()
    pO = ps([128, 512], F32); d_pO = P.dep()
    pS = ps([128, 512], F32); d_pS = P.dep()
    pss = ps([128, 512], F32); d_pss = P.dep()
    pz = ps([128, 512], F32); d_pz = P.dep()
    ptr = ps([128, 512], F32); d_ptr = P.dep()

    adaw = sb([128, 8, D], F32); d_adaw = P.dep()
    d_mod = P.dep()
    modcol = emit_modcols(cx, a, (0, 1), adaw, d_adaw, pz, d_pz, d_mod)
    P.op('vector', lambda e: e.tensor_scalar(out=modcol[:, 1, :], in0=modcol[:, 1, :], scalar1=1.0, scalar2=None, op0=ALU.add),
         reads=[d_mod], writes=[d_mod])

    wq = sb([128, 8, 128], BF16); wk = sb([128, 8, 128], BF16); wv = sb([128, 8, 256], BF16); wg = sb([128, 8, 256], BF16)
    wlr = sb([128, 8, 16], BF16); d_w = P.dep()
    for wt_, nm in ((wq, 'w_gq'), (wk, 'w_gk'), (wv, 'w_gv'), (wg, 'w_gg'), (wlr, 'w_glr')):
        P.op('gpsimd', lambda e, wt_=wt_, nm=nm: e.dma_start(out=wt_[:], in_=a[nm].rearrange("(j p) n -> p j n", p=128)), writes=[d_w], dma=True)
    w2f = sb([16, 128], F32); w2 = sb([16, 128], BF16)
    P.op('sync', lambda e: e.dma_start(out=w2f[:], in_=a['gk_w2']), writes=[d_w], dma=True)
    P.op('vector', lambda e: e.tensor_copy(out=w2[:], in_=w2f[:]), reads=[d_w], writes=[d_w])

    Sf = sb([128, 128], F32); d_Sf = P.dep()
    P.op('vector', lambda e: e.memset(Sf[:], 0.0), writes=[d_Sf])
    Sb = [sb([128, 128], BF16) for _ in range(3)]; d_Sb = P.deps(3)
    P.op('vector', lambda e: e.memset(Sb[0][:], 0.0), writes=[d_Sb[0]])

    xt = [sb([128, D], F32) for _ in range(2)]; d_xt = P.deps(2)
    hT = [sb([128, 8, GB], BF16) for _ in range(2)]; d_hT = P.deps(2)
    glr = sb([16, GB], BF16); d_glr = P.dep()
    e1 = sb([128, GB], F32); g_ = sb([128, GB], F32); bcs = sb([128, GB], F32); d2 = sb([128, GB], F32); d_gt = P.dep()
    eb = [sb([128, GB], F32) for _ in range(2)]; d_eb = P.deps(2)
    enb = sb([128, GB], F32); ekend = sb([128, GB], F32); d_en = P.dep()
    qe = [sb([128, GB], BF16) for _ in range(2)]; ke = [sb([128, GB], BF16) for _ in range(2)]; d_qk = P.deps(2)
    kendT = sb([128, GB], F32); d_kendT = P.dep()
    kend = [sb([128, 2, 128], BF16) for _ in range(2)]; d_kend = P.deps(2)
    Vt = [sb([128, 2, 256], BF16) for _ in range(2)]; d_Vt = P.deps(2)
    sgate = [sb([128, 2, GB], F32) for _ in range(2)]; d_sg = P.deps(2)
    ATm = [sb([128, 128], BF16) for _ in range(2)]; d_AT = P.deps(2)
    osb = [sb([128, 128], F32) for _ in range(2)]; osq = [sb([128, 128], F32) for _ in range(2)]; d_o = P.deps(2)
    rstd = [sb([128, 128], F32) for _ in range(2)]; d_rs = P.deps(2)
    yt = [sb([128, 128], F32) for _ in range(2)]; d_yt = P.deps(2)

    sbi = 0
    cnt = 0
    for n in range(NB):
        emit_bg(P, bg, n, NB)
        t0 = n * GB
        hb = n % 2
        emit_hT_block(cx, a['x'], t0, 2, xt, d_xt, pa, d_pa, modcol, d_mod, hT[hb], d_hT[hb])
        for j in range(8):
            P.op('tensor', lambda e, j=j, hb=hb: e.matmul(pz[0:16, 0:GB], lhsT=wlr[:, j, :], rhs=hT[hb][:, j, :], start=(j == 0), stop=(j == 7)),
                 reads=[d_w, d_hT[hb]], writes=[d_pz])
        P.op('vector', lambda e: e.tensor_copy(out=glr[:], in_=pz[0:16, 0:GB]), writes=[d_pz, d_glr])
        P.op('tensor', lambda e: e.matmul(pz[:, GB:2 * GB], lhsT=w2[:], rhs=glr[:], start=True, stop=True), reads=[d_w, d_glr], writes=[d_pz])
        P.op('scalar', lambda e: e.activation(out=e1[:], in_=pz[:, GB:2 * GB], func=AF.Exp, bias=negb[:], scale=-1.0), reads=[cx.d_c], writes=[d_pz, d_gt])
        P.op('scalar', lambda e: e.activation(out=e1[:], in_=e1[:], func=AF.Ln, bias=cx.ones[:, 0:1], scale=1.0), reads=[cx.d_c], writes=[d_gt])
        P.op('vector', lambda e: e.tensor_scalar(out=g_[:], in0=e1[:], scalar1=-1.0 / 16.0, scalar2=None, op0=ALU.mult), reads=[d_gt], writes=[d_gt])
        P.op('vector', lambda e: e.tensor_tensor_scan(out=bcs[:], data0=reset[:], data1=g_[:], initial=0.0, op0=ALU.mult, op1=ALU.add),
             reads=[d_gt, cx.d_c], writes=[d_gt])
        P.op('vector', lambda e: e.tensor_tensor(out=d2[:].rearrange("p (c t) -> p c t", t=64),
                                                 in0=bcs[:].rearrange("p (c t) -> p c t", t=64)[:, :, 63:64].broadcast_to([128, 4, 64]),
                                                 in1=bcs[:].rearrange("p (c t) -> p c t", t=64), op=ALU.subtract), reads=[d_gt], writes=[d_gt])
        P.op('scalar', lambda e, hb=hb: e.activation(out=eb[hb][:], in_=bcs[:], func=AF.Exp), reads=[d_gt], writes=[d_eb[hb]])
        P.op('scalar', lambda e: e.activation(out=enb[:], in_=bcs[:], func=AF.Exp, scale=-1.0), reads=[d_gt], writes=[d_en])
        P.op('scalar', lambda e: e.activation(out=ekend[:], in_=d2[:], func=AF.Exp), reads=[d_gt], writes=[d_en])
        for j in range(8):
            P.op('tensor', lambda e, j=j, hb=hb: e.matmul(pa[0][:, 0:GB], lhsT=wq[:, j, :], rhs=hT[hb][:, j, :], start=(j == 0), stop=(j == 7)),
                 reads=[d_w, d_hT[hb]], writes=[d_pa[0]])
        P.op('vector', lambda e, hb=hb: e.scalar_tensor_tensor(out=qe[hb][:], in0=pa[0][:, 0:GB], scalar=0.125, in1=eb[hb][:], op0=ALU.mult, op1=ALU.mult),
             reads=[d_eb[hb]], writes=[d_pa[0], d_qk[hb]])
        for j in range(8):
            P.op('tensor', lambda e, j=j, hb=hb: e.matmul(pa[1][:, 0:GB], lhsT=wk[:, j, :], rhs=hT[hb][:, j, :], start=(j == 0), stop=(j == 7)),
                 reads=[d_w, d_hT[hb]], writes=[d_pa[1]])
        P.op('vector', lambda e, hb=hb: e.tensor_tensor(out=ke[hb][:], in0=pa[1][:, 0:GB], in1=enb[:], op=ALU.mult), reads=[d_en], writes=[d_pa[1], d_qk[hb]])
        P.op('vector', lambda e: e.tensor_tensor(out=kendT[:], in0=pa[1][:, 0:GB], in1=ekend[:], op=ALU.mult), reads=[d_en], writes=[d_pa[1], d_kendT])
        for i in range(2):
            P.op('tensor', lambda e, i=i: e.transpose(ptr[:, i * 128:(i + 1) * 128], kendT[:, i * 128:(i + 1) * 128], cx.ident[:]),
                 reads=[d_kendT, cx.d_c], writes=[d_ptr])
        P.op('scalar', lambda e, hb=hb: e.activation(out=kend[hb][:].rearrange("p i d -> p (i d)"), in_=ptr[:, 0:256], func=AF.Identity), writes=[d_ptr, d_kend[hb]])
        for h in range(2):
            for j in range(8):
                P.op('tensor', lambda e, j=j, hb=hb, h=h: e.matmul(pa[h][:, 0:GB], lhsT=wg[:, j, h * 128:(h + 1) * 128], rhs=hT[hb][:, j, :], start=(j == 0), stop=(j == 7)),
                     reads=[d_w, d_hT[hb]], writes=[d_pa[h]])
            P.op('scalar', lambda e, hb=hb, h=h: e.activation(out=sgate[hb][:, h, :], in_=pa[h][:, 0:GB], func=AF.Silu), writes=[d_pa[h], d_sg[hb]])
        for i in range(2):
            for j in range(8):
                P.op('tensor', lambda e, i=i, j=j, hb=hb: e.matmul(pa[i][:, 0:256], lhsT=hT[hb][:, j, i * 128:(i + 1) * 128], rhs=wv[:, j, :], start=(j == 0), stop=(j == 7)),
                     reads=[d_w, d_hT[hb]], writes=[d_pa[i]])
            P.op('vector', lambda e, i=i, hb=hb: e.tensor_copy(out=Vt[hb][:, i, :], in_=pa[i][:, 0:256]), writes=[d_pa[i], d_Vt[hb]])
        for i in range(2):
            c0 = i * 128
            for h in range(2):
                hs = slice(h * 64, (h + 1) * 64)
                P.op('tensor', lambda e, hs=hs, hb=hb, c0=c0: e.matmul(pA[:, 0:128], lhsT=ke[hb][hs, c0:c0 + 128], rhs=qe[hb][hs, c0:c0 + 128], start=True, stop=True),
                     reads=[d_qk[hb]], writes=[d_pA])
                P.op('vector', lambda e, h=h: e.tensor_tensor(out=ATm[h][:], in0=pA[:, 0:128], in1=maskT[:], op=ALU.mult), reads=[cx.d_c], writes=[d_pA, d_AT[h]])
            sidx = [sbi, (sbi + 1) % 3, (sbi + 2) % 3]
            for c2 in range(2):
                cs = c0 + c2 * 64
                rows = slice(c2 * 64, (c2 + 1) * 64)
                nxt = sidx[c2 + 1]
                for h in range(2):
                    hs = slice(h * 64, (h + 1) * 64)
                    P.op('tensor', lambda e, h=h, hs=hs, hb=hb, i=i, rows=rows: e.matmul(pS[hs, 0:128], lhsT=kend[hb][rows, i, hs], rhs=Vt[hb][rows, i, h * 128:(h + 1) * 128],
                                                                                      start=True, stop=True), reads=[d_kend[hb], d_Vt[hb]], writes=[d_pS])
                P.op('vector', lambda e, hb=hb, cs=cs: e.scalar_tensor_tensor(out=Sf[:], in0=Sf[:], scalar=eb[hb][:, cs + 63:cs + 64], in1=pS[:, 0:128],
                                                                              op0=ALU.mult, op1=ALU.add), reads=[d_eb[hb]], writes=[d_pS, d_Sf])
                P.op('gpsimd', lambda e, nxt=nxt: e.tensor_copy(out=Sb[nxt][:], in_=Sf[:]), reads=[d_Sf], writes=[d_Sb[nxt]])
            sbi = sidx[2]
            for h in range(2):
                hs = slice(h * 64, (h + 1) * 64)
                P.op('tensor', lambda e, h=h, hb=hb, i=i: e.matmul(pO[:, 0:128], lhsT=Vt[hb][:, i, h * 128:(h + 1) * 128], rhs=ATm[h][:],
                                                                 start=True, stop=False), reads=[d_Vt[hb], d_AT[h]], writes=[d_pO])
                for c2 in range(2):
                    cs = c0 + c2 * 64
                    cur = sidx[c2]
                    P.op('tensor', lambda e, hs=hs, hb=hb, cs=cs, c2=c2, cur=cur: e.matmul(pO[:, c2 * 64:(c2 + 1) * 64], lhsT=Sb[cur][hs, :],
                                                                                         rhs=qe[hb][hs, cs:cs + 64], start=False, stop=(c2 == 1)),
                         reads=[d_Sb[cur], d_qk[hb]], writes=[d_pO])
                ob = h
                P.op('scalar', lambda e, ob=ob: e.activation(out=osb[ob][:], in_=pO[:, 0:128], func=AF.Identity), writes=[d_pO, d_o[ob]])
                P.op('scalar', lambda e, ob=ob: e.activation(out=osq[ob][:], in_=osb[ob][:], func=AF.Square), reads=[d_o[ob]], writes=[d_o[ob]])
                P.op('tensor', lambda e, ob=ob: e.matmul(pss[:, 0:128], lhsT=cx.ones[:], rhs=osq[ob][:], start=True, stop=True), reads=[cx.d_c, d_o[ob]], writes=[d_pss])
                P.op('scalar', lambda e, ob=ob: e.activation(out=rstd[ob][:], in_=pss[:, 0:128], func=AF.Ln, bias=neps[:], scale=1.0 / 128.0),
                     reads=[cx.d_c], writes=[d_pss, d_rs[ob]])
                P.op('scalar', lambda e, ob=ob: e.activation(out=rstd[ob][:], in_=rstd[ob][:], func=AF.Exp, scale=-0.5), writes=[d_rs[ob]])
                P.op('vector', lambda e, ob=ob: e.scalar_tensor_tensor(out=yt[ob][:], in0=osb[ob][:], scalar=wcol[:, 0:1], in1=rstd[ob][:], op0=ALU.mult, op1=ALU.mult),
                     reads=[d_o[ob], d_rs[ob], cx.d_c], writes=[d_yt[ob]])
                P.op('vector', lambda e, ob=ob, hb=hb, h=h, c0=c0: e.tensor_tensor(out=yt[ob][:], in0=yt[ob][:], in1=sgate[hb][:, h, c0:c0 + 128], op=ALU.mult),
                     reads=[d_sg[hb]], writes=[d_yt[ob]])
                P.op('sync', lambda e, ob=ob, h=h, t0=t0, c0=c0: e.dma_start(out=a['oT'][h * 128:(h + 1) * 128, t0 + c0:t0 + c0 + 128], in_=yt[ob][:]),
                     reads=[d_yt[ob]], dma=True, is_out=True)
    P.emit()


DB = 256
NORM_EPS = 1e-6
NEGM = -30000.0


def gdn_host_consts():
    t = np.arange(DB)
    reset = np.broadcast_to((t % 64 != 0).astype(np.float32)[None, :], (4, DB)).copy()
    i = np.arange(128)[:, None]
    j = np.arange(128)[None, :]
    same = (i // 64 == j // 64)
    m_incl = np.where(same & (j <= i), 0.0, NEGM).astype(np.float32)
    m_inclT = np.where(same & (i <= j), 0.0, NEGM).astype(np.float32)
    s01 = (same & (j < i)).astype(np.float32)
    s01T = (same & (i < j)).astype(np.float32)
    masks = np.stack([m_incl, m_inclT, s01, s01T]).astype(np.float32)
    sel = np.zeros((4, 4, 128), np.float32)
    for h in range(4):
        sel[h, h, :] = 1.0
    return reset, masks, sel


def emit_stage_gdn(nc, es, S, a, bg=None):
    bgq = bg
    cx = Ctx(nc, es)
    P = cx.P
    sb, ps = cx.sb, cx.ps
    NB = S // DB
    emit_consts(cx, a)
    d_c = cx.d_c
    reset = sb([4, DB], F32); masks = sb([128, 4, 128], F32); sel = sb([128, 4, 128], F32)
    P.op('vector', lambda e: e.memset(sel[:], 0.0), writes=[d_c])
    P.op('sync', lambda e: e.dma_start(out=reset[:], in_=a['reset']), writes=[d_c], dma=True)
    P.op('sync', lambda e: e.dma_start(out=masks[:], in_=a['masks'].rearrange("m p f -> p m f")), writes=[d_c], dma=True)
    P.op('sync', lambda e: e.dma_start(out=sel[0:4], in_=a['sel']), writes=[d_c], dma=True)
    identb = sb([128, 128], BF16)
    P.op('vector', lambda e: e.tensor_copy(out=identb[:], in_=cx.ident[:]), reads=[d_c], writes=[d_c])
    neps = sb([128, 1], F32); wcol = sb([128, 1], F32)
    P.op('vector', lambda e: e.memset(neps[:], NORM_EPS), writes=[d_c])
    P.op('sync', lambda e: e.dma_start(out=wcol[:], in_=a['norm_col']), writes=[d_c], dma=True)
    dtb = sb([4, 1], F32); negA = sb([4, 1], F32)
    P.op('sync', lambda e: e.dma_start(out=dtb[:], in_=a['dt_bias_col']), writes=[d_c], dma=True)
    P.op('sync', lambda e: e.dma_start(out=negA[:], in_=a['a_log_col']), writes=[d_c], dma=True)
    P.op('scalar', lambda e: e.activation(out=negA[:], in_=negA[:], func=AF.Exp), reads=[d_c], writes=[d_c])
    P.op('vector', lambda e: e.tensor_scalar(out=negA[:], in0=negA[:], scalar1=-1.0, scalar2=None, op0=ALU.mult), reads=[d_c], writes=[d_c])
    convw = sb([128, 12, 4], F32)
    P.op('sync', lambda e: e.dma_start(out=convw[:], in_=a['convw_col']), writes=[d_c], dma=True)

    pa = [ps([128, 512], F32) for _ in range(2)]; d_pa = P.deps(2)
    b1 = ps([128, 512], F32); b2 = ps([128, 512], F32); b3 = ps([128, 512], F32)
    d_b1, d_b2, d_b3 = P.deps(3)
    bg = ps([128, 512], F32); d_bg = P.dep()
    pO = ps([128, 512], F32); d_pO = P.dep()
    pV = ps([128, 512], F32); d_pV = P.dep()

    adaw = sb([128, 8, D], F32); d_adaw = P.dep()
    d_mod = P.dep()
    modcol = emit_modcols(cx, a, (0, 1), adaw, d_adaw, bg, d_bg, d_mod)
    P.op('vector', lambda e: e.tensor_scalar(out=modcol[:, 1, :], in0=modcol[:, 1, :], scalar1=1.0, scalar2=None, op0=ALU.add),
         reads=[d_mod], writes=[d_mod])

    wqkv = sb([128, 8, 1536], BF16); wgt = sb([128, 8, 512], BF16); wba = sb([128, 8, 2, 128], BF16); d_w = P.dep()
    P.op('vector', lambda e: e.memset(wba[:], 0.0), writes=[d_w])
    for k3, nm in enumerate(('w_q', 'w_k', 'w_v')):
        P.op('gpsimd', lambda e, k3=k3, nm=nm: e.dma_start(out=wqkv[:, :, k3 * 512:(k3 + 1) * 512], in_=a[nm].rearrange("(j p) n -> p j n", p=128)),
             writes=[d_w], dma=True)
    P.op('gpsimd', lambda e: e.dma_start(out=wgt[:], in_=a['w_gate'].rearrange("(j p) n -> p j n", p=128)), writes=[d_w], dma=True)
    for r_ in range(2):
        P.op('gpsimd', lambda e, r_=r_: e.dma_start(out=wba[:, :, r_, 0:4], in_=a['w_ba'][:, r_ * 4:(r_ + 1) * 4].rearrange("(j p) n -> p j n", p=128)), writes=[d_w], dma=True)

    Sf = sb([128, 4, 128], F32); Sb = sb([128, 4, 128], BF16); d_S = P.deps(4)
    for h in range(4):
        P.op('vector', lambda e, h=h: e.memset(Sf[:, h, :], 0.0), writes=[d_S[h]])
        P.op('vector', lambda e, h=h: e.memset(Sb[:, h, :], 0.0), writes=[d_S[h]])
    praw = sb([128, 12, 3 + DB], F32); d_praw = P.dep()
    P.op('vector', lambda e: e.memset(praw[:], 0.0), writes=[d_praw])

    xt = [sb([128, D], F32) for _ in range(2)]; d_xt = P.deps(2)
    hT = sb([128, 8, DB], BF16); d_hT = P.dep()
    cv = sb([128, 12, DB], F32); d_cv = P.dep()
    sq = sb([128, 8, DB], BF16); rn = sb([128, 8, DB], F32); d_rn = P.dep()
    onesb = sb([128, 128], BF16)
    P.op('vector', lambda e: e.memset(onesb[:], 1.0), writes=[d_c])
    qn = sb([128, 4, DB], F32); kn = sb([128, 4, DB], F32); d_qk = P.dep()
    sgate = sb([128, 4, DB], F32); d_sg = P.dep()
    RS = sb([128, 5, DB], F32); d_RS = P.dep()
    P.op('vector', lambda e: e.memset(RS[:], 0.0), writes=[d_RS])
    rtmp = sb([4, DB], F32)
    cols = sb([128, 2, 5, 4], F32); d_cols = P.dep()
    dl = sb([128, 4, 4], F32); d_dl = P.dep()
    kTb = sb([128, 4, DB], BF16); kbTb = sb([128, 4, DB], BF16); qTb = sb([128, 4, DB], BF16); qgTb = sb([128, 4, DB], BF16); d_fm = P.dep()
    vbeta = sb([128, 2, 4, 128], BF16); kbg = sb([128, 2, 4, 128], BF16); kend = sb([128, 2, 4, 128], BF16); d_tm = P.deps(2)
    dtmp = sb([128, 4, 128], F32); Dm = sb([128, 4, 128], F32); DmT = sb([128, 4, 128], F32); Ds = sb([128, 4, 128], F32); DsT = sb([128, 4, 128], F32)
    d_D = P.dep()
    X = [sb([128, 4, 128], BF16) for _ in range(2)]; XT = [sb([128, 4, 128], BF16) for _ in range(2)]; TT = [sb([128, 4, 128], BF16) for _ in range(2)]
    d_X = P.deps(2); d_XT = P.deps(2); d_TT = P.deps(2)
    attnT = sb([128, 4, 128], BF16); d_at = P.dep()
    wsb = sb([128, 4, 128], F32); kcT = sb([128, 4, 128], BF16); d_wk = P.dep()
    vnew = [[sb([128, 128], BF16) for _ in range(4)] for _ in range(2)]; d_vn = [P.deps(4) for _ in range(2)]
    for c2_ in range(2):
        for h_ in range(4):
            P.op('vector', lambda e, c2_=c2_, h_=h_: e.memset(vnew[c2_][h_][:], 0.0), writes=[d_vn[c2_][h_]])
    osb = [sb([128, 128], F32) for _ in range(2)]; osq = [sb([128, 128], BF16) for _ in range(2)]; d_o = P.deps(2)
    rstd = [sb([128, 128], F32) for _ in range(2)]; d_rs = P.deps(2)
    yt = [sb([128, 128], F32) for _ in range(2)]; d_yt = P.deps(2)
    osb4 = sb([128, 512], F32); osq4 = sb([128, 512], BF16); rstd4 = sb([128, 512], F32); yt4 = sb([128, 512], F32)
    d_o4, d_rs4, d_yt4 = P.deps(3)

    def v3(t):
        return t[:].rearrange("p (h f) -> p h f", h=4)

    for n in range(NB):
        emit_bg(P, bgq, n, NB)
        t0 = n * DB
        emit_hT_block(cx, a['x'], t0, 2, xt, d_xt, pa, d_pa, modcol, d_mod, hT, d_hT)
        for ct in range(12):
            pb = ct % 2
            for j in range(8):
                P.op('tensor', lambda e, ct=ct, j=j, pb=pb: e.matmul(pa[pb][:, 0:DB], lhsT=wqkv[:, j, ct * 128:(ct + 1) * 128], rhs=hT[:, j, :],
                                                                   start=(j == 0), stop=(j == 7)), reads=[d_w, d_hT], writes=[d_pa[pb]])
            P.op('scalar', lambda e, ct=ct, pb=pb: e.activation(out=praw[:, ct, 3:3 + DB], in_=pa[pb][:, 0:DB], func=AF.Identity),
                 writes=[d_pa[pb], d_praw])
        for h in range(4):
            pb = h % 2
            for j in range(8):
                P.op('tensor', lambda e, h=h, j=j, pb=pb: e.matmul(pa[pb][:, 0:DB], lhsT=wgt[:, j, h * 128:(h + 1) * 128], rhs=hT[:, j, :],
                                                                 start=(j == 0), stop=(j == 7)), reads=[d_w, d_hT], writes=[d_pa[pb]])
            P.op('scalar', lambda e, h=h, pb=pb: e.activation(out=sgate[:, h, :], in_=pa[pb][:, 0:DB], func=AF.Silu), writes=[d_pa[pb], d_sg])
        for r in range(2):
            for j in range(8):
                P.op('tensor', lambda e, r=r, j=j: e.matmul(bg[:, r * DB:(r + 1) * DB], lhsT=wba[:, j, r, :], rhs=hT[:, j, :],
                                                          start=(j == 0), stop=(j == 7)), reads=[d_w, d_hT], writes=[d_bg])
        P.op('scalar', lambda e: e.activation(out=RS[0:4, 0, :], in_=bg[0:4, 0:DB], func=AF.Sigmoid), writes=[d_bg, d_RS])
        P.op('scalar', lambda e: e.activation(out=rtmp[:], in_=bg[0:4, DB:2 * DB], func=AF.Exp, bias=dtb[:], scale=1.0), reads=[d_c], writes=[d_bg, d_RS])
        P.op('scalar', lambda e: e.activation(out=rtmp[:], in_=rtmp[:], func=AF.Ln, bias=cx.ones[0:4, 0:1], scale=1.0), reads=[d_c], writes=[d_RS])
        P.op('vector', lambda e: e.tensor_scalar(out=rtmp[:], in0=rtmp[:], scalar1=negA[:, 0:1], scalar2=None, op0=ALU.mult), reads=[d_c], writes=[d_RS])
        P.op('vector', lambda e: e.tensor_tensor_scan(out=RS[0:4, 1, :], data0=reset[:], data1=rtmp[:], initial=0.0, op0=ALU.mult, op1=ALU.add),
             reads=[d_c], writes=[d_RS])
        P.op('scalar', lambda e: e.activation(out=RS[0:4, 2, :], in_=RS[0:4, 1, :], func=AF.Exp), writes=[d_RS])
        P.op('vector', lambda e: e.tensor_tensor(out=RS[0:4, 3, :], in0=RS[0:4, 0, :], in1=RS[0:4, 2, :], op=ALU.mult), writes=[d_RS])
        P.op('vector', lambda e: e.tensor_tensor(out=rtmp[:].rearrange("p (c t) -> p c t", t=64),
                                                 in0=RS[0:4, 1, :].rearrange("p (c t) -> p c t", t=64)[:, :, 63:64].broadcast_to([4, 4, 64]),
                                                 in1=RS[0:4, 1, :].rearrange("p (c t) -> p c t", t=64), op=ALU.subtract), writes=[d_RS])
        P.op('scalar', lambda e: e.activation(out=RS[0:4, 4, :], in_=rtmp[:], func=AF.Exp), writes=[d_RS])
        for i in range(2):
            for q in range(5):
                P.op('tensor', lambda e, i=i, q=q: e.matmul(bg[:, (i * 5 + q) * 4:(i * 5 + q) * 4 + 4], lhsT=RS[:, q, i * 128:(i + 1) * 128], rhs=cx.ident[:, 0:4], start=True, stop=True),
                     reads=[d_RS, d_c], writes=[d_bg])
        P.op('vector', lambda e: e.tensor_copy(out=cols[:].rearrange("p i q h -> p (i q h)"), in_=bg[:, 0:40]), writes=[d_bg, d_cols])
        for ct in range(12):
            P.op('vector', lambda e, ct=ct: e.tensor_scalar(out=cv[:, ct, :], in0=praw[:, ct, 0:DB], scalar1=convw[:, ct, 0:1], scalar2=None, op0=ALU.mult),
                 reads=[d_praw, d_c], writes=[d_cv])
            for tap in range(1, 4):
                P.op('vector', lambda e, ct=ct, tap=tap: e.scalar_tensor_tensor(out=cv[:, ct, :], in0=praw[:, ct, tap:tap + DB], scalar=convw[:, ct, tap:tap + 1],
                                                                                 in1=cv[:, ct, :], op0=ALU.mult, op1=ALU.add),
                     reads=[d_praw, d_c], writes=[d_cv])
        P.op('gpsimd', lambda e: e.tensor_copy(out=praw[:, :, 0:3], in_=praw[:, :, DB:DB + 3]), writes=[d_praw])
        P.op('scalar', lambda e: e.activation(out=cv[:].rearrange("p c t -> p (c t)"), in_=cv[:].rearrange("p c t -> p (c t)"), func=AF.Silu), writes=[d_cv])
        P.op('scalar', lambda e: e.activation(out=sq[:].rearrange("p c t -> p (c t)"), in_=cv[:, 0:8, :].rearrange("p c t -> p (c t)"), func=AF.Square),
             reads=[d_cv], writes=[d_rn])
        for pr in range(4):
            bank, d_bank = ((b1, d_b1), (b2, d_b2))[pr % 2]
            for u in range(2):
                ct = pr * 2 + u
                P.op('tensor', lambda e, bank=bank, u=u, ct=ct: e.matmul(bank[:, u * DB:(u + 1) * DB], lhsT=onesb[:], rhs=sq[:, ct, :], start=True, stop=True),
                     reads=[d_c, d_rn], writes=[d_bank])
            P.op('scalar', lambda e, bank=bank, pr=pr: e.activation(out=rn[:, pr * 2:pr * 2 + 2, :].rearrange("p c t -> p (c t)"), in_=bank[:], func=AF.Ln,
                                                                    bias=neps[:], scale=1.0), reads=[d_c], writes=[d_bank, d_rn])
        P.op('scalar', lambda e: e.activation(out=rn[:].rearrange("p c t -> p (c t)"), in_=rn[:].rearrange("p c t -> p (c t)"), func=AF.Exp, scale=-0.5),
             writes=[d_rn])
        P.op('vector', lambda e: e.scalar_tensor_tensor(out=qn[:], in0=cv[:, 0:4, :], scalar=128 ** -0.5, in1=rn[:, 0:4, :], op0=ALU.mult, op1=ALU.mult),
             reads=[d_cv, d_rn], writes=[d_qk])
        P.op('vector', lambda e: e.tensor_tensor(out=kn[:], in0=cv[:, 4:8, :], in1=rn[:, 4:8, :], op=ALU.mult), reads=[d_cv, d_rn], writes=[d_qk])
        P.op('gpsimd', lambda e: e.tensor_copy(out=kTb[:], in_=kn[:]), reads=[d_qk], writes=[d_fm])
        P.op('gpsimd', lambda e: e.tensor_copy(out=qTb[:], in_=qn[:]), reads=[d_qk], writes=[d_fm])
        for h in range(4):
            for q2, qi in enumerate((0, 2)):
                P.op('tensor', lambda e, h=h, q2=q2, qi=qi: e.matmul(bg[:, q2 * DB:(q2 + 1) * DB], lhsT=sel[:, h, :], rhs=RS[:, qi, :], start=True, stop=True),
                     reads=[d_c, d_RS], writes=[d_bg])
            P.op('vector', lambda e, h=h: e.tensor_tensor(out=kbTb[:, h, :], in0=kn[:, h, :], in1=bg[:, 0:DB], op=ALU.mult), reads=[d_qk], writes=[d_bg, d_fm])
            P.op('vector', lambda e, h=h: e.tensor_tensor(out=qgTb[:, h, :], in0=qn[:, h, :], in1=bg[:, DB:2 * DB], op=ALU.mult), reads=[d_qk], writes=[d_bg, d_fm])
            P.op('vector', lambda e, h=h: e.tensor_copy(out=dl[:, h, :], in_=bg[:, DB:2 * DB].rearrange("p (c t) -> p c t", t=64)[:, :, 63]),
                 writes=[d_bg, d_dl])
        for i in range(2):
            c0 = i * 128
            for h in range(4):
                P.op('tensor', lambda e, h=h, c0=c0: e.transpose(b1[:, h * 128:(h + 1) * 128], cv[:, 8 + h, c0:c0 + 128], cx.ident[:]),
                     reads=[d_cv, d_c], writes=[d_b1])
                P.op('tensor', lambda e, h=h, c0=c0: e.transpose(b2[:, h * 128:(h + 1) * 128], kn[:, h, c0:c0 + 128], cx.ident[:]),
                     reads=[d_qk, d_c], writes=[d_b2])
            P.op('vector', lambda e, i=i: e.tensor_tensor(out=vbeta[:, i, :, :], in0=v3(b1), in1=cols[:, i, 0, :].unsqueeze(2).broadcast_to([128, 4, 128]), op=ALU.mult),
                 reads=[d_cols], writes=[d_b1, d_tm[i]])
            P.op('vector', lambda e, i=i: e.tensor_tensor(out=kbg[:, i, :, :], in0=v3(b2), in1=cols[:, i, 3, :].unsqueeze(2).broadcast_to([128, 4, 128]), op=ALU.mult),
                 reads=[d_cols], writes=[d_b2, d_tm[i]])
            P.op('vector', lambda e, i=i: e.tensor_tensor(out=kend[:, i, :, :], in0=v3(b2), in1=cols[:, i, 4, :].unsqueeze(2).broadcast_to([128, 4, 128]), op=ALU.mult),
                 reads=[d_cols], writes=[d_b2, d_tm[i]])
            for h in range(4):
                P.op('tensor', lambda e, h=h, c0=c0: e.matmul(bg[:, h * 128:(h + 1) * 128], lhsT=sel[:, h, :], rhs=RS[:, 1, c0:c0 + 128], start=True, stop=True),
                     reads=[d_c, d_RS], writes=[d_bg])
            gcol = cols[:, i, 1, :].unsqueeze(2).broadcast_to([128, 4, 128])
            P.op('vector', lambda e, gcol=gcol: e.tensor_tensor(out=dtmp[:], in0=gcol, in1=v3(bg), op=ALU.subtract), reads=[d_cols], writes=[d_bg, d_D])
            P.op('gpsimd', lambda e: e.tensor_tensor(out=dtmp[:], in0=dtmp[:], in1=masks[:, 0, :].unsqueeze(1).broadcast_to([128, 4, 128]), op=ALU.add),
                 reads=[d_c], writes=[d_D])
            P.op('scalar', lambda e: e.activation(out=Dm[:].rearrange("p h f -> p (h f)"), in_=dtmp[:].rearrange("p h f -> p (h f)"), func=AF.Exp), writes=[d_D])
            P.op('gpsimd', lambda e: e.tensor_tensor(out=Ds[:], in0=Dm[:], in1=masks[:, 2, :].unsqueeze(1).broadcast_to([128, 4, 128]), op=ALU.mult),
                 reads=[d_c], writes=[d_D])
            P.op('vector', lambda e, gcol=gcol: e.tensor_tensor(out=dtmp[:], in0=v3(bg), in1=gcol, op=ALU.subtract), reads=[d_cols], writes=[d_bg, d_D])
            P.op('gpsimd', lambda e: e.tensor_tensor(out=dtmp[:], in0=dtmp[:], in1=masks[:, 1, :].unsqueeze(1).broadcast_to([128, 4, 128]), op=ALU.add),
                 reads=[d_c], writes=[d_D])
            P.op('scalar', lambda e: e.activation(out=DmT[:].rearrange("p h f -> p (h f)"), in_=dtmp[:].rearrange("p h f -> p (h f)"), func=AF.Exp), writes=[d_D])
            P.op('gpsimd', lambda e: e.tensor_tensor(out=DsT[:], in0=DmT[:], in1=masks[:, 3, :].unsqueeze(1).broadcast_to([128, 4, 128]), op=ALU.mult),
                 reads=[d_c], writes=[d_D])
            for h in range(4):
                P.op('tensor', lambda e, h=h, c0=c0: e.matmul(b1[:, h * 128:(h + 1) * 128], lhsT=kbTb[:, h, c0:c0 + 128], rhs=kTb[:, h, c0:c0 + 128], start=True, stop=True),
                     reads=[d_fm], writes=[d_b1])
                P.op('tensor', lambda e, h=h, c0=c0: e.matmul(b2[:, h * 128:(h + 1) * 128], lhsT=kTb[:, h, c0:c0 + 128], rhs=kbTb[:, h, c0:c0 + 128], start=True, stop=True),
                     reads=[d_fm], writes=[d_b2])
                P.op('tensor', lambda e, h=h, c0=c0: e.matmul(b3[:, h * 128:(h + 1) * 128], lhsT=kTb[:, h, c0:c0 + 128], rhs=qTb[:, h, c0:c0 + 128], start=True, stop=True),
                     reads=[d_fm], writes=[d_b3])
            P.op('vector', lambda e: e.scalar_tensor_tensor(out=X[0][:], in0=v3(b1), scalar=-1.0, in1=Ds[:], op0=ALU.mult, op1=ALU.mult), reads=[d_D], writes=[d_b1, d_X[0]])
            P.op('vector', lambda e: e.scalar_tensor_tensor(out=XT[0][:], in0=v3(b2), scalar=-1.0, in1=DsT[:], op0=ALU.mult, op1=ALU.mult), reads=[d_D], writes=[d_b2, d_XT[0]])
            P.op('vector', lambda e: e.tensor_tensor(out=attnT[:], in0=v3(b3), in1=DmT[:], op=ALU.mult), reads=[d_D], writes=[d_b3, d_at])
            P.op('gpsimd', lambda e: e.tensor_tensor(out=TT[0][:], in0=XT[0][:], in1=identb[:].unsqueeze(1).broadcast_to([128, 4, 128]), op=ALU.add),
                 reads=[d_XT[0], d_c], writes=[d_TT[0]])
            cur = 0
            for k in range(1, 6):
                nx = 1 - cur
                for h in range(4):
                    P.op('tensor', lambda e, h=h, cur=cur: e.matmul(b1[:, h * 128:(h + 1) * 128], lhsT=XT[cur][:, h, :], rhs=X[cur][:, h, :], start=True, stop=True),
                         reads=[d_X[cur], d_XT[cur]], writes=[d_b1])
                if k < 5:
                    for h in range(4):
                        P.op('tensor', lambda e, h=h, cur=cur: e.matmul(b2[:, h * 128:(h + 1) * 128], lhsT=X[cur][:, h, :], rhs=XT[cur][:, h, :], start=True, stop=True),
                             reads=[d_X[cur], d_XT[cur]], writes=[d_b2])
                P.op('scalar', lambda e, nx=nx: e.activation(out=X[nx][:].rearrange("p h f -> p (h f)"), in_=b1[:], func=AF.Identity), writes=[d_b1, d_X[nx]])
                if k < 5:
                    P.op('vector', lambda e, nx=nx: e.tensor_copy(out=XT[nx][:].rearrange("p h f -> p (h f)"), in_=b2[:]), writes=[d_b2, d_XT[nx]])
                for h in range(4):
                    P.op('tensor', lambda e, h=h, cur=cur, nx=nx: e.matmul(b3[:, h * 128:(h + 1) * 128], lhsT=X[nx][:, h, :], rhs=TT[cur][:, h, :], start=True, stop=True),
                         reads=[d_X[nx], d_TT[cur]], writes=[d_b3])
                P.op('vector', lambda e, cur=cur, nx=nx: e.tensor_tensor(out=TT[nx][:], in0=v3(b3), in1=TT[cur][:], op=ALU.add), reads=[d_TT[cur]], writes=[d_b3, d_TT[nx]])
                cur = nx
            tt = cur
            for h in range(4):
                P.op('tensor', lambda e, h=h, i=i, tt=tt: e.matmul(b2[:, h * 128:(h + 1) * 128], lhsT=TT[tt][:, h, :], rhs=vbeta[:, i, h, :], start=True, stop=True),
                     reads=[d_TT[tt], d_tm[i]], writes=[d_b2])
                P.op('tensor', lambda e, h=h, i=i, tt=tt: e.matmul(b3[:, h * 128:(h + 1) * 128], lhsT=kbg[:, i, h, :], rhs=TT[tt][:, h, :], start=True, stop=True),
                     reads=[d_TT[tt], d_tm[i]], writes=[d_b3])
            P.op('scalar', lambda e: e.activation(out=wsb[:].rearrange("p h f -> p (h f)"), in_=b2[:], func=AF.Identity), writes=[d_b2, d_wk])
            P.op('vector', lambda e: e.tensor_copy(out=kcT[:].rearrange("p h f -> p (h f)"), in_=b3[:]), writes=[d_b3, d_wk])
            for c2 in range(2):
                cs = c0 + c2 * 64
                rows = slice(c2 * 64, (c2 + 1) * 64)
                cl = slice(c2 * 64, (c2 + 1) * 64)
                ci = i * 2 + c2
                for h in range(4):
                    P.op('tensor', lambda e, h=h, rows=rows, cl=cl: e.matmul(pV[:, h * 128:(h + 1) * 128], lhsT=kcT[:, h, :], rhs=Sb[:, h, :], start=True, stop=True),
                         reads=[d_wk, d_S[h]], writes=[d_pV])
                    P.op('vector', lambda e, h=h, rows=rows, c2=c2: e.tensor_tensor(out=vnew[c2][h][rows, :], in0=wsb[rows, h, :], in1=pV[rows, h * 128:(h + 1) * 128], op=ALU.subtract),
                         reads=[d_wk], writes=[d_pV, d_vn[c2][h]])
                for h in range(4):
                    P.op('tensor', lambda e, h=h, cs=cs, cl=cl: e.matmul(pO[:, h * 128 + c2 * 64:h * 128 + (c2 + 1) * 64] if False else pO[:, h * 128 + cl.start:h * 128 + cl.stop],
                                                                       lhsT=Sb[:, h, :], rhs=qgTb[:, h, cs:cs + 64], start=True, stop=False),
                         reads=[d_S[h], d_fm], writes=[d_pO])
                    P.op('tensor', lambda e, h=h, rows=rows, cl=cl, c2=c2: e.matmul(pO[:, h * 128 + cl.start:h * 128 + cl.stop], lhsT=vnew[c2][h][:], rhs=attnT[:, h, cl],
                                                                           start=False, stop=True), reads=[d_vn[c2][h], d_at], writes=[d_pO])
                    P.op('tensor', lambda e, h=h, c2=c2, i=i: e.matmul(b1[:, h * 128:(h + 1) * 128], lhsT=kend[:, i, h, :], rhs=vnew[c2][h][:], start=True, stop=True),
                         reads=[d_vn[c2][h], d_tm[i]], writes=[d_b1])
                for h in range(4):
                    P.op('vector', lambda e, h=h, ci=ci: e.scalar_tensor_tensor(out=Sf[:, h, :], in0=Sf[:, h, :], scalar=dl[:, h, ci:ci + 1], in1=b1[:, h * 128:(h + 1) * 128],
                                                                                 op0=ALU.mult, op1=ALU.add), reads=[d_dl], writes=[d_b1, d_S[h]])
                    P.op('gpsimd', lambda e, h=h: e.tensor_copy(out=Sb[:, h, :], in_=Sf[:, h, :]), writes=[d_S[h]])
            P.op('scalar', lambda e: e.activation(out=osb4[:], in_=pO[:], func=AF.Identity), writes=[d_pO, d_o4])
            P.op('scalar', lambda e: e.activation(out=osq4[:], in_=osb4[:], func=AF.Square), writes=[d_o4])
            P.op('tensor', lambda e: e.matmul(bg[:], lhsT=onesb[:], rhs=osq4[:], start=True, stop=True), reads=[d_c, d_o4], writes=[d_bg])
            P.op('scalar', lambda e: e.activation(out=rstd4[:], in_=bg[:], func=AF.Ln, bias=neps[:], scale=1.0 / 128.0), reads=[d_c], writes=[d_bg, d_rs4])
            P.op('scalar', lambda e: e.activation(out=rstd4[:], in_=rstd4[:], func=AF.Exp, scale=-0.5), writes=[d_rs4])
            P.op('vector', lambda e: e.scalar_tensor_tensor(out=yt4[:], in0=osb4[:], scalar=wcol[:, 0:1], in1=rstd4[:], op0=ALU.mult, op1=ALU.mult),
                 reads=[d_o4, d_rs4, d_c], writes=[d_yt4])
            P.op('vector', lambda e, c0=c0: e.tensor_tensor(out=yt4[:].rearrange("p (h f) -> p h f", h=4), in0=yt4[:].rearrange("p (h f) -> p h f", h=4),
                                                            in1=sgate[:, :, c0:c0 + 128], op=ALU.mult), reads=[d_sg], writes=[d_yt4])
            for h in range(4):
                P.op('sync', lambda e, h=h, t0=t0, c0=c0: e.dma_start(out=a['oT'][h * 128:(h + 1) * 128, t0 + c0:t0 + c0 + 128], in_=yt4[:, h * 128:(h + 1) * 128]),
                     reads=[d_yt4], dma=True, is_out=True)
    P.emit()

SEQ = 8192
NB_ = 4
NCORES = 4


def _col(v, n):
    return np.ascontiguousarray(np.asarray(v, np.float32).reshape(n, 128).T)


_NC_CACHE = {}


def _build_fused(S=SEQ):
    if S in _NC_CACHE:
        return _NC_CACHE[S]
    nc = bass.Bass("TRN2", target_bir_lowering=False)
    di = lambda name, shape: dram_in(nc, name, shape)
    I = dict(x=di("x", [S, D]), ccol=di("ccol", [128, 8]), ada_w=di("ada_w", [2, D, 6 * D]), ada_b_col=di("ada_b_col", [2, 128, 48]),
             ada_b_row=di("ada_b_row", [2, 1, 6 * D]), ident=di("ident", [128, 128]), ln=di("ln", [2, 4, D]),
             ev_w_in=di("ev_w_in", [D, 3088]), rtab=di("rtab", [4, 128, RW]), eoh=di("eoh", [32, 32 * 128]),
             gk_w2=di("gk_w2", [16, 256]), gk_b_col=di("gk_b_col", [2, 128, 1]), ev_norm_col=di("ev_norm_col", [128, 1]),
             reset_g=di("reset_g", [128, GB]), maskT_g=di("maskT_g", [128, 128]), ev_w_out=di("ev_w_out", [D, D]),
             od_w_in=di("od_w_in", [D, 4112]), w_ba=di("w_ba", [2, D, 8]), convw_col=di("convw_col", [2, 128, 12, 4]),
             dt_bias_col=di("dt_bias_col", [2, 4, 1]), a_log_col=di("a_log_col", [2, 4, 1]), od_norm_col=di("od_norm_col", [128, 1]),
             reset_d=di("reset_d", [4, DB]), masks_d=di("masks_d", [4, 128, 128]), sel_d=di("sel_d", [4, 4, 128]), od_w_out=di("od_w_out", [D, D]),
             router_w=di("router_w", [2, D, NE]), router_b=di("router_b", [2, 1, NE]),
             wg=di("wg", [2, NE, D, 256]), wu=di("wu", [2, NE, D, 256]), wd=di("wd", [2, NE, 256, D]),
             shg=di("shg", [2, D, 256]), shu=di("shu", [2, D, 256]), shd=di("shd", [2, 256, D]))
    y = dram_out(nc, "y", [S, D])
    mixT = nc.dram_tensor("mixT_scratch", [D, S], F32, kind="Internal").ap()
    xmid = nc.dram_tensor("xmid_scratch", [S, D], F32, kind="Internal").ap()

    def stage(fn, *args, **kw):
        with nc.cleanup_on_exit():
            with ExitStack() as es:
                fn(nc, es, *args, **kw)

    wbf = [nc.dram_tensor("wbf_scratch%d" % l, [NE + 1, 128, 6144], BF16, kind="Internal").ap() for l in range(2)]

    def conv_thunks(layer, e_lo, e_hi):
        th = []
        for e_i in range(e_lo, e_hi):
            if e_i < NE:
                srcs = (I['wg'][layer][e_i], I['wu'][layer][e_i], I['wd'][layer][e_i])
            else:
                srcs = (I['shg'][layer], I['shu'][layer], I['shd'][layer])
            dst = wbf[layer][e_i]
            for k3, (lo, n_) in enumerate(((0, 256), (2048, 256), (4096, D))):
                th.append(lambda e, s_=srcs[k3], d_=dst, lo=lo, n_=n_: e.dma_start(
                    out=d_[:, lo:lo + 2048].rearrange("p (j n) -> p j n", n=n_), in_=s_.rearrange("(j p) n -> p j n", p=128)))
        return th

    def com(layer, xin):
        return dict(x=xin, ccol=I['ccol'], ada_w=I['ada_w'][layer], ada_b_col=I['ada_b_col'][layer], ident=I['ident'])

    w = I['ev_w_in']
    for g in range(2):
        hs = slice(g * 256, (g + 1) * 256)
        stage(emit_stage_moba, S, dict(com(0, I['x']), w_q=w[:, hs], w_k=w[:, 512 + g * 256:512 + (g + 1) * 256],
                                       w_v=w[:, 1024 + g * 256:1024 + (g + 1) * 256], rtab=I['rtab'][2 * g:2 * g + 2], eoh=I['eoh'],
                                       oT=mixT[g * 256:(g + 1) * 256, :]), bg=conv_thunks(0, (0, 23)[g], (23, 46)[g]))
    for g in range(2):
        ks = slice(g * 128, (g + 1) * 128)
        stage(emit_stage_gla, S, dict(com(0, I['x']), w_gq=w[:, 1536 + g * 128:1536 + (g + 1) * 128], w_gk=w[:, 1792 + g * 128:1792 + (g + 1) * 128],
                                      w_gv=w[:, 2048 + g * 256:2048 + (g + 1) * 256], w_gg=w[:, 2560 + g * 256:2560 + (g + 1) * 256],
                                      w_glr=w[:, 3072:3088], gk_w2=I['gk_w2'][:, ks], gk_b_col=I['gk_b_col'][g], norm_col=I['ev_norm_col'],
                                      reset=I['reset_g'], maskT=I['maskT_g'], oT=mixT[512 + g * 256:512 + (g + 1) * 256, :]),
              bg=conv_thunks(0, (46, 56)[g], (56, 65)[g]))

    def cargs(layer, xin, w_out, out):
        return dict(mixT=mixT, x=xin, w_out=w_out, ccol=I['ccol'], ada_w=I['ada_w'][layer], ada_b_row=I['ada_b_row'][layer],
                    ada_b_col=I['ada_b_col'][layer], ln=I['ln'][layer], router_w=I['router_w'][layer], router_b=I['router_b'][layer],
                    wbf=wbf[layer],
                    ident=I['ident'], out=out)

    stage(emit_stage_c, S, cargs(0, I['x'], I['ev_w_out'], xmid))
    w = I['od_w_in']
    for g in range(2):
        stage(emit_stage_gdn, S, dict(com(1, xmid), w_q=w[:, g * 512:(g + 1) * 512], w_k=w[:, 1024 + g * 512:1024 + (g + 1) * 512],
                                      w_v=w[:, 2048 + g * 512:2048 + (g + 1) * 512], w_gate=w[:, 3072 + g * 512:3072 + (g + 1) * 512],
                                      w_ba=I['w_ba'][g], convw_col=I['convw_col'][g], dt_bias_col=I['dt_bias_col'][g], a_log_col=I['a_log_col'][g],
                                      norm_col=I['od_norm_col'], reset=I['reset_d'], masks=I['masks_d'], sel=I['sel_d'],
                                      oT=mixT[g * 512:(g + 1) * 512, :]), bg=conv_thunks(1, (0, 33)[g], (33, 65)[g]))
    stage(emit_stage_c, S, cargs(1, xmid, I['od_w_out'], y))
    _NC_CACHE[S] = nc
    return nc


def host_inputs(x, c, rpe_bias, ada_w, ada_b, ln_mix_g, ln_mix_b, ln_ffn_g, ln_ffn_b,
                ev_w_in, ev_gk_w2, ev_gk_b, ev_norm, ev_w_out,
                od_w_in, od_conv_w, od_a_log, od_dt_bias, od_norm, od_w_out,
                moe_router_w, moe_router_b, moe_w_gate, moe_w_up, moe_w_down,
                sh_w_gate, sh_w_up, sh_w_down):
    f = lambda t: np.ascontiguousarray(np.asarray(t, dtype=np.float32))
    ada_b = f(ada_b)
    eoh = np.zeros((32, 32, 128), np.float32)
    for j in range(32):
        eoh[j, j, :] = 1.0
    reset_g, maskT_g = gla_host_consts()
    reset_d, masks_d, sel_d = gdn_host_consts()
    w_in1 = f(od_w_in)[0]
    convw = f(od_conv_w)[0]
    w_ba, cwc = [], []
    for g in range(2):
        w_ba.append(np.concatenate([w_in1[:, 4096 + g * 4:4096 + (g + 1) * 4], w_in1[:, 4104 + g * 4:4104 + (g + 1) * 4]], axis=1))
        cw = np.concatenate([convw[:, g * 512:(g + 1) * 512], convw[:, 1024 + g * 512:1024 + (g + 1) * 512],
                             convw[:, 2048 + g * 512:2048 + (g + 1) * 512]], axis=1)
        cwc.append(cw.reshape(4, 12, 128).transpose(2, 1, 0))
    return dict(
        ada_w=f(ada_w), ada_b_col=f(np.stack([_col(ada_b[l], 48) for l in range(2)])), ada_b_row=f(ada_b.reshape(2, 1, -1)),
        ident=np.eye(128, dtype=np.float32),
        ln=f(np.stack([np.stack([f(ln_mix_g)[l], f(ln_mix_b)[l], f(ln_ffn_g)[l], f(ln_ffn_b)[l]]) for l in range(2)])),
        ev_w_in=f(ev_w_in)[0], rtab=moba_host_tables(f(rpe_bias), [0, 1, 2, 3]), eoh=eoh.reshape(32, -1),
        gk_w2=f(ev_gk_w2)[0], gk_b_col=f(f(ev_gk_b)[0].reshape(2, 128, 1)), ev_norm_col=f(f(ev_norm)[0].reshape(128, 1)),
        reset_g=reset_g, maskT_g=maskT_g, ev_w_out=f(ev_w_out)[0],
        od_w_in=w_in1, w_ba=f(np.stack(w_ba)), convw_col=f(np.stack(cwc)),
        dt_bias_col=f(f(od_dt_bias)[0].reshape(2, 4, 1)), a_log_col=f(f(od_a_log)[0].reshape(2, 4, 1)), od_norm_col=f(f(od_norm)[0].reshape(128, 1)),
        reset_d=reset_d, masks_d=masks_d, sel_d=sel_d, od_w_out=f(od_w_out)[0],
        router_w=f(moe_router_w), router_b=f(f(moe_router_b).reshape(2, 1, -1)),
        wg=f(moe_w_gate), wu=f(moe_w_up), wd=f(moe_w_down), shg=f(sh_w_gate), shu=f(sh_w_up), shd=f(sh_w_down))


def kernel(x, c, **kw):
    f = lambda t: np.ascontiguousarray(np.asarray(t, dtype=np.float32))
    x = f(x); c = f(c)
    shared = host_inputs(x, c, **kw)
    nc = _build_fused()
    in_maps = [dict(shared, x=x[b], ccol=_col(c[b], 8)) for b in range(NCORES)]
    res = run_bass_kernel_spmd(nc, in_maps, core_ids=list(range(NCORES)))
    return np.stack([res.results[b]["y"] for b in range(NCORES)])
```
